# Optimizing a Trainium2 kernel written in Bass

```python
import jax, jax.numpy as jnp
from jax import lax
import numpy as np

D_MODEL = 1024
BATCH = 4
SEQ = 8192
DEPTH = 1
DEC_BATCH = 128
DEC_SEQ = 4
PAST_LEN = 16384
PAGE_SIZE = 128

HEAD_DIM = 64
ATTN_DIM = D_MODEL // 2
N_Q_HEADS = ATTN_DIM // HEAD_DIM
N_KV_HEADS = N_Q_HEADS // 4
KV_DIM = N_KV_HEADS * HEAD_DIM
CONV_DIM = D_MODEL - ATTN_DIM
CONV_GROUPS = CONV_DIM // HEAD_DIM
CONV_WIDTH = 3
WINDOW = 128
ROPE_THETA = 10000.0
IN_PROJ_DIM = ATTN_DIM + 2 * KV_DIM + 3 * CONV_DIM
N_EXPERT_GROUPS = 4
EXPERTS_PER_GROUP = 8
N_EXPERTS = N_EXPERT_GROUPS * EXPERTS_PER_GROUP
TOP_K_IN_GROUP = 2
EXPERT_HIDDEN = D_MODEL // 2
MOE_BLOCK = 128
RMS_EPS = 1e-5

kernel_name = "hybrid_swa_sink_shortconv_hmoe_step"


def _rmsnorm(x, gain):
    xf = x.astype(jnp.float32)
    xf = xf * lax.rsqrt(jnp.mean(xf * xf, axis=-1, keepdims=True) + RMS_EPS)
    return (xf * gain.astype(jnp.float32)).astype(x.dtype)


def _group_rmsnorm(x, gain, n_groups):
    shp = x.shape
    xg = x.astype(jnp.float32).reshape(shp[:-1] + (n_groups, shp[-1] // n_groups))
    xg = xg * lax.rsqrt(jnp.mean(xg * xg, axis=-1, keepdims=True) + RMS_EPS)
    return (xg.reshape(shp) * gain.astype(jnp.float32)).astype(x.dtype)


def _adaln(c, w_mod, b_mod):
    m = jax.nn.silu(c) @ w_mod + b_mod
    return jnp.split(m[:, None, :], 6, axis=-1)


def _modulated_norm(x, gain, shift, scale):
    return _rmsnorm(x, gain) * (1 + scale) + shift


def _rope(x, pos):
    half = HEAD_DIM // 2
    inv_freq = ROPE_THETA ** (-jnp.arange(half, dtype=jnp.float32) / half)
    ang = pos.astype(jnp.float32)[:, None] * inv_freq[None, :]
    cos = jnp.cos(ang)[None, :, None, :]
    sin = jnp.sin(ang)[None, :, None, :]
    xf = x.astype(jnp.float32)
    x1, x2 = xf[..., :half], xf[..., half:]
    return jnp.concatenate([x1 * cos - x2 * sin, x2 * cos + x1 * sin], axis=-1).astype(x.dtype)


def _mixer_inputs(h, w_in, pos):
    n, s, _ = h.shape
    splits = [ATTN_DIM, ATTN_DIM + KV_DIM, ATTN_DIM + 2 * KV_DIM,
              ATTN_DIM + 2 * KV_DIM + CONV_DIM, ATTN_DIM + 2 * KV_DIM + 2 * CONV_DIM]
    q, k, v, h_conv, gate_b, gate_c = jnp.split(h @ w_in, splits, axis=-1)
    q = _rope(q.reshape(n, s, N_Q_HEADS, HEAD_DIM), pos)
    k = _rope(k.reshape(n, s, N_KV_HEADS, HEAD_DIM), pos)
    v = v.reshape(n, s, N_KV_HEADS, HEAD_DIM)
    return q, k, v, gate_c * h_conv, gate_b


def _sink_attention(q, k, v, q_pos, k_pos, sinks):
    n, nb, nq, _, _ = q.shape
    g = N_Q_HEADS // N_KV_HEADS
    qg = q.reshape(n, nb, nq, N_KV_HEADS, g, HEAD_DIM)
    s = jnp.einsum("nbqkgd,nbskd->nbkgqs", qg, k).astype(jnp.float32) * (HEAD_DIM ** -0.5)
    qp = q_pos[:, :, None]
    kp = k_pos[:, None, :]
    valid = (kp <= qp) & (qp - kp < WINDOW) & (kp >= 0)
    s = jnp.where(valid[None, :, None, None], s, -jnp.inf)
    sink = sinks.astype(jnp.float32).reshape(N_KV_HEADS, g)[None, None, :, :, None, None]
    m = jnp.maximum(jnp.max(s, axis=-1, keepdims=True), sink)
    p = jnp.exp(s - m)
    p = p / (jnp.sum(p, axis=-1, keepdims=True) + jnp.exp(sink - m))
    o = jnp.einsum("nbkgqs,nbskd->nbqkgd", p.astype(v.dtype), v)
    return o.reshape(n, nb * nq, N_Q_HEADS * HEAD_DIM)


def _band_attention(q, k, v, sinks):
    n, s = q.shape[:2]
    nb = s // WINDOW
    qb = q.reshape(n, nb, WINDOW, N_Q_HEADS, HEAD_DIM)

    def band(t):
        tb = t.reshape(n, nb, WINDOW, N_KV_HEADS, HEAD_DIM)
        prev = jnp.concatenate([jnp.zeros_like(tb[:, :1]), tb[:, :-1]], axis=1)
        return jnp.concatenate([prev, tb], axis=2)

    q_pos = jnp.arange(s, dtype=jnp.int32).reshape(nb, WINDOW)
    k_pos = jnp.concatenate([q_pos - WINDOW, q_pos], axis=1)
    return _sink_attention(qb, band(k), band(v), q_pos, k_pos, sinks)


def _depthwise_conv(u_ext, conv_w):
    return lax.conv_general_dilated(
        u_ext, conv_w[:, None, :].astype(u_ext.dtype), window_strides=(1,), padding="VALID",
        dimension_numbers=("NWC", "WIO", "NWC"), feature_group_count=CONV_DIM)


def _hier_moe(h, w_group, b_group, w_expert, b_expert, w_gate, w_up, w_down):
    shp = h.shape
    x = h.reshape(-1, D_MODEL)
    m = x.shape[0]
    lg = (x @ w_group + b_group).astype(jnp.float32)
    pg = jax.nn.softmax(lg, axis=-1)
    grp = jnp.argmax(lg, axis=-1).astype(jnp.int32)
    pg_sel = jnp.take_along_axis(pg, grp[:, None], axis=-1)
    le = (x @ w_expert + b_expert).astype(jnp.float32).reshape(m, N_EXPERT_GROUPS, EXPERTS_PER_GROUP)
    le = jnp.take_along_axis(le, grp[:, None, None], axis=1)[:, 0]
    top_v, top_i = lax.top_k(le, TOP_K_IN_GROUP)
    gate = pg_sel * jax.nn.softmax(top_v, axis=-1)
    expert = grp[:, None] * EXPERTS_PER_GROUP + top_i.astype(jnp.int32)
    a = m * TOP_K_IN_GROUP
    flat_e = expert.reshape(-1)
    order = jnp.argsort(flat_e)
    sorted_e = flat_e[order]
    counts = jnp.bincount(flat_e, length=N_EXPERTS)
    start = jnp.cumsum(counts) - counts
    padded = (counts + MOE_BLOCK - 1) // MOE_BLOCK * MOE_BLOCK
    pend = jnp.cumsum(padded)
    pstart = pend - padded
    dest = pstart[sorted_e] + jnp.arange(a, dtype=jnp.int32) - start[sorted_e]
    n_blocks = -(-(a + N_EXPERTS * (MOE_BLOCK - 1)) // MOE_BLOCK)
    p_rows = n_blocks * MOE_BLOCK
    tok_buf = jnp.full((p_rows,), m, jnp.int32).at[dest].set((order // TOP_K_IN_GROUP).astype(jnp.int32))
    w_buf = jnp.zeros((p_rows,), jnp.float32).at[dest].set(gate.reshape(-1)[order])
    block_e = jnp.minimum(
        jnp.searchsorted(pend, jnp.arange(n_blocks, dtype=jnp.int32) * MOE_BLOCK, side="right"),
        N_EXPERTS - 1)
    x_pad = jnp.concatenate([x, jnp.zeros((1, D_MODEL), x.dtype)], axis=0)
    xb = x_pad[tok_buf].reshape(n_blocks, MOE_BLOCK, D_MODEL)

    def expert_block(args):
        xblk, e = args
        return (jax.nn.silu(xblk @ w_gate[e]) * (xblk @ w_up[e])) @ w_down[e]

    yb = lax.map(expert_block, (xb, block_e))
    y = jnp.zeros((m + 1, D_MODEL), jnp.float32).at[tok_buf].add(
        yb.reshape(p_rows, D_MODEL).astype(jnp.float32) * w_buf[:, None])
    return y[:m].astype(h.dtype).reshape(shp)


def _layer(x, c, pos, k_past, v_past, u_past, p):
    (attn_norm, ffn_norm, w_mod, b_mod, w_in, conv_w, sinks, gn_attn, gn_conv, w_out,
     w_group, b_group, w_expert, b_expert, w_gate, w_up, w_down) = p
    shift_a, scale_a, gate_a, shift_f, scale_f, gate_f = _adaln(c, w_mod, b_mod)
    h = _modulated_norm(x, attn_norm, shift_a, scale_a)
    q, k, v, u, gate_b = _mixer_inputs(h, w_in, pos)
    if k_past is None:
        attn_o = _band_attention(q, k, v, sinks)
        k_all, v_all = k, v
        u_all = jnp.pad(u, ((0, 0), (CONV_WIDTH - 1, 0), (0, 0)))
        keep = min(WINDOW, x.shape[1])
    else:
        keep = k_past.shape[1]
        k_all = jnp.concatenate([k_past, k], axis=1)
        v_all = jnp.concatenate([v_past, v], axis=1)
        k_pos = jnp.concatenate([pos[0] - keep + jnp.arange(keep, dtype=jnp.int32), pos])
        attn_o = _sink_attention(q[:, None], k_all[:, None], v_all[:, None], pos[None], k_pos[None], sinks)
        u_all = jnp.concatenate([u_past, u], axis=1)
    conv_o = gate_b * _depthwise_conv(u_all, conv_w)
    mix = jnp.concatenate([_group_rmsnorm(attn_o, gn_attn, N_Q_HEADS),
                           _group_rmsnorm(conv_o, gn_conv, CONV_GROUPS)], axis=-1) @ w_out
    x = x + gate_a * mix
    h = _modulated_norm(x, ffn_norm, shift_f, scale_f)
    x = x + gate_f * _hier_moe(h, w_group, b_group, w_expert, b_expert, w_gate, w_up, w_down)
    return x, k_all[:, -keep:], v_all[:, -keep:], u_all[:, -(CONV_WIDTH - 1):]


def setup_inputs(seed: int = 0) -> dict:
    key = jax.random.key(seed)
    ks = jax.random.split(key, 25)
    f32 = jnp.float32
    keep = min(WINDOW, PAST_LEN)
    d_in = D_MODEL ** -0.5

    def nrm(k, shape, s):
        return jax.random.normal(k, shape, f32) * s

    return {
        "x_prompt": nrm(ks[0], (BATCH, SEQ, D_MODEL), 1.0),
        "x_sample": nrm(ks[1], (DEC_BATCH, DEC_SEQ, D_MODEL), 1.0),
        "cache_k": nrm(ks[2], (DEPTH, DEC_BATCH, keep, N_KV_HEADS, HEAD_DIM), 1.0),
        "cache_v": nrm(ks[3], (DEPTH, DEC_BATCH, keep, N_KV_HEADS, HEAD_DIM), 1.0),
        "state_conv": nrm(ks[4], (DEPTH, DEC_BATCH, CONV_WIDTH - 1, CONV_DIM), 1.0),
        "c_prompt": nrm(ks[5], (BATCH, D_MODEL), 1.0),
        "c_sample": nrm(ks[6], (DEC_BATCH, D_MODEL), 1.0),
        "attn_norm": 1.0 + nrm(ks[7], (DEPTH, D_MODEL), 0.1),
        "ffn_norm": 1.0 + nrm(ks[8], (DEPTH, D_MODEL), 0.1),
        "w_mod": nrm(ks[9], (DEPTH, D_MODEL, 6 * D_MODEL), 0.5 * d_in),
        "b_mod": nrm(ks[10], (DEPTH, 6 * D_MODEL), 0.02),
        "w_in": nrm(ks[11], (DEPTH, D_MODEL, IN_PROJ_DIM), d_in),
        "conv_w": nrm(ks[12], (DEPTH, CONV_WIDTH, CONV_DIM), CONV_WIDTH ** -0.5),
        "attn_sinks": nrm(ks[13], (DEPTH, N_Q_HEADS), 1.0),
        "out_norm_attn": 1.0 + nrm(ks[14], (DEPTH, ATTN_DIM), 0.1),
        "out_norm_conv": 1.0 + nrm(ks[15], (DEPTH, CONV_DIM), 0.1),
        "w_out": nrm(ks[16], (DEPTH, D_MODEL, D_MODEL), d_in),
        "w_group": nrm(ks[17], (DEPTH, D_MODEL, N_EXPERT_GROUPS), d_in),
        "b_group": nrm(ks[18], (DEPTH, N_EXPERT_GROUPS), 0.01),
        "w_expert": nrm(ks[19], (DEPTH, D_MODEL, N_EXPERTS), d_in),
        "b_expert": nrm(ks[20], (DEPTH, N_EXPERTS), 0.01),
        "w_gate": nrm(ks[21], (DEPTH, N_EXPERTS, D_MODEL, EXPERT_HIDDEN), d_in),
        "w_up": nrm(ks[22], (DEPTH, N_EXPERTS, D_MODEL, EXPERT_HIDDEN), d_in),
        "w_down": nrm(ks[23], (DEPTH, N_EXPERTS, EXPERT_HIDDEN, D_MODEL), EXPERT_HIDDEN ** -0.5),
        "final_norm": 1.0 + nrm(ks[24], (D_MODEL,), 0.1),
    }


def reference(x_prompt, x_sample, cache_k, cache_v, state_conv, c_prompt, c_sample,
              attn_norm, ffn_norm, w_mod, b_mod, w_in, conv_w, attn_sinks,
              out_norm_attn, out_norm_conv, w_out, w_group, b_group, w_expert, b_expert,
              w_gate, w_up, w_down, final_norm):
    pos_p = jnp.arange(x_prompt.shape[1], dtype=jnp.int32)
    pos_s = PAST_LEN + jnp.arange(x_sample.shape[1], dtype=jnp.int32)
    xp, xs = x_prompt, x_sample
    kp_l, vp_l, up_l, ks_l, vs_l, us_l = [], [], [], [], [], []
    for l in range(DEPTH):
        p = (attn_norm[l], ffn_norm[l], w_mod[l], b_mod[l], w_in[l], conv_w[l], attn_sinks[l],
             out_norm_attn[l], out_norm_conv[l], w_out[l], w_group[l], b_group[l],
             w_expert[l], b_expert[l], w_gate[l], w_up[l], w_down[l])
        xp, kp, vp, up = _layer(xp, c_prompt, pos_p, None, None, None, p)
        xs, k_s, v_s, u_s = _layer(xs, c_sample, pos_s, cache_k[l], cache_v[l], state_conv[l], p)
        kp_l.append(kp)
        vp_l.append(vp)
        up_l.append(up)
        ks_l.append(k_s)
        vs_l.append(v_s)
        us_l.append(u_s)
    y_prompt = _rmsnorm(xp, final_norm)
    y_sample = _rmsnorm(xs, final_norm)
    return (y_prompt, y_sample, jnp.stack(kp_l), jnp.stack(vp_l), jnp.stack(up_l),
            jnp.stack(ks_l), jnp.stack(vs_l), jnp.stack(us_l))
```

```python
import numpy as np
import ml_dtypes
from contextlib import ExitStack
import concourse.bass as bass
import concourse.mybir as mybir
from concourse.bass_utils import run_bass_kernel_spmd

F32 = mybir.dt.float32
BF16 = mybir.dt.bfloat16
I32 = mybir.dt.int32
AF = mybir.ActivationFunctionType
ALU = mybir.AluOpType
AX = mybir.AxisListType

D = 1024
NPB = 32
NBLK = NPB + 1
NSEQ_S = 16
NTOK = NPB * 128 + 64
NTT = 33
HROWS = 4224
NB = 97
NE = 32
EPS = 1e-5
NEG = -30000.0
OOB = 4096.0
WROW = 12288
NS_W = 5
PIPE_WINDOW = 3
STREAM_STARTS = [0, 20, 40, 59, 78, 97]
SAME_ENGINE_SYNC = ("pool", "act", "dve")


class Buf:
    __slots__ = ("name", "w", "r", "excl")

    def __init__(self, name="", excl=False):
        self.name = name
        self.w = None
        self.r = []
        self.excl = excl


class Eng:
    def __init__(self, S, name, eng, sem):
        self.S = S
        self.name = name
        self.eng = eng
        self.sem = sem
        self.n = 0
        self.seen = {}

    @staticmethod
    def _need(ev, deps):
        if ev is None:
            return
        k, v = ev
        if deps.get(k, 0) < v:
            deps[k] = v

    def sync(self, reads, writes):
        deps = {}
        for b in reads:
            self._need(b.w, deps)
            if b.excl:
                for ev in b.r:
                    if ev[0] != self.name:
                        self._need(ev, deps)
        for b in writes:
            self._need(b.w, deps)
            for ev in b.r:
                self._need(ev, deps)
        for k, v in deps.items():
            if k == self.name and self.name not in SAME_ENGINE_SYNC:
                continue
            if self.seen.get(k, 0) >= v:
                continue
            self.eng.wait_ge(self.S.sems[k], v)
            self.seen[k] = v

    def op(self, fn, reads=(), writes=(), cost=None):
        if self.S.rec is not None:
            self.S.rec.append(("op", self, fn, tuple(reads), tuple(writes), cost))
            return None
        self.sync(reads, writes)
        ins = fn(self.eng)
        self.n += 1
        ins.then_inc(self.sem, 1)
        ev = (self.name, self.n)
        for b in reads:
            b.r.append(ev)
            if len(b.r) > 24:
                b.r = self._compact(b.r)
        for b in writes:
            b.w = ev
            b.r = []
        return ins

    @staticmethod
    def _compact(evs):
        best = {}
        for k, v in evs:
            if best.get(k, 0) < v:
                best[k] = v
        return list(best.items())

    def dma(self, dsem, out, in_, reads=(), writes=(), indirect=None, group=False, **kw):
        if self.S.rec is not None:
            kw2 = dict(kw)
            kw2.update(indirect=indirect, group=group)
            self.S.rec.append(("dma", self, (dsem, out, in_), tuple(reads), tuple(writes), kw2))
            return None
        self.sync(reads, writes)
        if (not group or self.name == "pool") and self.S.dcount[dsem] > 0:
            self.wait_ev((dsem, self.S.dcount[dsem]))
        if indirect is None:
            ins = self.eng.dma_start(out=out, in_=in_, **kw)
        else:
            ins = self.eng.indirect_dma_start(out=out, in_=in_, **indirect, **kw)
        self.S.dcount[dsem] += 16
        ins.then_inc(self.S.sems[dsem], 16)
        ev = (dsem, self.S.dcount[dsem])
        for b in reads:
            b.r.append(ev)
        for b in writes:
            b.w = ev
            b.r = []
        return ins

    def wait_ev(self, ev):
        k, v = ev
        if self.seen.get(k, 0) >= v:
            return
        self.eng.wait_ge(self.S.sems[k], v)
        self.seen[k] = v


class Sched:
    def __init__(self, nc, stack, n_dma_sems):
        self.nc = nc
        self.sems = {}
        self.dcount = {}
        self.E = {}
        self.rec = None
        for name, eng in (("pe", nc.tensor), ("act", nc.scalar), ("dve", nc.vector),
                          ("pool", nc.gpsimd), ("sp", nc.sync)):
            s = stack.enter_context(nc.semaphore("s_" + name))
            self.sems[name] = s
            self.E[name] = Eng(self, name, eng, s)
        self.free = []
        for i in range(n_dma_sems):
            k = "d%d" % i
            self.sems[k] = stack.enter_context(nc.semaphore(k))
            self.dcount[k] = 0
            self.free.append(k)

    def dsem(self):
        return self.free.pop()

    def defer(self, fn, reads=(), writes=()):
        if self.rec is not None:
            self.rec.append(("call", None, fn, tuple(reads), tuple(writes), None))
        else:
            fn()

    def record(self, gen):
        assert self.rec is None
        self.rec = []
        for _ in gen:
            pass
        r, self.rec = self.rec, None
        return r

    def merge_emit(self, progs, window=2):
        ENG_COST = {"pe": 0.09, "dve": 0.35, "act": 0.55, "pool": 1.3, "sp": 0.05}
        eng_free = {}
        buf_ready = {}
        active = []
        nxt = 0

        def pend(rs):
            pr, pw = {}, {}
            for rec in rs:
                for b in rec[3]:
                    pr[id(b)] = pr.get(id(b), 0) + 1
                for b in rec[4]:
                    pw[id(b)] = pw.get(id(b), 0) + 1
            return pr, pw

        while nxt < len(progs) or active:
            while nxt < len(progs) and len(active) < window:
                pr, pw = pend(progs[nxt])
                active.append([progs[nxt], 0, pr, pw])
                nxt += 1
            best = None
            for ai, a in enumerate(active):
                rec = a[0][a[1]]
                ok = True
                for o in active[:ai]:
                    for b in rec[4]:
                        if o[2].get(id(b), 0) or o[3].get(id(b), 0):
                            ok = False
                            break
                    if ok:
                        for b in rec[3]:
                            if o[3].get(id(b), 0):
                                ok = False
                                break
                    if not ok:
                        break
                if not ok:
                    continue
                kind, eng = rec[0], rec[1]
                en = eng.name if eng is not None else "sp"
                t0 = eng_free.get(en, 0.0)
                for b in rec[3] + rec[4]:
                    t0 = max(t0, buf_ready.get(id(b), 0.0))
                if best is None or t0 < best[0] - 1e-9:
                    best = (t0, ai, en)
            t0, ai, en = best
            a = active[ai]
            rec = a[0][a[1]]
            kind, eng = rec[0], rec[1]
            if kind == "op":
                eng.op(rec[2], reads=rec[3], writes=rec[4])
                c = rec[5] if rec[5] is not None else ENG_COST[en]
                eng_free[en] = t0 + c
                done = t0 + c + 0.15
            elif kind == "dma":
                dsem, out, in_ = rec[2]
                eng.dma(dsem, out, in_, reads=rec[3], writes=rec[4], **rec[5])
                eng_free[en] = t0 + 0.05
                done = t0 + 2.5
            else:
                rec[2]()
                done = t0
            for b in rec[4]:
                buf_ready[id(b)] = done
                a[3][id(b)] -= 1
            for b in rec[3]:
                a[2][id(b)] -= 1
            a[1] += 1
            if a[1] == len(a[0]):
                active.pop(ai)


class T:
    def __init__(self, t, name):
        self.t = t
        self.b = Buf(name)


def build_program():
    nc = bass.Bass("TRN2", target_bir_lowering=False)

    def din(name, shape, dt=F32):
        return nc.dram_tensor(name, list(shape), dt, kind="ExternalInput").ap()

    def dout(name, shape, dt=F32):
        return nc.dram_tensor(name, list(shape), dt, kind="ExternalOutput").ap()

    def dscr(name, shape, dt):
        return nc.dram_tensor(name, list(shape), dt, kind="Internal").ap()

    xp = din("xp", [NBLK * 128, D])
    xs = din("xs", [64, D])
    cT = din("cT", [D, 65])
    w_mod = din("w_mod", [D, 6 * D])
    b_mod = din("b_mod", [1, 6 * D])
    attn_norm = din("attn_norm", [1, D])
    ffn_norm = din("ffn_norm", [1, D])
    final_norm = din("final_norm", [1, D])
    w_in = din("w_in", [D, 2304])
    w_out = din("w_out", [D, D])
    w_r = din("w_r", [D, 36])
    b_r = din("b_r", [1, 36])
    convwT = din("convwT", [128, 12])
    sinks = din("sinks", [1, 8])
    gn_attn = din("gn_attn", [1, 512])
    gn_convT = din("gn_convT", [128, 4])
    wall = din("wall", [NE * 128, WROW])
    kcT_d = din("kcT", [128, 2, NSEQ_S * 128])
    vc_d = din("vc", [128, NSEQ_S, 128])
    ck_nat = din("ck_nat", [NSEQ_S, 128, 128])
    cv_nat = din("cv_nat", [NSEQ_S, 128, 128])
    stT = din("stT", [128, 4, NSEQ_S, 2])
    ident_d = din("ident", [128, 128], BF16)
    triu_d = din("triu", [128, 128])
    ones_d = din("ones", [128, 128])
    g64_d = din("g64", [128, 128])
    maskp_d = din("maskp", [128, 256], BF16)
    mask0_d = din("mask0", [128, 256], BF16)
    masks_d = din("masks", [128, 2112], BF16)
    cosp_d = din("cosp", [128, NBLK, 32])
    sinp_d = din("sinp", [128, NBLK, 32])
    coss_d = din("coss", [64, 32])
    sins_d = din("sins", [64, 32])
    flag_d = din("flag", [128, 1])
    tokid_d = din("tokid", [128, NTT], I32)
    piota_d = din("piota", [128, 1])
    thr_d = din("thr", [1, 128])
    twinit_d = din("twinit", [128, 4], I32)
    y_p = dout("y_p", [NPB * 128, D])
    y_s = dout("y_s", [64, D])
    nk_p = dout("nk_p", [128, 128])
    nv_p = dout("nv_p", [128, 128])
    ncv_p = dout("ncv_p", [2, 512])
    nk_s = dout("nk_s", [NSEQ_S, 128, 128])
    nv_s = dout("nv_s", [NSEQ_S, 128, 128])
    ncv_s = dout("ncv_s", [NSEQ_S, 2, 512])
    H2 = dscr("H2", [HROWS, D], BF16)
    X1 = dscr("X1", [HROWS, D], F32)
    Y = dscr("Y", [2 * HROWS, D], F32)
    TW = dscr("TW", [NB * 128, 4], I32)
    MODV = dscr("MODV", [6, 65, D], F32)

    dr = {k: Buf(k) for k in ("H2", "X1", "Y", "TW", "MODV", "out")}

    with ExitStack() as st0:
        S = Sched(nc, st0, 64)
        pe, act, dve, pool, sp = (S.E[k] for k in ("pe", "act", "dve", "pool", "sp"))

        def mk(stack, name, shape, dt):
            return T(stack.enter_context(nc.sbuf_tensor("sb_" + name, list(shape), dt)), name)

        def breg(name, val):
            r = nc.gpsimd.alloc_register(name)
            nc.gpsimd.reg_mov(r, val)
            return r

        bc_tw = breg("bc_tw", NB * 128 - 1)
        bc_w = breg("bc_w", NE * 128 - 1)
        bc_y = breg("bc_y", 2 * HROWS - 1)
        ps = st0.enter_context(nc.psum_tensor("ps", [128, 4096], F32))
        PB = [Buf("pb%d" % i, excl=True) for i in range(8)]

        def bank(i, n=1):
            return ps[:, i * 512:(i + n) * 512]

        def bank_bf(i, n=1):
            return ps[:, i * 512:(i + n) * 512].bitcast(BF16)

        ident = mk(st0, "ident", [128, 128], BF16)
        onesf = mk(st0, "onesf", [128, 128], F32)
        triu = mk(st0, "triu", [128, 128], F32)
        dC = S.dsem()
        sp.dma(dC, ident.t[:], ident_d, writes=[ident.b], group=True)
        sp.dma(dC, onesf.t[:], ones_d, writes=[onesf.b], group=True)
        sp.dma(dC, triu.t[:], triu_d, writes=[triu.b], group=True)
        dOut = S.dsem()
        dNK, dNV = S.dsem(), S.dsem()
        qk_f0 = mk(st0, "qk_f", [128, 640], F32)
        v_f0 = mk(st0, "v_f", [128, 128], F32)
        qk_fL = [qk_f0, qk_f0]
        v_fL = [v_f0, v_f0]
        uext = [mk(st0, "uext%d" % i, [128, 4, 130], F32) for i in range(2)]
        uexts = mk(st0, "uexts", [128, 4, NSEQ_S * 6], F32)
        for t_ in (ident, onesf, triu):
            t_.b.w = (dC, S.dcount[dC])
        twi = mk(st0, "twi", [128, 4], I32)
        dI = S.dsem()
        sp.dma(dI, twi.t[:], twinit_d, writes=[twi.b])
        for b_ in range(NB):
            sp.dma(dI, TW[b_ * 128:(b_ + 1) * 128, :], twi.t[:], reads=[twi.b], writes=[dr["TW"]], group=True)
        dr["TW"].w = (dI, S.dcount[dI])
        gate_all = mk(st0, "gate_all", [128, NTT, NE], F32)
        rs_all = mk(st0, "rs_all", [128, NTT, NE], F32)
        sel_all = mk(st0, "sel_all", [128, NTT, NE], F32)
        cumsel = mk(st0, "cumsel", [128, NE], F32)
        pool.op(lambda e: e.memset(gate_all.t[:], 0.0), writes=[gate_all.b])
        pool.op(lambda e: e.memset(rs_all.t[:], 0.0), writes=[rs_all.b])
        pool.op(lambda e: e.memset(sel_all.t[:], 0.0), writes=[sel_all.b])
        pool.op(lambda e: e.memset(cumsel.t[:], 0.0), writes=[cumsel.b])
        sW = ExitStack()
        w_in_bf = mk(sW, "w_in_bf", [128, 8, 2304], BF16)
        w_out_bf = mk(sW, "w_out_bf", [128, 8, D], BF16)
        w_r_bf = mk(sW, "w_r_bf", [128, 8, 36], BF16)
        kcT = mk(sW, "kcT", [128, 2, NSEQ_S * 128], BF16)
        vc = mk(sW, "vc", [128, NSEQ_S, 128], BF16)
        w_in_parts = [Buf("w_in_c%d" % c) for c in range(8)]
        for c in range(8):
            pool.dma(S.dsem(), w_in_bf.t[:, c, :], w_in[c * 128:(c + 1) * 128, :], writes=[w_in_parts[c]])
        pool.dma(S.dsem(), w_out_bf.t[:], w_out.rearrange("(c p) n -> p c n", p=128), writes=[w_out_bf.b])
        pool.dma(S.dsem(), w_r_bf.t[:], w_r.rearrange("(c p) n -> p c n", p=128), writes=[w_r_bf.b])
        pool.dma(S.dsem(), kcT.t[:], kcT_d, writes=[kcT.b])
        pool.dma(S.dsem(), vc.t[:], vc_d, writes=[vc.b])

        with ExitStack() as s0:
            cTt = mk(s0, "cTt", [128, 8, 65], F32)
            silu = mk(s0, "silu", [128, 8, 65], F32)
            wmb = [mk(s0, "wmb%d" % i, [128, 8, 512], F32) for i in range(2)]
            mod17 = mk(s0, "mod17", [65, 6 * D], F32)
            bm17 = mk(s0, "bm17", [65, 6 * D], F32)
            an17 = mk(s0, "an17", [65, D], F32)
            fn17 = mk(s0, "fn17", [65, D], F32)
            a17 = mk(s0, "a17", [65, 2 * D], F32)
            d0 = S.dsem()
            sp.dma(d0, cTt.t[:], cT.rearrange("(c p) r -> p c r", p=128), writes=[cTt.b], group=True)
            sp.dma(d0, bm17.t[:], b_mod.partition_broadcast(65), writes=[bm17.b], group=True)
            sp.dma(d0, an17.t[:], attn_norm.partition_broadcast(65), writes=[an17.b], group=True)
            sp.dma(d0, fn17.t[:], ffn_norm.partition_broadcast(65), writes=[fn17.b], group=True)
            for t_ in (cTt, bm17, an17, fn17):
                t_.b.w = (d0, S.dcount[d0])
            act.op(lambda e: e.activation(out=silu.t[:], in_=cTt.t[:], func=AF.Silu), reads=[cTt.b], writes=[silu.b])
            dW = [S.dsem(), S.dsem()]
            for j in range(12):
                wb = wmb[j % 2]
                sp.dma(dW[j % 2], wb.t[:], w_mod[:, j * 512:(j + 1) * 512].rearrange("(c p) n -> p c n", p=128),
                       writes=[wb.b])
                pb = PB[j % 2]
                for c in range(8):
                    pe.op(lambda e, c=c, wb=wb, j=j: e.matmul(bank(j % 2)[0:65, :], lhsT=silu.t[:, c, :], rhs=wb.t[:, c, :],
                                                              start=(c == 0), stop=(c == 7)),
                          reads=[silu.b, wb.b], writes=[pb])
                dve.op(lambda e, j=j: e.tensor_tensor(out=mod17.t[:, j * 512:(j + 1) * 512], in0=bank(j % 2)[0:65, :],
                                                      in1=bm17.t[:, j * 512:(j + 1) * 512], op=ALU.add),
                       reads=[pb, bm17.b], writes=[mod17.b])
            dve.op(lambda e: e.scalar_tensor_tensor(out=a17.t[:, 0:D], in0=mod17.t[:, D:2 * D], scalar=1.0, in1=an17.t[:],
                                                    op0=ALU.add, op1=ALU.mult), reads=[mod17.b, an17.b], writes=[a17.b])
            dve.op(lambda e: e.scalar_tensor_tensor(out=a17.t[:, D:2 * D], in0=mod17.t[:, 4 * D:5 * D], scalar=1.0, in1=fn17.t[:],
                                                    op0=ALU.add, op1=ALU.mult), reads=[mod17.b, fn17.b], writes=[a17.b])
            dM = S.dsem()
            srcs = [a17.t[:, 0:D], mod17.t[:, 0:D], mod17.t[:, 2 * D:3 * D], a17.t[:, D:2 * D],
                    mod17.t[:, 3 * D:4 * D], mod17.t[:, 5 * D:6 * D]]
            for k in range(6):
                sp.dma(dM, MODV[k], srcs[k], reads=[a17.b, mod17.b], writes=[dr["MODV"]], group=True)
            dr["MODV"].w = (dM, S.dcount[dM])
            for e_ in S.E.values():
                e_.wait_ev(dr["MODV"].w)
            barrier_all(S)

        with ExitStack() as s1:
            BC = [mk(s1, "bc%d" % k, [128, D], F32) for k in range(6)]
            g64 = mk(s1, "g64", [128, 128], F32)
            maskp = mk(s1, "maskp", [128, 256], BF16)
            mask0 = mk(s1, "mask0", [128, 256], BF16)
            masks = mk(s1, "masks", [128, 2112], BF16)
            cosp = mk(s1, "cosp", [128, NBLK, 32], F32)
            sinp = mk(s1, "sinp", [128, NBLK, 32], F32)
            coss = mk(s1, "coss", [64, 32], F32)
            sins = mk(s1, "sins", [64, 32], F32)
            flag = mk(s1, "flag", [128, 1], F32)
            convw = mk(s1, "convw", [128, 12], F32)
            gncv = mk(s1, "gncv", [128, 4], F32)
            gnat = mk(s1, "gnat", [128, 512], F32)
            nsink = mk(s1, "nsink", [128, 8], F32)
            psink = mk(s1, "psink", [128, 8], F32)
            brt = mk(s1, "brt", [128, 36], F32)
            dK = S.dsem()
            cl = [(g64, g64_d), (maskp, maskp_d), (mask0, mask0_d), (masks, masks_d), (cosp, cosp_d), (sinp, sinp_d),
                  (coss, coss_d), (sins, sins_d), (flag, flag_d), (convw, convwT), (gncv, gn_convT),
                  (gnat, gn_attn.partition_broadcast(128)), (psink, sinks.partition_broadcast(128)),
                  (brt, b_r.partition_broadcast(128))]
            for t_, src in cl:
                sp.dma(dK, t_.t[:], src, writes=[t_.b], group=True)
            for k in range(6):
                sp.dma(dK, BC[k].t[:], MODV[k, 0:1, :].partition_broadcast(128), reads=[dr["MODV"]], writes=[BC[k].b], group=True)
            for t_, _ in cl:
                t_.b.w = (dK, S.dcount[dK])
            for k in range(6):
                BC[k].b.w = (dK, S.dcount[dK])
            dve.op(lambda e: e.tensor_scalar(out=nsink.t[:], in0=psink.t[:], scalar1=-1.0, scalar2=None, op0=ALU.mult),
                   reads=[psink.b], writes=[nsink.b])

            def mk2(name, shape, dt):
                return [mk(s1, "%s_%d" % (name, i), shape, dt) for i in range(2)]

            def mk1(name, shape, dt):
                t_ = mk(s1, name, shape, dt)
                return [t_, t_]

            def mk3(name, shape, dt):
                return [mk(s1, "%s_%d" % (name, i), shape, dt) for i in range(3)]

            Xt = mk3("xt", [128, D], F32)
            tmpL = mk3("tmp", [128, D], F32)
            hTL = mk3("hT", [128, 8 * 128], BF16)
            h2TL = hTL
            stt = mk3("stt", [128, 64], F32)
            caccL = mk3("cacc", [128, 4, 128], F32)
            csqL = mk3("csq", [128, 4, 128], F32)
            crsL = csqL
            kT = [mk(s1, "kT_%d" % i, [128, 2, 128], BF16) for i in range(3)]
            vb = [mk(s1, "vb_%d" % i, [128, 128], BF16) for i in range(3)]
            junkL = mk1("junk", [128, D], BF16)
            junk2 = mk(s1, "junk2", [128, D], BF16)
            h_bfL = mk1("h_bf", [128, D], BF16)
            qkvL = mk1("qkv_sb", [128, 768], F32)
            cvL = mk1("cv_sb", [128, 12, 128], F32)
            qkdL = mk1("qkd", [128, 768], BF16)
            qTL = mk2("qT", [128, 4, 128], BF16)
            r1L = mk1("r1", [128, 10, 32], F32)
            r2L = mk1("r2", [128, 10, 32], F32)
            P_bfL = mk1("P_bf", [128, 2176], BF16)
            PT_sbL = mk1("PT_sb", [128, 1088], BF16)
            O_sbL = mk1("O_sb", [128, 512], F32)
            On_bfL = mk1("On_bf", [128, 512], BF16)
            mixTL = mk1("mixT", [128, 8, 128], BF16)
            h2_bfL = mk1("h2_bf", [128, D], BF16)
            lgL = mk1("lg", [128, 36], F32)
            rtL = mk1("rt", [128, 160], F32)
            h2_bf = h2_bfL[0]

            dX = [S.dsem(), S.dsem(), S.dsem()]
            dOX = [S.dsem(), S.dsem()]
            dOH = [S.dsem(), S.dsem()]
            dS = S.dsem()
            for c in range(4):
                sp.dma(dS, uexts.t[:, c, :].rearrange("p (s l) -> p s l", s=NSEQ_S)[:, :, 0:2], stT[:, c, :, :], writes=[uexts.b],
                       allow_slow_non_contiguous=True, group=True)
            pool.op(lambda e: e.memset(h2_bf.t[:], 0.0), writes=[h2_bf.b])
            sp.dma(dS, H2[NTOK:NTOK + 64, :], h2_bf.t[0:64, :], reads=[h2_bf.b], writes=[dr["H2"]], group=True)
            sp.wait_ev((dS, S.dcount[dS]))
            uexts.b.w = (dS, S.dcount[dS])
            h2_bf.b.r = [(dS, S.dcount[dS])]

            def block(bi, kind):
                NQ = 64 if kind == "sample" else 128
                par = bi % 2
                r3, r3p = bi % 3, (bi - 1) % 3
                xt = Xt[r3]
                sq = stt[r3]
                tmp, junk, h_bf, hT, h2T = tmpL[r3], junkL[par], h_bfL[par], hTL[r3], h2TL[r3]
                qkv_sb, cv_sb, qkd, qT, r1, r2 = qkvL[par], cvL[par], qkdL[par], qTL[par], r1L[par], r2L[par]
                P_bf, PT_sb, O_sb, On_bf, mixT = P_bfL[par], PT_sbL[par], O_sbL[par], On_bfL[par], mixTL[par]
                cacc, csq, crs, x1, h2_bf, lg, rt = caccL[r3], csqL[r3], crsL[r3], Xt[r3], h2_bfL[par], lgL[par], rtL[par]
                qk_f, v_f = qk_fL[par], v_fL[par]
                if kind == "sample":
                    src = xs
                    cos_ap, sin_ap = coss.t[0:64, :], sins.t[0:64, :]
                    bk = dict(T=0, S=3, PT=1, O=0, T2=0, GS=3, MIX=4, H2T=0, LG=6)
                    G, NK, nchunk = 1, 2112, 17
                    for k in (0, 1):
                        sp.dma(dK, BC[k].t[0:64, :], MODV[k, 1:65, :], reads=[dr["MODV"]], writes=[BC[k].b], group=True)
                    def _fix01():
                        for k in (0, 1):
                            BC[k].b.w = (dK, S.dcount[dK])
                    S.defer(_fix01, writes=[BC[0].b, BC[1].b])
                else:
                    src = xp[bi * 128:(bi + 1) * 128, :]
                    cos_ap, sin_ap = cosp.t[:, bi, :], sinp.t[:, bi, :]
                    bk = dict(T=0, S=3, PT=4, O=5, T2=6, GS=7, MIX=6, H2T=6, LG=7)
                    G, NK, nchunk = 2, 256, 2
                rows = slice(0, NQ)
                sp.dma(dX[r3], xt.t[rows, :], src, writes=[xt.b])
                dve.op(lambda e: e.memset(sq.t[:], 0.0), writes=[sq.b])
                act.op(lambda e: e.activation(out=junk.t[rows, :], in_=xt.t[rows, :], func=AF.Square, accum_out=sq.t[rows, 0:1]),
                       reads=[xt.b], writes=[junk.b, sq.b])
                act.op(lambda e: e.activation(out=sq.t[rows, 1:2], in_=sq.t[rows, 0:1], func=AF.Ln, scale=1.0 / D, bias=EPS),
                       reads=[sq.b], writes=[sq.b])
                act.op(lambda e: e.activation(out=sq.t[rows, 1:2], in_=sq.t[rows, 1:2], func=AF.Exp, scale=-0.5), reads=[sq.b], writes=[sq.b])
                dve.op(lambda e: e.scalar_tensor_tensor(out=tmp.t[rows, :], in0=xt.t[rows, :], scalar=sq.t[rows, 1:2], in1=BC[0].t[rows, :],
                                                        op0=ALU.mult, op1=ALU.mult), reads=[xt.b, sq.b, BC[0].b], writes=[tmp.b])
                pool.op(lambda e: e.tensor_tensor(out=h_bf.t[rows, :], in0=tmp.t[rows, :], in1=BC[1].t[rows, :], op=ALU.add),
                        reads=[tmp.b, BC[1].b], writes=[h_bf.b])
                yield
                tb = bank_bf(bk["T"])
                for c in range(8):
                    pe.op(lambda e, c=c: e.transpose(out=tb[:, c * NQ:(c + 1) * NQ], in_=h_bf.t[rows, c * 128:(c + 1) * 128],
                                                     identity=ident.t[rows, rows]), reads=[h_bf.b, ident.b], writes=[PB[bk["T"]]])
                act.op(lambda e: e.copy(out=hT.t[:, 0:8 * NQ], in_=tb[:, 0:8 * NQ]), reads=[PB[bk["T"]]], writes=[hT.b])
                for lo, hi, iob in ((0, 512, 1), (512, 768, 2)):
                    for c in range(8):
                        pe.op(lambda e, c=c, lo=lo, hi=hi, iob=iob: e.matmul(ps[rows, iob * 512: iob * 512 + (hi - lo)], lhsT=hT.t[:, c * NQ:(c + 1) * NQ],
                                                                             rhs=w_in_bf.t[:, c, lo:hi], start=(c == 0), stop=(c == 7)),
                              reads=[hT.b, w_in_parts[c]], writes=[PB[iob]])
                    if iob == 1:
                        act.op(lambda e, lo=lo, hi=hi, iob=iob: e.copy(out=qkv_sb.t[rows, lo:hi], in_=ps[rows, iob * 512: iob * 512 + (hi - lo)]),
                               reads=[PB[iob]], writes=[qkv_sb.b])
                    else:
                        dve.op(lambda e, lo=lo, hi=hi, iob=iob: e.tensor_copy(out=qkv_sb.t[rows, lo:hi], in_=ps[rows, iob * 512: iob * 512 + (hi - lo)]),
                               reads=[PB[iob]], writes=[qkv_sb.b])
                for i_ in range(3):
                    iob = 1 + (i_ % 2)
                    for j in range(4 * i_, 4 * i_ + 4):
                        for c in range(8):
                            pe.op(lambda e, c=c, j=j, iob=iob: e.matmul(ps[:, iob * 512 + (j % 4) * 128: iob * 512 + (j % 4) * 128 + NQ],
                                                                        lhsT=w_in_bf.t[:, c, 768 + j * 128: 768 + (j + 1) * 128],
                                                                        rhs=hT.t[:, c * NQ:(c + 1) * NQ], start=(c == 0), stop=(c == 7)),
                                  reads=[hT.b, w_in_parts[c]], writes=[PB[iob]])
                    if i_ != 1:
                        act.op(lambda e, i_=i_, iob=iob: e.copy(out=cv_sb.t[:, 4 * i_:4 * i_ + 4, 0:NQ],
                                                                in_=bank(iob).rearrange("p (c n) -> p c n", c=4)[:, :, 0:NQ]),
                               reads=[PB[iob]], writes=[cv_sb.b])
                    else:
                        dve.op(lambda e, i_=i_, iob=iob: e.tensor_copy(out=cv_sb.t[:, 4 * i_:4 * i_ + 4, 0:NQ],
                                                                       in_=bank(iob).rearrange("p (c n) -> p c n", c=4)[:, :, 0:NQ]),
                               reads=[PB[iob]], writes=[cv_sb.b])
                yield
                qkvs = qkv_sb.t[rows, :]
                xv = qkvs[:, 0:640].rearrange("p (h t d) -> p h t d", h=10, t=2)
                ov = qk_f.t[rows, :].rearrange("p (h t d) -> p h t d", h=10, t=2)
                cb_ = cos_ap.unsqueeze(1).to_broadcast([NQ, 10, 32])
                sb_ = sin_ap.unsqueeze(1).to_broadcast([NQ, 10, 32])
                rq = [qkv_sb.b]
                dve.op(lambda e: e.tensor_tensor(out=r1.t[rows], in0=xv[:, :, 0, :], in1=cb_, op=ALU.mult), reads=rq + [cosp.b, coss.b], writes=[r1.b])
                dve.op(lambda e: e.tensor_tensor(out=r2.t[rows], in0=xv[:, :, 1, :], in1=sb_, op=ALU.mult), reads=rq + [sinp.b, sins.b], writes=[r2.b])
                pool.op(lambda e: e.tensor_tensor(out=ov[:, :, 0, :], in0=r1.t[rows], in1=r2.t[rows], op=ALU.subtract),
                        reads=[r1.b, r2.b], writes=[qk_f.b])
                dve.op(lambda e: e.tensor_tensor(out=r1.t[rows], in0=xv[:, :, 1, :], in1=cb_, op=ALU.mult), reads=rq, writes=[r1.b])
                dve.op(lambda e: e.tensor_tensor(out=r2.t[rows], in0=xv[:, :, 0, :], in1=sb_, op=ALU.mult), reads=rq, writes=[r2.b])
                pool.op(lambda e: e.tensor_tensor(out=ov[:, :, 1, :], in0=r1.t[rows], in1=r2.t[rows], op=ALU.add),
                        reads=[r1.b, r2.b], writes=[qk_f.b])
                act.op(lambda e: e.copy(out=qkd.t[rows, 0:512], in_=qk_f.t[rows, 0:512]), reads=[qk_f.b], writes=[qkd.b])
                kdup = qkd.t[rows, 512:768].rearrange("p (g t d) -> p g t d", g=2, t=2)
                ksrc = qk_f.t[rows, 512:640].rearrange("p (g d) -> p g d", g=2)
                pool.op(lambda e: e.tensor_copy(out=kdup[:, :, 0, :], in_=ksrc), reads=[qk_f.b], writes=[qkd.b])
                pool.op(lambda e: e.tensor_copy(out=kdup[:, :, 1, :], in_=ksrc), reads=[qk_f.b], writes=[qkd.b])
                pool.op(lambda e: e.tensor_copy(out=vb[r3].t[rows, :], in_=qkvs[:, 640:768]), reads=[qkv_sb.b], writes=[vb[r3].b])
                last_p = (kind == "prompt" and bi == NPB)
                if last_p or kind == "sample":
                    pool.op(lambda e: e.tensor_copy(out=v_f.t[rows, :], in_=qkvs[:, 640:768]), reads=[qkv_sb.b], writes=[v_f.b])
                NS, L = (NSEQ_S, 4) if kind == "sample" else (1, 128)
                ue = uexts if kind == "sample" else uext[par]
                W_ = NS * (L + 2)

                def cps(j):
                    return cv_sb.t[:, j, 0:NQ]

                for c in range(4):
                    uv = ue.t[:, c, 0:W_].rearrange("p (s l) -> p s l", s=NS)
                    pool.op(lambda e, c=c, uv=uv: e.tensor_tensor(out=uv[:, :, 2:2 + L], in0=cps(8 + c).rearrange("p (s l) -> p s l", s=NS),
                                                                  in1=cps(c).rearrange("p (s l) -> p s l", s=NS), op=ALU.mult),
                            reads=[cv_sb.b], writes=[ue.b])
                if kind != "sample":
                    prev = uext[1 - par]
                    if bi == 1:
                        pool.op(lambda e: e.tensor_scalar(out=ue.t[:, :, 0:2], in0=prev.t[:, :, 128:130], scalar1=flag.t[:, 0:1], scalar2=None,
                                                          op0=ALU.mult), reads=[prev.b, flag.b], writes=[ue.b])
                    elif bi > 1:
                        pool.op(lambda e: e.tensor_copy(out=ue.t[:, :, 0:2], in_=prev.t[:, :, 128:130]), reads=[prev.b], writes=[ue.b])
                if kind == "halo":
                    tbk = bank_bf(bk["T"])
                    for j in range(2):
                        pe.op(lambda e, j=j: e.transpose(out=tbk[:, j * 128:(j + 1) * 128], in_=qkd.t[:, 512 + j * 128: 640 + j * 128],
                                                         identity=ident.t[:, :]), reads=[qkd.b, ident.b], writes=[PB[bk["T"]]])
                    act.op(lambda e: e.copy(out=kT[r3].t[:].rearrange("p g n -> p (g n)"), in_=tbk[:, 0:256]),
                           reads=[PB[bk["T"]]], writes=[kT[r3].b])
                    return
                for c in range(4):
                    uv = ue.t[:, c, 0:W_].rearrange("p (s l) -> p s l", s=NS)
                    av = cacc.t[:, c, 0:NQ].rearrange("p (s l) -> p s l", s=NS)
                    dve.op(lambda e, c=c, uv=uv, av=av: e.tensor_scalar(out=av, in0=uv[:, :, 0:L], scalar1=convw.t[:, c * 3:c * 3 + 1], scalar2=None,
                                                                      op0=ALU.mult), reads=[ue.b, convw.b], writes=[cacc.b])
                    for k in (1, 2):
                        dve.op(lambda e, c=c, uv=uv, av=av, k=k: e.scalar_tensor_tensor(out=av, in0=uv[:, :, k:k + L],
                                                                                       scalar=convw.t[:, c * 3 + k:c * 3 + k + 1], in1=av,
                                                                                       op0=ALU.mult, op1=ALU.add),
                               reads=[ue.b, convw.b, cacc.b], writes=[cacc.b])
                    pool.op(lambda e, c=c: e.tensor_tensor(out=cacc.t[:, c, 0:NQ], in0=cps(4 + c), in1=cacc.t[:, c, 0:NQ], op=ALU.mult),
                            reads=[cv_sb.b, cacc.b], writes=[cacc.b])
                act.op(lambda e: e.activation(out=csq.t[:, :, 0:NQ], in_=cacc.t[:, :, 0:NQ], func=AF.Square), reads=[cacc.b], writes=[csq.b])
                tbk = bank_bf(bk["T"])
                for j in range(6):
                    pe.op(lambda e, j=j: e.transpose(out=tbk[:, j * NQ:(j + 1) * NQ], in_=qkd.t[rows, j * 128:(j + 1) * 128],
                                                     identity=ident.t[rows, rows]), reads=[qkd.b, ident.b], writes=[PB[bk["T"]]])
                act.op(lambda e: e.copy(out=qT.t[:, :, 0:NQ], in_=tbk[:, 0:4 * NQ].rearrange("p (j n) -> p j n", j=4)),
                       reads=[PB[bk["T"]]], writes=[qT.b])
                dve.op(lambda e: e.tensor_copy(out=kT[r3].t[:, :, 0:NQ], in_=tbk[:, 4 * NQ:6 * NQ].rearrange("p (j n) -> p j n", j=2)),
                       reads=[PB[bk["T"]]], writes=[kT[r3].b])
                yield
                if kind == "sample":
                    def kchunk(kc, g, half):
                        if kc < 16:
                            return kcT.t[half * 64:(half + 1) * 64, g, kc * 128:(kc + 1) * 128], 128, kcT.b
                        return kT[r3].t[half * 64:(half + 1) * 64, g, 0:64], 64, kT[r3].b

                    def vchunk(kc, g):
                        if kc < 16:
                            return vc.t[:, kc, g * 64:(g + 1) * 64], 128, vc.b
                        return vb[r3].t[0:64, g * 64:(g + 1) * 64], 64, vb[r3].b
                    mask_t = masks
                else:
                    def kchunk(kc, g, half):
                        t_ = kT[r3p] if kc == 0 else kT[r3]
                        return t_.t[half * 64:(half + 1) * 64, g, :], 128, t_.b

                    def vchunk(kc, g):
                        t_ = vb[r3p] if kc == 0 else vb[r3]
                        return t_.t[:, g * 64:(g + 1) * 64], 128, t_.b
                    mask_t = mask0 if bi == 1 else maskp
                sbk = bk["S"]
                nsb = (G * NK + 511) // 512
                SB = [PB[sbk + i] for i in range(nsb)]
                ob = bk["O"]
                ptb = bk["PT"]
                npt = 2 if kind == "sample" else 1
                PTB = [PB[ptb + i_] for i_ in range(npt)]
                ptv = bank_bf(ptb, npt)
                for hg in range(8 // G):
                    for gi in range(G):
                        h = hg * G + gi
                        half, g = h % 2, h // 4
                        base = sbk * 512 + gi * NK
                        for kc in range(nchunk):
                            kap, kn, kbuf = kchunk(kc, g, half)
                            pe.op(lambda e, kc=kc, kap=kap, kn=kn, base=base, half=half, h=h: e.matmul(
                                ps[rows, base + kc * 128: base + kc * 128 + kn], lhsT=qT.t[half * 64:(half + 1) * 64, h // 2, 0:NQ], rhs=kap,
                                start=True, stop=False), reads=[qT.b, kbuf], writes=SB)
                            mrow = slice(half * 64, half * 64 + 64) if NQ == 64 else rows
                            pe.op(lambda e, kc=kc, kn=kn, base=base, mrow=mrow: e.matmul(
                                ps[rows, base + kc * 128: base + kc * 128 + kn], lhsT=ident.t[mrow, mrow],
                                rhs=mask_t.t[mrow, kc * 128: kc * 128 + kn], start=False, stop=True),
                                reads=[ident.b, mask_t.b], writes=SB)
                    sv = ps[rows, sbk * 512: sbk * 512 + G * NK].rearrange("p (g n) -> p g n", g=G)
                    h0 = hg * G
                    dve.op(lambda e, h0=h0: e.tensor_reduce(out=sq.t[rows, 8 + h0: 8 + h0 + G], in_=sv, axis=AX.X, op=ALU.max),
                           reads=SB, writes=[sq.b])
                    dve.op(lambda e, h0=h0: e.scalar_tensor_tensor(out=sq.t[rows, 16 + h0:16 + h0 + G], in0=sq.t[rows, 8 + h0: 8 + h0 + G], scalar=-0.125,
                                                                   in1=nsink.t[rows, h0:h0 + G], op0=ALU.mult, op1=ALU.min),
                           reads=[sq.b, nsink.b], writes=[sq.b])
                    for gi in range(G):
                        h = h0 + gi
                        act.op(lambda e, gi=gi, h=h: e.activation(out=P_bf.t[rows, gi * NK:(gi + 1) * NK], in_=sv[:, gi, :], func=AF.Exp,
                                                                  bias=sq.t[rows, 16 + h:17 + h], scale=0.125, accum_out=sq.t[rows, 24 + h:25 + h]),
                               reads=SB + [sq.b], writes=[P_bf.b, sq.b])
                    slot = 0
                    for gi in range(G):
                        for kc in range(nchunk):
                            kn = 64 if (kind == "sample" and kc == 16) else 128
                            pe.op(lambda e, gi=gi, kc=kc, kn=kn, slot=slot: e.transpose(
                                out=ptv[0:kn, slot * NQ:(slot + 1) * NQ], in_=P_bf.t[rows, gi * NK + kc * 128: gi * NK + kc * 128 + kn],
                                identity=ident.t[rows, rows]), reads=[P_bf.b, ident.b], writes=PTB)
                            slot += 1
                    nsl = slot
                    hs = (nsl // 2) * NQ
                    if kind == "sample":
                        if hg % 2 == 0:
                            act.op(lambda e: e.copy(out=PT_sb.t[:, 0:16 * NQ], in_=ptv[:, 0:16 * NQ]), reads=PTB, writes=[PT_sb.b])
                            act.op(lambda e: e.copy(out=PT_sb.t[0:64, 16 * NQ:17 * NQ], in_=ptv[0:64, 16 * NQ:17 * NQ]), reads=PTB, writes=[PT_sb.b])
                        else:
                            dve.op(lambda e: e.tensor_copy(out=PT_sb.t[:, 0:16 * NQ], in_=ptv[:, 0:16 * NQ]), reads=PTB, writes=[PT_sb.b])
                            dve.op(lambda e: e.tensor_copy(out=PT_sb.t[0:64, 16 * NQ:17 * NQ], in_=ptv[0:64, 16 * NQ:17 * NQ]), reads=PTB, writes=[PT_sb.b])
                    elif hg % 2 == 0:
                        act.op(lambda e: e.copy(out=PT_sb.t[:, 0:nsl * NQ], in_=ptv[:, 0:nsl * NQ]), reads=PTB, writes=[PT_sb.b])
                    else:
                        dve.op(lambda e: e.tensor_copy(out=PT_sb.t[:, 0:nsl * NQ], in_=ptv[:, 0:nsl * NQ]), reads=PTB, writes=[PT_sb.b])
                    slot = 0
                    for gi in range(G):
                        h = h0 + gi
                        g = h // 4
                        for kc in range(nchunk):
                            vap, kn, vbuf = vchunk(kc, g)
                            pe.op(lambda e, h=h, kc=kc, vap=vap, kn=kn, slot=slot: e.matmul(
                                ps[rows, ob * 512 + h * 64: ob * 512 + (h + 1) * 64], lhsT=PT_sb.t[0:kn, slot * NQ:(slot + 1) * NQ], rhs=vap,
                                start=(kc == 0), stop=(kc == nchunk - 1)), reads=[PT_sb.b, vbuf], writes=[PB[ob]])
                            slot += 1
                    yield
                dve.op(lambda e: e.tensor_tensor(out=sq.t[rows, 32:40], in0=sq.t[rows, 16:24], in1=psink.t[rows, :], op=ALU.add),
                       reads=[sq.b, psink.b], writes=[sq.b])
                act.op(lambda e: e.activation(out=sq.t[rows, 32:40], in_=sq.t[rows, 32:40], func=AF.Exp), reads=[sq.b], writes=[sq.b])
                dve.op(lambda e: e.tensor_tensor(out=sq.t[rows, 32:40], in0=sq.t[rows, 32:40], in1=sq.t[rows, 24:32], op=ALU.add),
                       reads=[sq.b], writes=[sq.b])
                dve.op(lambda e: e.reciprocal(out=sq.t[rows, 32:40], in_=sq.t[rows, 32:40]), reads=[sq.b], writes=[sq.b])
                ov3 = O_sb.t[rows, :].rearrange("p (h d) -> p h d", h=8)
                dve.op(lambda e: e.tensor_tensor(out=ov3, in0=ps[rows, ob * 512:(ob + 1) * 512].rearrange("p (h d) -> p h d", h=8),
                                                 in1=sq.t[rows, 32:40].unsqueeze(2).to_broadcast([NQ, 8, 64]), op=ALU.mult),
                       reads=[PB[ob], sq.b], writes=[O_sb.b])
                pool.op(lambda e: e.tensor_tensor(out=tmp.t[rows, 0:512], in0=O_sb.t[rows, :], in1=O_sb.t[rows, :], op=ALU.mult),
                        reads=[O_sb.b], writes=[tmp.b])
                dve.op(lambda e: e.tensor_reduce(out=sq.t[rows, 40:48], in_=tmp.t[rows, 0:512].rearrange("p (h d) -> p h d", h=8), axis=AX.X, op=ALU.add),
                       reads=[tmp.b], writes=[sq.b])
                act.op(lambda e: e.activation(out=sq.t[rows, 40:48], in_=sq.t[rows, 40:48], func=AF.Ln, scale=1.0 / 64, bias=EPS),
                       reads=[sq.b], writes=[sq.b])
                act.op(lambda e: e.activation(out=sq.t[rows, 40:48], in_=sq.t[rows, 40:48], func=AF.Exp, scale=-0.5), reads=[sq.b], writes=[sq.b])
                dve.op(lambda e: e.tensor_tensor(out=ov3, in0=ov3, in1=sq.t[rows, 40:48].unsqueeze(2).to_broadcast([NQ, 8, 64]), op=ALU.mult),
                       reads=[O_sb.b, sq.b], writes=[O_sb.b])
                pool.op(lambda e: e.tensor_tensor(out=On_bf.t[rows, :], in0=O_sb.t[rows, :], in1=gnat.t[rows, :], op=ALU.mult),
                        reads=[O_sb.b, gnat.b], writes=[On_bf.b])
                tbo = bank_bf(bk["T2"])
                for j in range(4):
                    pe.op(lambda e, j=j: e.transpose(out=tbo[:, j * NQ:(j + 1) * NQ], in_=On_bf.t[rows, j * 128:(j + 1) * 128],
                                                     identity=ident.t[rows, rows]), reads=[On_bf.b, ident.b], writes=[PB[bk["T2"]]])
                act.op(lambda e: e.copy(out=mixT.t[:, 0:4, 0:NQ], in_=tbo[:, 0:4 * NQ].rearrange("p (j n) -> p j n", j=4)),
                       reads=[PB[bk["T2"]]], writes=[mixT.b])
                gb = bk["GS"]
                for c in range(4):
                    pe.op(lambda e, c=c: e.matmul(ps[:, gb * 512 + c * 128: gb * 512 + c * 128 + NQ], lhsT=g64.t[:, :], rhs=csq.t[:, c, 0:NQ],
                                                  start=True, stop=True), reads=[g64.b, csq.b], writes=[PB[gb]])
                gsv = ps[:, gb * 512:(gb + 1) * 512].rearrange("p (c n) -> p c n", c=4)[:, :, 0:NQ]
                act.op(lambda e: e.activation(out=crs.t[:, :, 0:NQ], in_=gsv, func=AF.Ln, scale=1.0 / 64, bias=EPS), reads=[PB[gb]], writes=[crs.b])
                act.op(lambda e: e.activation(out=crs.t[:, :, 0:NQ], in_=crs.t[:, :, 0:NQ], func=AF.Exp, scale=-0.5), reads=[crs.b], writes=[crs.b])
                pool.op(lambda e: e.tensor_tensor(out=crs.t[:, :, 0:NQ], in0=crs.t[:, :, 0:NQ], in1=cacc.t[:, :, 0:NQ], op=ALU.mult),
                        reads=[crs.b, cacc.b], writes=[crs.b])
                for c in range(4):
                    dve.op(lambda e, c=c: e.tensor_scalar(out=mixT.t[:, 4 + c, 0:NQ], in0=crs.t[:, c, 0:NQ], scalar1=gncv.t[:, c:c + 1], scalar2=None,
                                                          op0=ALU.mult), reads=[crs.b, gncv.b], writes=[mixT.b])
                if kind == "sample":
                    for k in (2, 3, 4):
                        sp.dma(dK, BC[k].t[0:64, :], MODV[k, 1:65, :], reads=[dr["MODV"]], writes=[BC[k].b], group=True)
                    def _fix234():
                        for k in (2, 3, 4):
                            BC[k].b.w = (dK, S.dcount[dK])
                    S.defer(_fix234, writes=[BC[2].b, BC[3].b, BC[4].b])
                mb = bk["MIX"]
                for hf in range(2):
                    for c in range(8):
                        pe.op(lambda e, c=c, hf=hf: e.matmul(ps[rows, (mb + hf) * 512:(mb + hf + 1) * 512], lhsT=mixT.t[:, c, 0:NQ],
                                                             rhs=w_out_bf.t[:, c, hf * 512:(hf + 1) * 512], start=(c == 0), stop=(c == 7)),
                              reads=[mixT.b, w_out_bf.b], writes=[PB[mb + hf]])
                dve.op(lambda e: e.tensor_tensor(out=tmp.t[rows, :], in0=ps[rows, mb * 512:(mb + 2) * 512], in1=BC[2].t[rows, :], op=ALU.mult),
                       reads=[PB[mb], PB[mb + 1], BC[2].b], writes=[tmp.b])
                pool.op(lambda e: e.tensor_tensor(out=x1.t[rows, :], in0=tmp.t[rows, :], in1=xt.t[rows, :], op=ALU.add),
                        reads=[tmp.b, xt.b], writes=[x1.b])
                tok0 = NPB * 128 if kind == "sample" else (bi - 1) * 128
                sp.dma(dOX[par], X1[tok0:tok0 + NQ, :], x1.t[rows, :], reads=[x1.b], writes=[])
                yield
                act.op(lambda e: e.activation(out=junk2.t[rows, :], in_=x1.t[rows, :], func=AF.Square, accum_out=sq.t[rows, 2:3]),
                       reads=[x1.b], writes=[junk2.b, sq.b])
                act.op(lambda e: e.activation(out=sq.t[rows, 3:4], in_=sq.t[rows, 2:3], func=AF.Ln, scale=1.0 / D, bias=EPS),
                       reads=[sq.b], writes=[sq.b])
                act.op(lambda e: e.activation(out=sq.t[rows, 3:4], in_=sq.t[rows, 3:4], func=AF.Exp, scale=-0.5), reads=[sq.b], writes=[sq.b])
                dve.op(lambda e: e.scalar_tensor_tensor(out=tmp.t[rows, :], in0=x1.t[rows, :], scalar=sq.t[rows, 3:4], in1=BC[3].t[rows, :],
                                                        op0=ALU.mult, op1=ALU.mult), reads=[x1.b, sq.b, BC[3].b], writes=[tmp.b])
                pool.op(lambda e: e.tensor_tensor(out=h2_bf.t[rows, :], in0=tmp.t[rows, :], in1=BC[4].t[rows, :], op=ALU.add),
                        reads=[tmp.b, BC[4].b], writes=[h2_bf.b])
                sp.dma(dOH[par], H2[tok0:tok0 + NQ, :], h2_bf.t[rows, :], reads=[h2_bf.b], writes=[])
                tb2 = bank_bf(bk["H2T"])
                for c in range(8):
                    pe.op(lambda e, c=c: e.transpose(out=tb2[:, c * NQ:(c + 1) * NQ], in_=h2_bf.t[rows, c * 128:(c + 1) * 128],
                                                     identity=ident.t[rows, rows]), reads=[h2_bf.b, ident.b], writes=[PB[bk["H2T"]]])
                act.op(lambda e: e.copy(out=h2T.t[:, 0:8 * NQ], in_=tb2[:, 0:8 * NQ]), reads=[PB[bk["H2T"]]], writes=[h2T.b])
                lb = bk["LG"]
                for c in range(8):
                    pe.op(lambda e, c=c: e.matmul(ps[rows, lb * 512: lb * 512 + 36], lhsT=h2T.t[:, c * NQ:(c + 1) * NQ], rhs=w_r_bf.t[:, c, :],
                                                  start=(c == 0), stop=(c == 7)), reads=[h2T.b, w_r_bf.b], writes=[PB[lb]])
                dve.op(lambda e: e.tensor_tensor(out=lg.t[rows, :], in0=ps[rows, lb * 512: lb * 512 + 36], in1=brt.t[rows, :], op=ALU.add),
                       reads=[PB[lb], brt.b], writes=[lg.b])
                ti = NPB if kind == "sample" else bi - 1
                R = rt.t
                dve.op(lambda e: e.memset(R[rows, 0:8], 0.0), writes=[rt.b])
                dve.op(lambda e: e.tensor_reduce(out=R[rows, 0:1], in_=lg.t[rows, 0:4], axis=AX.X, op=ALU.max), reads=[lg.b], writes=[rt.b])
                dve.op(lambda e: e.tensor_scalar(out=R[rows, 1:2], in0=R[rows, 0:1], scalar1=-1.0, scalar2=None, op0=ALU.mult), reads=[rt.b], writes=[rt.b])
                act.op(lambda e: e.activation(out=R[rows, 4:8], in_=lg.t[rows, 0:4], func=AF.Exp, bias=R[rows, 1:2], scale=1.0, accum_out=R[rows, 2:3]),
                       reads=[lg.b, rt.b], writes=[rt.b])
                dve.op(lambda e: e.tensor_scalar(out=R[rows, 8:12], in0=lg.t[rows, 0:4], scalar1=R[rows, 0:1], scalar2=-1.0e30,
                                                 op0=ALU.is_lt, op1=ALU.mult), reads=[lg.b, rt.b], writes=[rt.b])
                dve.op(lambda e: e.tensor_tensor(out=R[rows, 16:48].rearrange("p (g n) -> p g n", g=4),
                                                 in0=lg.t[rows, 4:36].rearrange("p (g n) -> p g n", g=4),
                                                 in1=R[rows, 8:12].unsqueeze(2).to_broadcast([NQ, 4, 8]), op=ALU.add), reads=[lg.b, rt.b], writes=[rt.b])
                dve.op(lambda e: e.max(out=R[rows, 48:56], in_=R[rows, 16:48]), reads=[rt.b], writes=[rt.b])
                dve.op(lambda e: e.tensor_scalar(out=R[rows, 56:57], in0=R[rows, 48:49], scalar1=-1.0, scalar2=None, op0=ALU.mult), reads=[rt.b], writes=[rt.b])
                act.op(lambda e: e.activation(out=R[rows, 64:96], in_=R[rows, 16:48], func=AF.Exp, bias=R[rows, 56:57], scale=1.0),
                       reads=[rt.b], writes=[rt.b])
                selv = sel_all.t[rows, ti, :]
                dve.op(lambda e: e.tensor_scalar(out=selv, in0=R[rows, 16:48], scalar1=R[rows, 49:50], scalar2=None, op0=ALU.is_ge),
                       reads=[rt.b], writes=[sel_all.b])
                dve.op(lambda e: e.tensor_tensor(out=R[rows, 64:96], in0=R[rows, 64:96], in1=selv, op=ALU.mult), reads=[rt.b, sel_all.b], writes=[rt.b])
                dve.op(lambda e: e.tensor_reduce(out=R[rows, 57:58], in_=R[rows, 64:96], axis=AX.X, op=ALU.add), reads=[rt.b], writes=[rt.b])
                dve.op(lambda e: e.tensor_tensor(out=R[rows, 58:59], in0=R[rows, 57:58], in1=R[rows, 2:3], op=ALU.mult), reads=[rt.b], writes=[rt.b])
                dve.op(lambda e: e.reciprocal(out=R[rows, 58:59], in_=R[rows, 58:59]), reads=[rt.b], writes=[rt.b])
                dve.op(lambda e: e.tensor_scalar(out=gate_all.t[rows, ti, :], in0=R[rows, 64:96], scalar1=R[rows, 58:59], scalar2=None, op0=ALU.mult),
                       reads=[rt.b], writes=[gate_all.b])
                for i_, (l_, r_) in enumerate(((triu.t[rows, rows], selv), (onesf.t[:, rows], cumsel.t[:, :]))):
                    pe.op(lambda e, l_=l_, r_=r_, i_=i_: e.matmul(ps[rows, lb * 512 + 64: lb * 512 + 96], lhsT=l_, rhs=r_, start=(i_ == 0), stop=(i_ == 1)),
                          reads=[triu.b, onesf.b, sel_all.b, cumsel.b], writes=[PB[lb]])
                dve.op(lambda e: e.tensor_tensor(out=rs_all.t[rows, ti, :], in0=ps[rows, lb * 512 + 64: lb * 512 + 96], in1=selv, op=ALU.mult),
                       reads=[PB[lb], sel_all.b], writes=[rs_all.b])
                pool.op(lambda e: e.tensor_tensor(out=cumsel.t[rows, :], in0=cumsel.t[rows, :], in1=selv, op=ALU.add),
                        reads=[cumsel.b, sel_all.b], writes=[cumsel.b])
                if last_p:
                    act.dma(dNK, nk_p, qk_f.t[:, 512:640], reads=[qk_f.b], writes=[])
                    act.dma(dNV, nv_p, v_f.t[:, :], reads=[v_f.b], writes=[])
                    for c in range(4):
                        act.dma(dOut, ncv_p[:, c * 128:(c + 1) * 128].rearrange("j p -> p j"), ue.t[:, c, 128:130], reads=[ue.b], writes=[],
                                group=True, allow_slow_non_contiguous=True)
                if kind == "sample":
                    act.dma(dOut, nk_s[:, 0:124, :], ck_nat[:, 4:128, :], group=True)
                    act.dma(dOut, nv_s[:, 0:124, :], cv_nat[:, 4:128, :], group=True)
                    for s_ in range(NSEQ_S):
                        act.dma(dOut, nk_s[s_, 124:128, :], qk_f.t[4 * s_:4 * s_ + 4, 512:640], reads=[qk_f.b], writes=[], group=True)
                        act.dma(dOut, nv_s[s_, 124:128, :], v_f.t[4 * s_:4 * s_ + 4, :], reads=[v_f.b], writes=[], group=True)
                    for c in range(4):
                        for j_ in range(2):
                            act.dma(dOut, ncv_s[:, j_, c * 128:(c + 1) * 128].rearrange("s p -> p s"),
                                    ue.t[:, c, :].rearrange("p (s l) -> p s l", s=NSEQ_S)[:, :, 4 + j_], reads=[ue.b], writes=[],
                                    group=True, allow_slow_non_contiguous=True)

            progs = [S.record(block(0, "halo"))] + [S.record(block(bi, "prompt")) for bi in range(1, NBLK)]
            progs.append(S.record(block(NBLK, "sample")))
            S.merge_emit(progs, window=PIPE_WINDOW)
            for k_ in ("X1", "H2"):
                dr[k_].w = None
                dr[k_].r = []
            barrier_all(S, dOX + dOH + [dNK, dNV])

        sW.close()
        with ExitStack() as s2:
            cnt = mk(s2, "cnt", [128, NE], F32)
            thr = mk(s2, "thr", [128, 128], F32)
            big = mk(s2, "big", [128, NB * NE], F32)
            nblk = mk(s2, "nblk", [128, NE], F32)
            pend = [mk(s2, "pend%d" % i, [128, NE], F32) for i in range(2)]
            pstart = mk(s2, "pstart", [128, NE], F32)
            blke = mk(s2, "blke", [128, NB], F32)
            widx_f = mk(s2, "widx_f", [128, NB], F32)
            widx = mk(s2, "widx", [128, NB], I32)
            piota = mk(s2, "piota", [128, 1], F32)
            tokid = mk(s2, "tokid", [128, NTT], I32)
            dmt = mk(s2, "dmt", [128, NTT, NE], F32)
            eqt = mk(s2, "eqt", [128, NTT, NE], F32)
            dA = mk(s2, "dA", [128, NTT], F32)
            dB = mk(s2, "dB", [128, NTT], F32)
            gA = mk(s2, "gA", [128, NTT], F32)
            gB = mk(s2, "gB", [128, NTT], F32)
            dAi = mk(s2, "dAi", [128, NTT], I32)
            dBi = mk(s2, "dBi", [128, NTT], I32)
            twA = mk(s2, "twA", [128, NTT, 4], I32)
            twB = mk(s2, "twB", [128, NTT, 4], I32)
            d2 = S.dsem()
            sp.dma(d2, thr.t[:], thr_d.partition_broadcast(128), writes=[thr.b], group=True)
            sp.dma(d2, piota.t[:], piota_d, writes=[piota.b], group=True)
            sp.dma(d2, tokid.t[:], tokid_d, writes=[tokid.b], group=True)
            for t_ in (thr, piota, tokid):
                t_.b.w = (d2, S.dcount[d2])
            pe.op(lambda e: e.matmul(bank(0)[:, 0:NE], lhsT=onesf.t[:, :], rhs=cumsel.t[:, :], start=True, stop=True),
                  reads=[onesf.b, cumsel.b], writes=[PB[0]])
            dve.op(lambda e: e.tensor_copy(out=cnt.t[:], in_=bank(0)[:, 0:NE]), reads=[PB[0]], writes=[cnt.b])
            bv = big.t[:, 0:NE * 66].rearrange("p (e j) -> p e j", e=NE)
            dve.op(lambda e: e.tensor_tensor(out=bv, in0=cnt.t[:].unsqueeze(2).to_broadcast([128, NE, 66]),
                                             in1=thr.t[:, 0:66].unsqueeze(1).to_broadcast([128, NE, 66]), op=ALU.is_gt),
                   reads=[cnt.b, thr.b], writes=[big.b])
            dve.op(lambda e: e.tensor_reduce(out=nblk.t[:], in_=bv, axis=AX.X, op=ALU.add), reads=[big.b], writes=[nblk.b])
            dve.op(lambda e: e.tensor_scalar(out=nblk.t[:], in0=nblk.t[:], scalar1=128.0, scalar2=None, op0=ALU.mult), reads=[nblk.b], writes=[nblk.b])
            dve.op(lambda e: e.tensor_copy(out=pend[0].t[:], in_=nblk.t[:]), reads=[nblk.b], writes=[pend[0].b])
            cur = 0
            for sh in (1, 2, 4, 8, 16):
                a_, b_ = pend[cur], pend[1 - cur]
                dve.op(lambda e, a_=a_, b_=b_: e.tensor_copy(out=b_.t[:, 0:sh], in_=a_.t[:, 0:sh]), reads=[a_.b], writes=[b_.b])
                dve.op(lambda e, a_=a_, b_=b_, sh=sh: e.tensor_tensor(out=b_.t[:, sh:NE], in0=a_.t[:, sh:NE], in1=a_.t[:, 0:NE - sh], op=ALU.add),
                       reads=[a_.b], writes=[b_.b])
                cur = 1 - cur
            pe_ = pend[cur]
            dve.op(lambda e: e.tensor_tensor(out=pstart.t[:], in0=pe_.t[:], in1=nblk.t[:], op=ALU.subtract), reads=[pe_.b, nblk.b], writes=[pstart.b])
            bv2 = big.t[:, 0:NB * NE].rearrange("p (b e) -> p b e", b=NB)
            dve.op(lambda e: e.tensor_tensor(out=bv2, in0=pe_.t[:].unsqueeze(1).to_broadcast([128, NB, NE]),
                                             in1=thr.t[:, 0:NB].unsqueeze(2).to_broadcast([128, NB, NE]), op=ALU.is_le),
                   reads=[pe_.b, thr.b], writes=[big.b])
            dve.op(lambda e: e.tensor_reduce(out=blke.t[:], in_=bv2, axis=AX.X, op=ALU.add), reads=[big.b], writes=[blke.b])
            dve.op(lambda e: e.tensor_scalar(out=blke.t[:], in0=blke.t[:], scalar1=float(NE - 1), scalar2=None, op0=ALU.min), reads=[blke.b], writes=[blke.b])
            dve.op(lambda e: e.tensor_scalar(out=widx_f.t[:], in0=blke.t[:], scalar1=128.0, scalar2=piota.t[:, 0:1], op0=ALU.mult, op1=ALU.add),
                   reads=[blke.b, piota.b], writes=[widx_f.b])
            dve.op(lambda e: e.tensor_tensor(out=big.t[:, 0:NB - 1], in0=blke.t[:, 1:NB], in1=blke.t[:, 0:NB - 1], op=ALU.is_equal),
                   reads=[blke.b], writes=[big.b])
            dve.op(lambda e: e.scalar_tensor_tensor(out=widx_f.t[:, 1:NB], in0=big.t[:, 0:NB - 1], scalar=OOB, in1=widx_f.t[:, 1:NB],
                                                    op0=ALU.mult, op1=ALU.add), reads=[big.b, widx_f.b], writes=[widx_f.b])
            for b0 in STREAM_STARTS[1:NS_W]:
                dve.op(lambda e, b0=b0: e.tensor_scalar(out=widx_f.t[:, b0:b0 + 1], in0=blke.t[:, b0:b0 + 1], scalar1=128.0, scalar2=piota.t[:, 0:1],
                                                        op0=ALU.mult, op1=ALU.add), reads=[blke.b, piota.b, widx_f.b], writes=[widx_f.b])
            dve.op(lambda e: e.tensor_copy(out=widx.t[:], in_=widx_f.t[:]), reads=[widx_f.b], writes=[widx.b])
            dve.op(lambda e: e.tensor_tensor(out=dmt.t[:], in0=sel_all.t[:], in1=pstart.t[:].unsqueeze(1).to_broadcast([128, NTT, NE]), op=ALU.mult),
                   reads=[sel_all.b, pstart.b], writes=[dmt.b])
            dve.op(lambda e: e.tensor_tensor(out=dmt.t[:], in0=dmt.t[:], in1=rs_all.t[:], op=ALU.add), reads=[dmt.b, rs_all.b], writes=[dmt.b])
            dve.op(lambda e: e.tensor_reduce(out=dA.t[:], in_=dmt.t[:], axis=AX.X, op=ALU.max), reads=[dmt.b], writes=[dA.b])
            dve.op(lambda e: e.tensor_reduce(out=dB.t[:], in_=dmt.t[:], axis=AX.X, op=ALU.add), reads=[dmt.b], writes=[dB.b])
            dve.op(lambda e: e.tensor_tensor(out=dB.t[:], in0=dB.t[:], in1=dA.t[:], op=ALU.subtract), reads=[dB.b, dA.b], writes=[dB.b])
            dve.op(lambda e: e.tensor_tensor(out=eqt.t[:], in0=dmt.t[:], in1=dA.t[:].unsqueeze(2).to_broadcast([128, NTT, NE]), op=ALU.is_equal),
                   reads=[dmt.b, dA.b], writes=[eqt.b])
            dve.op(lambda e: e.tensor_tensor(out=eqt.t[:], in0=eqt.t[:], in1=gate_all.t[:], op=ALU.mult), reads=[eqt.b, gate_all.b], writes=[eqt.b])
            dve.op(lambda e: e.tensor_reduce(out=gA.t[:], in_=eqt.t[:], axis=AX.X, op=ALU.add), reads=[eqt.b], writes=[gA.b])
            dve.op(lambda e: e.tensor_reduce(out=gB.t[:], in_=gate_all.t[:], axis=AX.X, op=ALU.add), reads=[gate_all.b], writes=[gB.b])
            dve.op(lambda e: e.tensor_tensor(out=gB.t[:], in0=gB.t[:], in1=gA.t[:], op=ALU.subtract), reads=[gB.b, gA.b], writes=[gB.b])
            dve.op(lambda e: e.tensor_copy(out=dAi.t[:], in_=dA.t[:]), reads=[dA.b], writes=[dAi.b])
            dve.op(lambda e: e.tensor_copy(out=dBi.t[:], in_=dB.t[:]), reads=[dB.b], writes=[dBi.b])
            for tw_, g_, off in ((twA, gA, 0), (twB, gB, HROWS)):
                pool.op(lambda e, tw_=tw_: e.memset(tw_.t[:], 0), writes=[tw_.b])
                pool.op(lambda e, tw_=tw_: e.tensor_copy(out=tw_.t[:, :, 0], in_=tokid.t[:]), reads=[tokid.b], writes=[tw_.b])
                pool.op(lambda e, tw_=tw_, off=off: e.tensor_single_scalar(out=tw_.t[:, :, 1], in_=tokid.t[:], scalar=off, op=ALU.add),
                        reads=[tokid.b], writes=[tw_.b])
                pool.op(lambda e, tw_=tw_, g_=g_: e.tensor_copy(out=tw_.t[:].bitcast(F32)[:, :, 2], in_=g_.t[:]), reads=[g_.b], writes=[tw_.b])
            dScs = [S.dsem() for _ in range(6)]
            nsc = 0
            for ti in range(NTT):
                nq = 64 if ti == NPB else 128
                for tw_, di_ in ((twA, dAi), (twB, dBi)):
                    dSc = dScs[nsc % 6]
                    nsc += 1
                    pool.dma(dSc, TW[:, :], tw_.t[0:nq, ti, :], reads=[tw_.b, di_.b, dr["TW"]], writes=[],
                             indirect=dict(out_offset=bass.IndirectOffsetOnAxis(ap=di_.t[0:nq, ti:ti + 1], axis=0), in_offset=None,
                                           bounds_check=bc_tw, oob_is_err=False))
            for dSc in dScs:
                pool.wait_ev((dSc, S.dcount[dSc]))
                sp.wait_ev((dSc, S.dcount[dSc]))
            dr["TW"].w = None
            dr["TW"].r = []

            with ExitStack() as s3:
                Wb = [mk(s3, "Wb%d" % i, [128, WROW], BF16) for i in range(NS_W)]
                order = []
                for i_ in range(max(STREAM_STARTS[k_ + 1] - STREAM_STARTS[k_] for k_ in range(NS_W))):
                    for k_ in range(NS_W):
                        if STREAM_STARTS[k_] + i_ < STREAM_STARTS[k_ + 1]:
                            order.append((STREAM_STARTS[k_] + i_, k_))
                assert sorted(b_ for b_, _ in order) == list(range(NB))
                twb = [mk(s3, "twb%d" % i, [128, 4], I32) for i in range(4)]
                xg = [mk(s3, "xg%d" % i, [128, D], BF16) for i in range(2)]
                xgT = [mk(s3, "xgT%d" % i, [128, 8, 128], BF16) for i in range(2)]
                sg = [mk(s3, "sg%d" % i, [128, 512], F32) for i in range(2)]
                aT = [mk(s3, "aT%d" % i, [128, 4, 128], BF16) for i in range(2)]
                ysb = [mk(s3, "ysb%d" % i, [128, D], F32) for i in range(2)]
                dWg = [S.dsem() for _ in range(NS_W)]
                dTw = [S.dsem() for _ in range(4)]
                dXg = [S.dsem(), S.dsem()]
                dYs = [S.dsem(), S.dsem()]

                def load_tw(pos):
                    b, ws = order[pos]
                    p4 = pos % 4
                    sp.dma(dTw[p4], twb[p4].t[:], TW[b * 128:(b + 1) * 128, :], reads=[dr["TW"]], writes=[twb[p4].b])

                def load_w(pos):
                    b, ws = order[pos]
                    pool.dma(dWg[ws], Wb[ws].t[:, :], wall[:, :], reads=[widx.b], writes=[Wb[ws].b],
                             indirect=dict(out_offset=None, in_offset=bass.IndirectOffsetOnAxis(ap=widx.t[:, b:b + 1], axis=0),
                                           bounds_check=bc_w, oob_is_err=False))

                def load_x(pos):
                    p2, p4 = pos % 2, pos % 4
                    pool.dma(dXg[p2], xg[p2].t[:, :], H2[:, :], reads=[twb[p4].b, dr["H2"]], writes=[xg[p2].b],
                             indirect=dict(out_offset=None, in_offset=bass.IndirectOffsetOnAxis(ap=twb[p4].t[:, 0:1], axis=0)))

                for pos in range(3):
                    load_tw(pos)
                for pos in range(NS_W):
                    load_w(pos)
                load_x(0)
                load_x(1)
                for pos in range(NB):
                    if pos + 3 < NB:
                        load_tw(pos + 3)
                    b, ws = order[pos]
                    p2, p4 = pos % 2, pos % 4
                    W = Wb[ws]
                    tbm = bank_bf(0)
                    for c in range(8):
                        pe.op(lambda e, c=c: e.transpose(out=tbm[:, c * 128:(c + 1) * 128], in_=xg[p2].t[:, c * 128:(c + 1) * 128], identity=ident.t[:, :]),
                              reads=[xg[p2].b, ident.b], writes=[PB[0]])
                    act.op(lambda e: e.copy(out=xgT[p2].t[:].rearrange("p c n -> p (c n)"), in_=tbm[:, 0:1024]), reads=[PB[0]], writes=[xgT[p2].b])
                    if pos + 2 < NB:
                        load_x(pos + 2)
                    gbk, ubk = 1 + p2, 3 + p2
                    for (bk_, off) in ((gbk, 0), (ubk, 4096)):
                        for j in range(4):
                            for c in range(8):
                                pe.op(lambda e, c=c, j=j, bk_=bk_, off=off: e.matmul(
                                    ps[:, bk_ * 512 + j * 128: bk_ * 512 + (j + 1) * 128],
                                    lhsT=W.t[:, off + c * 512 + j * 128: off + c * 512 + (j + 1) * 128], rhs=xgT[p2].t[:, c, :],
                                    start=(c == 0), stop=(c == 7)), reads=[W.b, xgT[p2].b], writes=[PB[bk_]])
                    act.op(lambda e: e.activation(out=sg[p2].t[:], in_=bank(gbk), func=AF.Silu), reads=[PB[gbk]], writes=[sg[p2].b])
                    dve.op(lambda e: e.tensor_tensor(out=aT[p2].t[:].rearrange("p j n -> p (j n)"), in0=bank(ubk), in1=sg[p2].t[:], op=ALU.mult),
                           reads=[PB[ubk], sg[p2].b], writes=[aT[p2].b])
                    for hf in range(2):
                        for j in range(4):
                            pe.op(lambda e, hf=hf, j=j: e.matmul(ps[:, (5 + hf) * 512:(6 + hf) * 512], lhsT=aT[p2].t[:, j, :],
                                                                 rhs=W.t[:, 8192 + j * 1024 + hf * 512: 8192 + j * 1024 + (hf + 1) * 512],
                                                                 start=(j == 0), stop=(j == 3)), reads=[aT[p2].b, W.b], writes=[PB[5 + hf]])
                    gcol = twb[p4].t[:].bitcast(F32)[:, 2:3]
                    act.op(lambda e: e.mul(out=ysb[p2].t[:, 0:512], in_=bank(5), mul=gcol),
                           reads=[PB[5], twb[p4].b], writes=[ysb[p2].b])
                    dve.op(lambda e: e.tensor_scalar(out=ysb[p2].t[:, 512:1024], in0=bank(6), scalar1=gcol, scalar2=None, op0=ALU.mult),
                           reads=[PB[6], twb[p4].b], writes=[ysb[p2].b])
                    pool.dma(dYs[p2], Y[:, :], ysb[p2].t[:, :], reads=[ysb[p2].b, twb[p4].b], writes=[],
                             indirect=dict(out_offset=bass.IndirectOffsetOnAxis(ap=twb[p4].t[:, 1:2], axis=0), in_offset=None,
                                           bounds_check=bc_y, oob_is_err=False))
                    if pos + NS_W < NB:
                        load_w(pos + NS_W)
                for k_ in dYs:
                    pool.wait_ev((k_, S.dcount[k_]))
                    sp.wait_ev((k_, S.dcount[k_]))
                barrier_all(S)

        with ExitStack() as s4:
            GFp = mk(s4, "GFp", [128, D], F32)
            GFs = mk(s4, "GFs", [64, D], F32)
            FN = mk(s4, "FN", [128, D], F32)
            d4 = S.dsem()
            sp.dma(d4, GFp.t[:], MODV[5, 0:1, :].partition_broadcast(128), writes=[GFp.b], group=True)
            sp.dma(d4, GFs.t[:, :], MODV[5, 1:65, :], writes=[GFs.b], group=True)
            sp.dma(d4, FN.t[:], final_norm.partition_broadcast(128), writes=[FN.b], group=True)
            for t_ in (GFp, GFs, FN):
                t_.b.w = (d4, S.dcount[d4])
            NR = 3
            xa = [mk(s4, "xa%d" % i, [128, D], F32) for i in range(NR)]
            ya = [mk(s4, "ya%d" % i, [128, D], F32) for i in range(NR)]
            yb = [mk(s4, "yb%d" % i, [128, D], F32) for i in range(NR)]
            yo = [mk(s4, "yo%d" % i, [128, D], F32) for i in range(NR)]
            jk = mk(s4, "jk", [128, D], BF16)
            sf = [mk(s4, "sf%d" % i, [128, 4], F32) for i in range(NR)]
            dL = [S.dsem() for _ in range(NR)]
            dSt = [S.dsem() for _ in range(NR)]

            def ftile(ti):
                p = ti % NR
                nq = 64 if ti == NPB else 128
                r_ = slice(0, nq)
                t0 = ti * 128
                GF = GFs if ti == NPB else GFp
                sp.dma(dL[p], xa[p].t[r_, :], X1[t0:t0 + nq, :], writes=[xa[p].b], group=True)
                sp.dma(dL[p], ya[p].t[r_, :], Y[t0:t0 + nq, :], writes=[ya[p].b], group=True)
                sp.dma(dL[p], yb[p].t[r_, :], Y[HROWS + t0:HROWS + t0 + nq, :], writes=[yb[p].b], group=True)

                def _fix():
                    for t_ in (xa[p], ya[p], yb[p]):
                        t_.b.w = (dL[p], S.dcount[dL[p]])
                S.defer(_fix, writes=[xa[p].b, ya[p].b, yb[p].b])
                dve.op(lambda e: e.tensor_tensor(out=ya[p].t[r_, :], in0=ya[p].t[r_, :], in1=yb[p].t[r_, :], op=ALU.add),
                       reads=[ya[p].b, yb[p].b], writes=[ya[p].b])
                dve.op(lambda e: e.tensor_tensor(out=ya[p].t[r_, :], in0=ya[p].t[r_, :], in1=GF.t[r_, :], op=ALU.mult),
                       reads=[ya[p].b, GF.b], writes=[ya[p].b])
                pool.op(lambda e: e.tensor_tensor(out=xa[p].t[r_, :], in0=xa[p].t[r_, :], in1=ya[p].t[r_, :], op=ALU.add),
                        reads=[xa[p].b, ya[p].b], writes=[xa[p].b])
                dve.op(lambda e: e.memset(sf[p].t[:], 0.0), writes=[sf[p].b])
                act.op(lambda e: e.activation(out=jk.t[r_, :], in_=xa[p].t[r_, :], func=AF.Square, accum_out=sf[p].t[r_, 0:1]),
                       reads=[xa[p].b, sf[p].b], writes=[jk.b, sf[p].b])
                act.op(lambda e: e.activation(out=sf[p].t[r_, 1:2], in_=sf[p].t[r_, 0:1], func=AF.Ln, scale=1.0 / D, bias=EPS),
                       reads=[sf[p].b], writes=[sf[p].b])
                act.op(lambda e: e.activation(out=sf[p].t[r_, 1:2], in_=sf[p].t[r_, 1:2], func=AF.Exp, scale=-0.5), reads=[sf[p].b], writes=[sf[p].b])
                dve.op(lambda e: e.scalar_tensor_tensor(out=yo[p].t[r_, :], in0=xa[p].t[r_, :], scalar=sf[p].t[r_, 1:2], in1=FN.t[r_, :],
                                                        op0=ALU.mult, op1=ALU.mult), reads=[xa[p].b, sf[p].b, FN.b], writes=[yo[p].b])
                dst = y_s if ti == NPB else y_p[t0:t0 + nq, :]
                sp.dma(dSt[p], dst, yo[p].t[r_, :], reads=[yo[p].b], writes=[])
                yield

            S.merge_emit([S.record(ftile(ti)) for ti in range(NTT)], window=NR)
            for k_ in dSt:
                sp.wait_ev((k_, S.dcount[k_]))
            sp.wait_ev((dOut, S.dcount[dOut]))
            barrier_all(S)
    return nc


def barrier_all(S, dsems=()):
    snap = {k: e.n for k, e in S.E.items()}
    for name, e in S.E.items():
        for k, v in snap.items():
            if k == name or v == 0:
                continue
            e.wait_ev((k, v))
        for k in dsems:
            if S.dcount[k] > 0:
                e.wait_ev((k, S.dcount[k]))


_CACHE = {}


def _consts():
    if "c" in _CACHE:
        return _CACHE["c"]
    bf = ml_dtypes.bfloat16
    c = {}
    c["ident"] = np.eye(128, dtype=np.float32).astype(bf)
    c["triu"] = np.triu(np.ones((128, 128), np.float32), 1)
    c["ones"] = np.ones((128, 128), np.float32)
    g = np.zeros((128, 128), np.float32)
    g[:64, :64] = 1
    g[64:, 64:] = 1
    c["g64"] = g
    i = np.arange(128)[:, None]
    j = np.arange(128)[None, :]
    prev = np.where(j > i, 0.0, NEG)
    cur = np.where(j <= i, 0.0, NEG)
    c["maskp"] = np.concatenate([prev, cur], 1).astype(np.float32).astype(bf)
    c["mask0_first"] = np.concatenate([np.full((128, 128), NEG), cur], 1).astype(np.float32).astype(bf)
    ms = np.full((64, 2112), NEG, np.float32)
    for t in range(64):
        s, jj = t // 4, t % 4
        ms[t, s * 128 + jj + 1:(s + 1) * 128] = 0.0
        ms[t, 2048 + 4 * s: 2048 + 4 * s + jj + 1] = 0.0
    c["masks"] = np.concatenate([ms, ms], 0).astype(bf)
    c["thr"] = (128.0 * np.arange(128, dtype=np.float32))[None, :]
    c["piota"] = np.arange(128, dtype=np.float32)[:, None]
    tok = (np.arange(NTT)[None, :] * 128 + np.arange(128)[:, None]).astype(np.int32)
    c["tokid"] = tok
    tw = np.zeros((128, 4), np.int32)
    tw[:, 0] = NTOK
    tw[:, 1] = 2 * HROWS
    c["twinit"] = tw
    _CACHE["c"] = c
    return c


def _rope_tables(pos):
    half = 32
    inv = (10000.0 ** (-np.arange(half, dtype=np.float32) / half)).astype(np.float32)
    ang = pos.astype(np.float32)[:, None] * inv[None, :]
    return np.cos(ang).astype(np.float32), np.sin(ang).astype(np.float32)


def kernel(x_prompt, x_sample, cache_k, cache_v, state_conv, c_prompt, c_sample,
           attn_norm, ffn_norm, w_mod, b_mod, w_in, conv_w, attn_sinks,
           out_norm_attn, out_norm_conv, w_out, w_group, b_group, w_expert, b_expert,
           w_gate, w_up, w_down, final_norm):
    f = lambda a: np.ascontiguousarray(np.asarray(a, dtype=np.float32))
    x_prompt, x_sample, cache_k, cache_v, state_conv = map(f, (x_prompt, x_sample, cache_k, cache_v, state_conv))
    c_prompt, c_sample = f(c_prompt), f(c_sample)
    C = _consts()
    if "nc" not in _CACHE:
        _CACHE["nc"] = build_program()
    nc = _CACHE["nc"]

    wg, wu, wd = f(w_gate)[0], f(w_up)[0], f(w_down)[0]
    wall = np.concatenate([
        wg.reshape(NE, 8, 128, 512).transpose(0, 2, 1, 3).reshape(NE, 128, 4096),
        wu.reshape(NE, 8, 128, 512).transpose(0, 2, 1, 3).reshape(NE, 128, 4096),
        wd.reshape(NE, 4, 128, 1024).transpose(0, 2, 1, 3).reshape(NE, 128, 4096)], axis=2).reshape(NE * 128, WROW)
    wall = np.ascontiguousarray(wall)
    shared = {
        "w_mod": f(w_mod)[0], "b_mod": f(b_mod)[0][None, :], "attn_norm": f(attn_norm)[0][None, :], "ffn_norm": f(ffn_norm)[0][None, :],
        "final_norm": f(final_norm)[None, :], "w_in": f(w_in)[0], "w_out": f(w_out)[0],
        "w_r": np.ascontiguousarray(np.concatenate([f(w_group)[0], f(w_expert)[0]], 1)),
        "b_r": np.concatenate([f(b_group)[0], f(b_expert)[0]])[None, :],
        "convwT": np.ascontiguousarray(f(conv_w)[0].reshape(3, 4, 128).transpose(2, 1, 0).reshape(128, 12)),
        "sinks": f(attn_sinks)[0][None, :], "gn_attn": f(out_norm_attn)[0][None, :],
        "gn_convT": np.ascontiguousarray(f(out_norm_conv)[0].reshape(4, 128).T),
        "wall": wall,
        "ident": C["ident"], "triu": C["triu"], "ones": C["ones"], "g64": C["g64"], "maskp": C["maskp"], "masks": C["masks"],
        "tokid": C["tokid"], "piota": C["piota"], "thr": C["thr"], "twinit": C["twinit"],
    }
    cs_s, sn_s = _rope_tables(16384 + (np.arange(64) % 4))
    in_maps = []
    for i in range(8):
        seq, half = i // 2, i % 2
        xpc = np.zeros((NBLK * 128, D), np.float32)
        xpc[128:] = x_prompt[seq, half * 4096:(half + 1) * 4096]
        if half == 1:
            xpc[:128] = x_prompt[seq, 4096 - 128:4096]
        pos = half * 4096 - 128 + np.arange(NBLK * 128)
        cs, sn = _rope_tables(np.maximum(pos, 0))
        ss = slice(16 * i, 16 * i + 16)
        ck = cache_k[0, ss]
        kct = ck.transpose(2, 3, 0, 1).reshape(2, 64, 16 * 128)
        kct = np.concatenate([kct, kct], axis=1).transpose(1, 0, 2)
        m = dict(shared)
        m.update({
            "xp": xpc, "xs": np.ascontiguousarray(x_sample[ss].reshape(64, D)),
            "cT": np.ascontiguousarray(np.concatenate([c_prompt[seq:seq + 1], np.repeat(c_sample[ss], 4, axis=0)], 0).T),
            "kcT": np.ascontiguousarray(kct),
            "vc": np.ascontiguousarray(cache_v[0, ss].reshape(16, 128, 128).transpose(1, 0, 2)),
            "ck_nat": np.ascontiguousarray(ck.reshape(16, 128, 128)),
            "cv_nat": np.ascontiguousarray(cache_v[0, ss].reshape(16, 128, 128)),
            "stT": np.ascontiguousarray(state_conv[0, ss].reshape(16, 2, 4, 128).transpose(3, 2, 0, 1)),
            "mask0": C["mask0_first"] if half == 0 else C["maskp"],
            "cosp": np.ascontiguousarray(cs.reshape(NBLK, 128, 32).transpose(1, 0, 2)),
            "sinp": np.ascontiguousarray(sn.reshape(NBLK, 128, 32).transpose(1, 0, 2)),
            "coss": cs_s, "sins": sn_s,
            "flag": np.full((128, 1), float(half), np.float32),
        })
        in_maps.append(m)
    res = run_bass_kernel_spmd(nc, in_maps, core_ids=list(range(8)))
    R = res.results
    y_prompt = np.stack([np.concatenate([R[2 * s]["y_p"], R[2 * s + 1]["y_p"]], 0) for s in range(4)], 0)
    y_sample = np.concatenate([R[i]["y_s"].reshape(16, 4, D) for i in range(8)], 0)
    nkp = np.stack([R[2 * s + 1]["nk_p"].reshape(128, 2, 64) for s in range(4)], 0)[None]
    nvp = np.stack([R[2 * s + 1]["nv_p"].reshape(128, 2, 64) for s in range(4)], 0)[None]
    ncp = np.stack([R[2 * s + 1]["ncv_p"] for s in range(4)], 0)[None]
    nks = np.concatenate([R[i]["nk_s"].reshape(16, 128, 2, 64) for i in range(8)], 0)[None]
    nvs = np.concatenate([R[i]["nv_s"].reshape(16, 128, 2, 64) for i in range(8)], 0)[None]
    ncs = np.concatenate([R[i]["ncv_s"] for i in range(8)], 0)[None]
    return (y_prompt.astype(np.float32), y_sample.astype(np.float32), nkp.astype(np.float32), nvp.astype(np.float32),
            ncp.astype(np.float32), nks.astype(np.float32), nvs.astype(np.float32), ncs.astype(np.float32))
```

```python
import numpy as np
import ml_dtypes
from contextlib import ExitStack
import concourse.bass as bass
import concourse.mybir as mybir
from concourse.bass_utils import run_bass_kernel_spmd

F32 = mybir.dt.float32
BF16 = mybir.dt.bfloat16
I32 = mybir.dt.int32
AF = mybir.ActivationFunctionType
ALU = mybir.AluOpType
AX = mybir.AxisListType

D = 1024
NPB = 32
NBLK = NPB + 1
NSEQ_S = 16
NTOK = NPB * 128 + 64
NTT = 33
HROWS = 4224
NB = 97
NE = 32
EPS = 1e-5
NEG = -30000.0
OOB = 4096.0
WROW = 12288
NS_W = 5
PIPE_WINDOW = 3
STREAM_STARTS = [0, 20, 40, 59, 78, 97]
SAME_ENGINE_SYNC = ("pool", "act", "dve")


class Buf:
    __slots__ = ("name", "w", "r", "excl")

    def __init__(self, name="", excl=False):
        self.name = name
        self.w = None
        self.r = []
        self.excl = excl


class Eng:
    def __init__(self, S, name, eng, sem):
        self.S = S
        self.name = name
        self.eng = eng
        self.sem = sem
        self.n = 0
        self.seen = {}

    @staticmethod
    def _need(ev, deps):
        if ev is None:
            return
        k, v = ev
        if deps.get(k, 0) < v:
            deps[k] = v

    def sync(self, reads, writes):
        deps = {}
        for b in reads:
            self._need(b.w, deps)
            if b.excl:
                for ev in b.r:
                    if ev[0] != self.name:
                        self._need(ev, deps)
        for b in writes:
            self._need(b.w, deps)
            for ev in b.r:
                self._need(ev, deps)
        for k, v in deps.items():
            if k == self.name and self.name not in SAME_ENGINE_SYNC:
                continue
            if self.seen.get(k, 0) >= v:
                continue
            self.eng.wait_ge(self.S.sems[k], v)
            self.seen[k] = v

    def op(self, fn, reads=(), writes=(), cost=None):
        if self.S.rec is not None:
            self.S.rec.append(("op", self, fn, tuple(reads), tuple(writes), cost))
            return None
        self.sync(reads, writes)
        ins = fn(self.eng)
        self.n += 1
        ins.then_inc(self.sem, 1)
        ev = (self.name, self.n)
        for b in reads:
            b.r.append(ev)
            if len(b.r) > 24:
                b.r = self._compact(b.r)
        for b in writes:
            b.w = ev
            b.r = []
        return ins

    @staticmethod
    def _compact(evs):
        best = {}
        for k, v in evs:
            if best.get(k, 0) < v:
                best[k] = v
        return list(best.items())

    def dma(self, dsem, out, in_, reads=(), writes=(), indirect=None, group=False, **kw):
        if self.S.rec is not None:
            kw2 = dict(kw)
            kw2.update(indirect=indirect, group=group)
            self.S.rec.append(("dma", self, (dsem, out, in_), tuple(reads), tuple(writes), kw2))
            return None
        self.sync(reads, writes)
        if (not group or self.name == "pool") and self.S.dcount[dsem] > 0:
            self.wait_ev((dsem, self.S.dcount[dsem]))
        if indirect is None:
            ins = self.eng.dma_start(out=out, in_=in_, **kw)
        else:
            ins = self.eng.indirect_dma_start(out=out, in_=in_, **indirect, **kw)
        self.S.dcount[dsem] += 16
        ins.then_inc(self.S.sems[dsem], 16)
        ev = (dsem, self.S.dcount[dsem])
        for b in reads:
            b.r.append(ev)
        for b in writes:
            b.w = ev
            b.r = []
        return ins

    def wait_ev(self, ev):
        k, v = ev
        if self.seen.get(k, 0) >= v:
            return
        self.eng.wait_ge(self.S.sems[k], v)
        self.seen[k] = v


class Sched:
    def __init__(self, nc, stack, n_dma_sems):
        self.nc = nc
        self.sems = {}
        self.dcount = {}
        self.E = {}
        self.rec = None
        for name, eng in (("pe", nc.tensor), ("act", nc.scalar), ("dve", nc.vector),
                          ("pool", nc.gpsimd), ("sp", nc.sync)):
            s = stack.enter_context(nc.semaphore("s_" + name))
            self.sems[name] = s
            self.E[name] = Eng(self, name, eng, s)
        self.free = []
        for i in range(n_dma_sems):
            k = "d%d" % i
            self.sems[k] = stack.enter_context(nc.semaphore(k))
            self.dcount[k] = 0
            self.free.append(k)

    def dsem(self):
        return self.free.pop()

    def defer(self, fn, reads=(), writes=()):
        if self.rec is not None:
            self.rec.append(("call", None, fn, tuple(reads), tuple(writes), None))
        else:
            fn()

    def record(self, gen):
        assert self.rec is None
        self.rec = []
        for _ in gen:
            pass
        r, self.rec = self.rec, None
        return r

    def merge_emit(self, progs, window=2):
        ENG_COST = {"pe": 0.09, "dve": 0.43, "act": 0.50, "pool": 0.85, "sp": 0.05}
        eng_free = {}
        buf_ready = {}
        active = []
        nxt = 0

        def pend(rs):
            pr, pw = {}, {}
            for rec in rs:
                for b in rec[3]:
                    pr[id(b)] = pr.get(id(b), 0) + 1
                for b in rec[4]:
                    pw[id(b)] = pw.get(id(b), 0) + 1
            return pr, pw

        while nxt < len(progs) or active:
            while nxt < len(progs) and len(active) < window:
                pr, pw = pend(progs[nxt])
                active.append([progs[nxt], 0, pr, pw])
                nxt += 1
            best = None
            for ai, a in enumerate(active):
                rec = a[0][a[1]]
                ok = True
                for o in active[:ai]:
                    for b in rec[4]:
                        if o[2].get(id(b), 0) or o[3].get(id(b), 0):
                            ok = False
                            break
                    if ok:
                        for b in rec[3]:
                            if o[3].get(id(b), 0):
                                ok = False
                                break
                    if not ok:
                        break
                if not ok:
                    continue
                kind, eng = rec[0], rec[1]
                en = eng.name if eng is not None else "sp"
                t0 = eng_free.get(en, 0.0)
                for b in rec[3] + rec[4]:
                    t0 = max(t0, buf_ready.get(id(b), 0.0))
                if best is None or t0 < best[0] - 1e-9:
                    best = (t0, ai, en)
            t0, ai, en = best
            a = active[ai]
            rec = a[0][a[1]]
            kind, eng = rec[0], rec[1]
            if kind == "op":
                eng.op(rec[2], reads=rec[3], writes=rec[4])
                c = rec[5] if rec[5] is not None else ENG_COST[en]
                eng_free[en] = t0 + c
                done = t0 + c + 0.15
            elif kind == "dma":
                dsem, out, in_ = rec[2]
                eng.dma(dsem, out, in_, reads=rec[3], writes=rec[4], **rec[5])
                eng_free[en] = t0 + 0.05
                done = t0 + 2.5
            else:
                rec[2]()
                done = t0
            for b in rec[4]:
                buf_ready[id(b)] = done
                a[3][id(b)] -= 1
            for b in rec[3]:
                a[2][id(b)] -= 1
            a[1] += 1
            if a[1] == len(a[0]):
                active.pop(ai)


class T:
    def __init__(self, t, name):
        self.t = t
        self.b = Buf(name)


def build_program():
    nc = bass.Bass("TRN2", target_bir_lowering=False)

    def din(name, shape, dt=F32):
        return nc.dram_tensor(name, list(shape), dt, kind="ExternalInput").ap()

    def dout(name, shape, dt=F32):
        return nc.dram_tensor(name, list(shape), dt, kind="ExternalOutput").ap()

    def dscr(name, shape, dt):
        return nc.dram_tensor(name, list(shape), dt, kind="Internal").ap()

    xp = din("xp", [NBLK * 128, D])
    xs = din("xs", [64, D])
    cT = din("cT", [D, 65])
    w_mod = din("w_mod", [D, 6 * D])
    b_mod = din("b_mod", [1, 6 * D])
    attn_norm = din("attn_norm", [1, D])
    ffn_norm = din("ffn_norm", [1, D])
    final_norm = din("final_norm", [1, D])
    w_in = din("w_in", [D, 2304])
    w_out = din("w_out", [D, D])
    w_r = din("w_r", [D, 36])
    b_r = din("b_r", [1, 36])
    convwT = din("convwT", [128, 12])
    sinks = din("sinks", [1, 8])
    gn_attn = din("gn_attn", [1, 512])
    gn_convT = din("gn_convT", [128, 4])
    wall = din("wall", [NE * 128, WROW])
    kcT_d = din("kcT", [128, 2, NSEQ_S * 128])
    vc_d = din("vc", [128, NSEQ_S, 128])
    ck_nat = din("ck_nat", [NSEQ_S, 128, 128])
    cv_nat = din("cv_nat", [NSEQ_S, 128, 128])
    stT = din("stT", [128, 4, NSEQ_S, 2])
    ident_d = din("ident", [128, 128], BF16)
    triu_d = din("triu", [128, 128])
    ones_d = din("ones", [128, 128])
    g64_d = din("g64", [128, 128])
    maskp_d = din("maskp", [128, 256], BF16)
    mask0_d = din("mask0", [128, 256], BF16)
    masks_d = din("masks", [128, 2112], BF16)
    cosp_d = din("cosp", [128, NBLK, 32])
    sinp_d = din("sinp", [128, NBLK, 32])
    coss_d = din("coss", [64, 32])
    sins_d = din("sins", [64, 32])
    flag_d = din("flag", [128, 1])
    tokid_d = din("tokid", [128, NTT], I32)
    piota_d = din("piota", [128, 1])
    thr_d = din("thr", [1, 128])
    twinit_d = din("twinit", [128, 4], I32)
    y_p = dout("y_p", [NPB * 128, D])
    y_s = dout("y_s", [64, D])
    nk_p = dout("nk_p", [128, 128])
    nv_p = dout("nv_p", [128, 128])
    ncv_p = dout("ncv_p", [2, 512])
    nk_s = dout("nk_s", [NSEQ_S, 128, 128])
    nv_s = dout("nv_s", [NSEQ_S, 128, 128])
    ncv_s = dout("ncv_s", [NSEQ_S, 2, 512])
    H2 = dscr("H2", [HROWS, D], BF16)
    X1 = dscr("X1", [HROWS, D], F32)
    Y = dscr("Y", [2 * HROWS, D], F32)
    TW = dscr("TW", [NB * 128, 4], I32)
    MODV = dscr("MODV", [6, 65, D], F32)

    dr = {k: Buf(k) for k in ("H2", "X1", "Y", "TW", "MODV", "out")}

    with ExitStack() as st0:
        S = Sched(nc, st0, 64)
        pe, act, dve, pool, sp = (S.E[k] for k in ("pe", "act", "dve", "pool", "sp"))

        def mk(stack, name, shape, dt):
            return T(stack.enter_context(nc.sbuf_tensor("sb_" + name, list(shape), dt)), name)

        def breg(name, val):
            r = nc.gpsimd.alloc_register(name)
            nc.gpsimd.reg_mov(r, val)
            return r

        bc_tw = breg("bc_tw", NB * 128 - 1)
        bc_w = breg("bc_w", NE * 128 - 1)
        bc_y = breg("bc_y", 2 * HROWS - 1)
        ps = st0.enter_context(nc.psum_tensor("ps", [128, 4096], F32))
        PB = [Buf("pb%d" % i, excl=True) for i in range(8)]

        def bank(i, n=1):
            return ps[:, i * 512:(i + n) * 512]

        def bank_bf(i, n=1):
            return ps[:, i * 512:(i + n) * 512].bitcast(BF16)

        ident = mk(st0, "ident", [128, 128], BF16)
        onesf = mk(st0, "onesf", [128, 128], F32)
        triu = mk(st0, "triu", [128, 128], F32)
        dC = S.dsem()
        sp.dma(dC, ident.t[:], ident_d, writes=[ident.b], group=True)
        sp.dma(dC, onesf.t[:], ones_d, writes=[onesf.b], group=True)
        sp.dma(dC, triu.t[:], triu_d, writes=[triu.b], group=True)
        dOut = S.dsem()
        dNK, dNV = S.dsem(), S.dsem()
        qk_f0 = mk(st0, "qk_f", [128, 640], F32)
        v_f0 = mk(st0, "v_f", [128, 128], F32)
        qk_fL = [qk_f0, qk_f0]
        v_fL = [v_f0, v_f0]
        uext = [mk(st0, "uext%d" % i, [128, 4, 130], F32) for i in range(2)]
        uexts = mk(st0, "uexts", [128, 4, NSEQ_S * 6], F32)
        for t_ in (ident, onesf, triu):
            t_.b.w = (dC, S.dcount[dC])
        twi = mk(st0, "twi", [128, 4], I32)
        dI = S.dsem()
        act.dma(dI, twi.t[:], twinit_d, writes=[twi.b])
        for b_ in range(NB):
            act.dma(dI, TW[b_ * 128:(b_ + 1) * 128, :], twi.t[:], reads=[twi.b], writes=[dr["TW"]], group=True)
        dr["TW"].w = (dI, S.dcount[dI])
        gate_all = mk(st0, "gate_all", [128, NTT, NE], F32)
        rs_all = mk(st0, "rs_all", [128, NTT, NE], F32)
        sel_all = mk(st0, "sel_all", [128, NTT, NE], F32)
        cumsel = mk(st0, "cumsel", [128, NE], F32)
        pool.op(lambda e: e.memset(gate_all.t[:], 0.0), writes=[gate_all.b])
        pool.op(lambda e: e.memset(rs_all.t[:], 0.0), writes=[rs_all.b])
        pool.op(lambda e: e.memset(sel_all.t[:], 0.0), writes=[sel_all.b])
        pool.op(lambda e: e.memset(cumsel.t[:], 0.0), writes=[cumsel.b])
        sW = ExitStack()
        w_in_bf = mk(sW, "w_in_bf", [128, 8, 2304], BF16)
        w_out_bf = mk(sW, "w_out_bf", [128, 8, D], BF16)
        w_r_bf = mk(sW, "w_r_bf", [128, 8, 36], BF16)
        kcT = mk(sW, "kcT", [128, 2, NSEQ_S * 128], BF16)
        vc = mk(sW, "vc", [128, NSEQ_S, 128], BF16)
        w_in_parts = [Buf("w_in_c%d" % c) for c in range(8)]
        for c in range(8):
            pool.dma(S.dsem(), w_in_bf.t[:, c, :], w_in[c * 128:(c + 1) * 128, :], writes=[w_in_parts[c]])
        pool.dma(S.dsem(), w_out_bf.t[:], w_out.rearrange("(c p) n -> p c n", p=128), writes=[w_out_bf.b])
        pool.dma(S.dsem(), w_r_bf.t[:], w_r.rearrange("(c p) n -> p c n", p=128), writes=[w_r_bf.b])
        pool.dma(S.dsem(), kcT.t[:], kcT_d, writes=[kcT.b])
        pool.dma(S.dsem(), vc.t[:], vc_d, writes=[vc.b])

        with ExitStack() as s0:
            cTt = mk(s0, "cTt", [128, 8, 65], F32)
            silu = mk(s0, "silu", [128, 8, 65], F32)
            wmb = [mk(s0, "wmb%d" % i, [128, 8, 512], F32) for i in range(2)]
            mod17 = mk(s0, "mod17", [65, 6 * D], F32)
            bm17 = mk(s0, "bm17", [65, 6 * D], F32)
            an17 = mk(s0, "an17", [65, D], F32)
            fn17 = mk(s0, "fn17", [65, D], F32)
            a17 = mk(s0, "a17", [65, 2 * D], F32)
            d0 = S.dsem()
            sp.dma(d0, cTt.t[:], cT.rearrange("(c p) r -> p c r", p=128), writes=[cTt.b], group=True)
            sp.dma(d0, bm17.t[:], b_mod.partition_broadcast(65), writes=[bm17.b], group=True)
            sp.dma(d0, an17.t[:], attn_norm.partition_broadcast(65), writes=[an17.b], group=True)
            sp.dma(d0, fn17.t[:], ffn_norm.partition_broadcast(65), writes=[fn17.b], group=True)
            for t_ in (cTt, bm17, an17, fn17):
                t_.b.w = (d0, S.dcount[d0])
            act.op(lambda e: e.activation(out=silu.t[:], in_=cTt.t[:], func=AF.Silu), reads=[cTt.b], writes=[silu.b])
            dW = [S.dsem(), S.dsem()]
            for j in range(12):
                wb = wmb[j % 2]
                sp.dma(dW[j % 2], wb.t[:], w_mod[:, j * 512:(j + 1) * 512].rearrange("(c p) n -> p c n", p=128),
                       writes=[wb.b])
                pb = PB[j % 2]
                for c in range(8):
                    pe.op(lambda e, c=c, wb=wb, j=j: e.matmul(bank(j % 2)[0:65, :], lhsT=silu.t[:, c, :], rhs=wb.t[:, c, :],
                                                              start=(c == 0), stop=(c == 7)),
                          reads=[silu.b, wb.b], writes=[pb])
                dve.op(lambda e, j=j: e.tensor_tensor(out=mod17.t[:, j * 512:(j + 1) * 512], in0=bank(j % 2)[0:65, :],
                                                      in1=bm17.t[:, j * 512:(j + 1) * 512], op=ALU.add),
                       reads=[pb, bm17.b], writes=[mod17.b])
            dve.op(lambda e: e.scalar_tensor_tensor(out=a17.t[:, 0:D], in0=mod17.t[:, D:2 * D], scalar=1.0, in1=an17.t[:],
                                                    op0=ALU.add, op1=ALU.mult), reads=[mod17.b, an17.b], writes=[a17.b])
            dve.op(lambda e: e.scalar_tensor_tensor(out=a17.t[:, D:2 * D], in0=mod17.t[:, 4 * D:5 * D], scalar=1.0, in1=fn17.t[:],
                                                    op0=ALU.add, op1=ALU.mult), reads=[mod17.b, fn17.b], writes=[a17.b])
            dM = S.dsem()
            srcs = [a17.t[:, 0:D], mod17.t[:, 0:D], mod17.t[:, 2 * D:3 * D], a17.t[:, D:2 * D],
                    mod17.t[:, 3 * D:4 * D], mod17.t[:, 5 * D:6 * D]]
            for k in range(6):
                sp.dma(dM, MODV[k], srcs[k], reads=[a17.b, mod17.b], writes=[dr["MODV"]], group=True)
            dr["MODV"].w = (dM, S.dcount[dM])
            for e_ in S.E.values():
                e_.wait_ev(dr["MODV"].w)
            barrier_all(S)

        with ExitStack() as s1:
            BC = [mk(s1, "bc%d" % k, [128, D], F32) for k in range(6)]
            g64 = mk(s1, "g64", [128, 128], F32)
            maskp = mk(s1, "maskp", [128, 256], BF16)
            mask0 = mk(s1, "mask0", [128, 256], BF16)
            masks = mk(s1, "masks", [128, 2112], BF16)
            cosp = mk(s1, "cosp", [128, NBLK, 32], F32)
            sinp = mk(s1, "sinp", [128, NBLK, 32], F32)
            coss = mk(s1, "coss", [64, 32], F32)
            sins = mk(s1, "sins", [64, 32], F32)
            flag = mk(s1, "flag", [128, 1], F32)
            convw = mk(s1, "convw", [128, 12], F32)
            gncv = mk(s1, "gncv", [128, 4], F32)
            gnat = mk(s1, "gnat", [128, 512], F32)
            nsink = mk(s1, "nsink", [128, 8], F32)
            psink = mk(s1, "psink", [128, 8], F32)
            brt = mk(s1, "brt", [128, 36], F32)
            dK = S.dsem()
            cl = [(g64, g64_d), (maskp, maskp_d), (mask0, mask0_d), (masks, masks_d), (cosp, cosp_d), (sinp, sinp_d),
                  (coss, coss_d), (sins, sins_d), (flag, flag_d), (convw, convwT), (gncv, gn_convT),
                  (gnat, gn_attn.partition_broadcast(128)), (psink, sinks.partition_broadcast(128)),
                  (brt, b_r.partition_broadcast(128))]
            for t_, src in cl:
                sp.dma(dK, t_.t[:], src, writes=[t_.b], group=True)
            for k in range(6):
                sp.dma(dK, BC[k].t[:], MODV[k, 0:1, :].partition_broadcast(128), reads=[dr["MODV"]], writes=[BC[k].b], group=True)
            for t_, _ in cl:
                t_.b.w = (dK, S.dcount[dK])
            for k in range(6):
                BC[k].b.w = (dK, S.dcount[dK])
            dve.op(lambda e: e.tensor_scalar(out=nsink.t[:], in0=psink.t[:], scalar1=-1.0, scalar2=None, op0=ALU.mult),
                   reads=[psink.b], writes=[nsink.b])

            def mk2(name, shape, dt):
                return [mk(s1, "%s_%d" % (name, i), shape, dt) for i in range(2)]

            def mk1(name, shape, dt):
                t_ = mk(s1, name, shape, dt)
                return [t_, t_]

            def mk3(name, shape, dt):
                return [mk(s1, "%s_%d" % (name, i), shape, dt) for i in range(3)]

            Xt = mk3("xt", [128, D], F32)
            tmpL = mk3("tmp", [128, D], F32)
            hTL = mk3("hT", [128, 8 * 128], BF16)
            h2TL = hTL
            stt = mk3("stt", [128, 64], F32)
            caccL = mk3("cacc", [128, 4, 128], F32)
            csqL = mk3("csq", [128, 4, 128], F32)
            crsL = csqL
            kT = [mk(s1, "kT_%d" % i, [128, 2, 128], BF16) for i in range(3)]
            vb = [mk(s1, "vb_%d" % i, [128, 128], BF16) for i in range(3)]
            junkL = mk1("junk", [128, D], BF16)
            junk2 = mk(s1, "junk2", [128, D], BF16)
            h_bfL = mk1("h_bf", [128, D], BF16)
            qkvL = mk1("qkv_sb", [128, 768], F32)
            cvL = mk1("cv_sb", [128, 12, 128], F32)
            qkdL = mk1("qkd", [128, 768], BF16)
            qTL = mk2("qT", [128, 4, 128], BF16)
            r1L = mk1("r1", [128, 10, 32], F32)
            r2L = mk1("r2", [128, 10, 32], F32)
            P_bfL = mk1("P_bf", [128, 2176], BF16)
            PT_sbL = mk1("PT_sb", [128, 1088], BF16)
            O_sbL = mk1("O_sb", [128, 512], F32)
            On_bfL = mk1("On_bf", [128, 512], BF16)
            mixTL = mk1("mixT", [128, 8, 128], BF16)
            h2_bfL = mk1("h2_bf", [128, D], BF16)
            lgL = mk1("lg", [128, 36], F32)
            rtL = mk1("rt", [128, 160], F32)
            h2_bf = h2_bfL[0]

            dX = [S.dsem(), S.dsem(), S.dsem()]
            dOX = [S.dsem(), S.dsem()]
            dOH = [S.dsem(), S.dsem()]
            dS = S.dsem()
            for c in range(4):
                sp.dma(dS, uexts.t[:, c, :].rearrange("p (s l) -> p s l", s=NSEQ_S)[:, :, 0:2], stT[:, c, :, :], writes=[uexts.b],
                       allow_slow_non_contiguous=True, group=True)
            pool.op(lambda e: e.memset(h2_bf.t[:], 0.0), writes=[h2_bf.b])
            sp.dma(dS, H2[NTOK:NTOK + 64, :], h2_bf.t[0:64, :], reads=[h2_bf.b], writes=[dr["H2"]], group=True)
            sp.wait_ev((dS, S.dcount[dS]))
            uexts.b.w = (dS, S.dcount[dS])
            h2_bf.b.r = [(dS, S.dcount[dS])]

            def block(bi, kind):
                NQ = 64 if kind == "sample" else 128
                par = bi % 2
                r3, r3p = bi % 3, (bi - 1) % 3
                xt = Xt[r3]
                sq = stt[r3]
                tmp, junk, h_bf, hT, h2T = tmpL[r3], junkL[par], h_bfL[par], hTL[r3], h2TL[r3]
                qkv_sb, cv_sb, qkd, qT, r1, r2 = qkvL[par], cvL[par], qkdL[par], qTL[par], r1L[par], r2L[par]
                P_bf, PT_sb, O_sb, On_bf, mixT = P_bfL[par], PT_sbL[par], O_sbL[par], On_bfL[par], mixTL[par]
                cacc, csq, crs, x1, h2_bf, lg, rt = caccL[r3], csqL[r3], crsL[r3], Xt[r3], h2_bfL[par], lgL[par], rtL[par]
                qk_f, v_f = qk_fL[par], v_fL[par]
                if kind == "sample":
                    src = xs
                    cos_ap, sin_ap = coss.t[0:64, :], sins.t[0:64, :]
                    bk = dict(T=0, S=3, PT=1, O=0, T2=0, GS=3, MIX=4, H2T=0, LG=6)
                    G, NK, nchunk = 1, 2112, 17
                    for k in (0, 1):
                        sp.dma(dK, BC[k].t[0:64, :], MODV[k, 1:65, :], reads=[dr["MODV"]], writes=[BC[k].b], group=True)
                    def _fix01():
                        for k in (0, 1):
                            BC[k].b.w = (dK, S.dcount[dK])
                    S.defer(_fix01, writes=[BC[0].b, BC[1].b])
                else:
                    src = xp[bi * 128:(bi + 1) * 128, :]
                    cos_ap, sin_ap = cosp.t[:, bi, :], sinp.t[:, bi, :]
                    bk = dict(T=0, S=3, PT=4, O=5, T2=6, GS=7, MIX=6, H2T=6, LG=7)
                    G, NK, nchunk = 2, 256, 2
                rows = slice(0, NQ)
                sp.dma(dX[r3], xt.t[rows, :], src, writes=[xt.b])
                dve.op(lambda e: e.memset(sq.t[:], 0.0), writes=[sq.b])
                act.op(lambda e: e.activation(out=junk.t[rows, :], in_=xt.t[rows, :], func=AF.Square, accum_out=sq.t[rows, 0:1]),
                       reads=[xt.b], writes=[junk.b, sq.b])
                act.op(lambda e: e.activation(out=sq.t[rows, 1:2], in_=sq.t[rows, 0:1], func=AF.Ln, scale=1.0 / D, bias=EPS),
                       reads=[sq.b], writes=[sq.b])
                act.op(lambda e: e.activation(out=sq.t[rows, 1:2], in_=sq.t[rows, 1:2], func=AF.Exp, scale=-0.5), reads=[sq.b], writes=[sq.b])
                dve.op(lambda e: e.scalar_tensor_tensor(out=tmp.t[rows, :], in0=xt.t[rows, :], scalar=sq.t[rows, 1:2], in1=BC[0].t[rows, :],
                                                        op0=ALU.mult, op1=ALU.mult), reads=[xt.b, sq.b, BC[0].b], writes=[tmp.b])
                pool.op(lambda e: e.tensor_tensor(out=h_bf.t[rows, :], in0=tmp.t[rows, :], in1=BC[1].t[rows, :], op=ALU.add),
                        reads=[tmp.b, BC[1].b], writes=[h_bf.b])
                yield
                tb = bank_bf(bk["T"])
                for c in range(8):
                    pe.op(lambda e, c=c: e.transpose(out=tb[:, c * NQ:(c + 1) * NQ], in_=h_bf.t[rows, c * 128:(c + 1) * 128],
                                                     identity=ident.t[rows, rows]), reads=[h_bf.b, ident.b], writes=[PB[bk["T"]]])
                act.op(lambda e: e.copy(out=hT.t[:, 0:8 * NQ], in_=tb[:, 0:8 * NQ]), reads=[PB[bk["T"]]], writes=[hT.b])
                for lo, hi, iob in ((0, 512, 1), (512, 768, 2)):
                    for c in range(8):
                        pe.op(lambda e, c=c, lo=lo, hi=hi, iob=iob: e.matmul(ps[rows, iob * 512: iob * 512 + (hi - lo)], lhsT=hT.t[:, c * NQ:(c + 1) * NQ],
                                                                             rhs=w_in_bf.t[:, c, lo:hi], start=(c == 0), stop=(c == 7)),
                              reads=[hT.b, w_in_parts[c]], writes=[PB[iob]])
                    if iob == 1:
                        act.op(lambda e, lo=lo, hi=hi, iob=iob: e.copy(out=qkv_sb.t[rows, lo:hi], in_=ps[rows, iob * 512: iob * 512 + (hi - lo)]),
                               reads=[PB[iob]], writes=[qkv_sb.b])
                    else:
                        dve.op(lambda e, lo=lo, hi=hi, iob=iob: e.tensor_copy(out=qkv_sb.t[rows, lo:hi], in_=ps[rows, iob * 512: iob * 512 + (hi - lo)]),
                               reads=[PB[iob]], writes=[qkv_sb.b])
                for i_ in range(3):
                    iob = 1 + (i_ % 2)
                    for j in range(4 * i_, 4 * i_ + 4):
                        for c in range(8):
                            pe.op(lambda e, c=c, j=j, iob=iob: e.matmul(ps[:, iob * 512 + (j % 4) * 128: iob * 512 + (j % 4) * 128 + NQ],
                                                                        lhsT=w_in_bf.t[:, c, 768 + j * 128: 768 + (j + 1) * 128],
                                                                        rhs=hT.t[:, c * NQ:(c + 1) * NQ], start=(c == 0), stop=(c == 7)),
                                  reads=[hT.b, w_in_parts[c]], writes=[PB[iob]])
                    if i_ != 1:
                        act.op(lambda e, i_=i_, iob=iob: e.copy(out=cv_sb.t[:, 4 * i_:4 * i_ + 4, 0:NQ],
                                                                in_=bank(iob).rearrange("p (c n) -> p c n", c=4)[:, :, 0:NQ]),
                               reads=[PB[iob]], writes=[cv_sb.b])
                    else:
                        dve.op(lambda e, i_=i_, iob=iob: e.tensor_copy(out=cv_sb.t[:, 4 * i_:4 * i_ + 4, 0:NQ],
                                                                       in_=bank(iob).rearrange("p (c n) -> p c n", c=4)[:, :, 0:NQ]),
                               reads=[PB[iob]], writes=[cv_sb.b])
                yield
                qkvs = qkv_sb.t[rows, :]
                xv = qkvs[:, 0:640].rearrange("p (h t d) -> p h t d", h=10, t=2)
                ov = qk_f.t[rows, :].rearrange("p (h t d) -> p h t d", h=10, t=2)
                cb_ = cos_ap.unsqueeze(1).to_broadcast([NQ, 10, 32])
                sb_ = sin_ap.unsqueeze(1).to_broadcast([NQ, 10, 32])
                rq = [qkv_sb.b]
                dve.op(lambda e: e.tensor_tensor(out=r1.t[rows], in0=xv[:, :, 0, :], in1=cb_, op=ALU.mult), reads=rq + [cosp.b, coss.b], writes=[r1.b])
                dve.op(lambda e: e.tensor_tensor(out=r2.t[rows], in0=xv[:, :, 1, :], in1=sb_, op=ALU.mult), reads=rq + [sinp.b, sins.b], writes=[r2.b])
                pool.op(lambda e: e.tensor_tensor(out=ov[:, :, 0, :], in0=r1.t[rows], in1=r2.t[rows], op=ALU.subtract),
                        reads=[r1.b, r2.b], writes=[qk_f.b])
                dve.op(lambda e: e.tensor_tensor(out=r1.t[rows], in0=xv[:, :, 1, :], in1=cb_, op=ALU.mult), reads=rq, writes=[r1.b])
                dve.op(lambda e: e.tensor_tensor(out=r2.t[rows], in0=xv[:, :, 0, :], in1=sb_, op=ALU.mult), reads=rq, writes=[r2.b])
                pool.op(lambda e: e.tensor_tensor(out=ov[:, :, 1, :], in0=r1.t[rows], in1=r2.t[rows], op=ALU.add),
                        reads=[r1.b, r2.b], writes=[qk_f.b])
                act.op(lambda e: e.copy(out=qkd.t[rows, 0:512], in_=qk_f.t[rows, 0:512]), reads=[qk_f.b], writes=[qkd.b])
                kdup = qkd.t[rows, 512:768].rearrange("p (g t d) -> p g t d", g=2, t=2)
                ksrc = qk_f.t[rows, 512:640].rearrange("p (g d) -> p g d", g=2)
                pool.op(lambda e: e.tensor_copy(out=kdup[:, :, 0, :], in_=ksrc), reads=[qk_f.b], writes=[qkd.b])
                pool.op(lambda e: e.tensor_copy(out=kdup[:, :, 1, :], in_=ksrc), reads=[qk_f.b], writes=[qkd.b])
                pool.op(lambda e: e.tensor_copy(out=vb[r3].t[rows, :], in_=qkvs[:, 640:768]), reads=[qkv_sb.b], writes=[vb[r3].b])
                last_p = (kind == "prompt" and bi == NPB)
                if last_p or kind == "sample":
                    pool.op(lambda e: e.tensor_copy(out=v_f.t[rows, :], in_=qkvs[:, 640:768]), reads=[qkv_sb.b], writes=[v_f.b])
                NS, L = (NSEQ_S, 4) if kind == "sample" else (1, 128)
                ue = uexts if kind == "sample" else uext[par]
                W_ = NS * (L + 2)

                def cps(j):
                    return cv_sb.t[:, j, 0:NQ]

                for c in range(4):
                    uv = ue.t[:, c, 0:W_].rearrange("p (s l) -> p s l", s=NS)
                    pool.op(lambda e, c=c, uv=uv: e.tensor_tensor(out=uv[:, :, 2:2 + L], in0=cps(8 + c).rearrange("p (s l) -> p s l", s=NS),
                                                                  in1=cps(c).rearrange("p (s l) -> p s l", s=NS), op=ALU.mult),
                            reads=[cv_sb.b], writes=[ue.b])
                if kind != "sample":
                    prev = uext[1 - par]
                    if bi == 1:
                        pool.op(lambda e: e.tensor_scalar(out=ue.t[:, :, 0:2], in0=prev.t[:, :, 128:130], scalar1=flag.t[:, 0:1], scalar2=None,
                                                          op0=ALU.mult), reads=[prev.b, flag.b], writes=[ue.b])
                    elif bi > 1:
                        pool.op(lambda e: e.tensor_copy(out=ue.t[:, :, 0:2], in_=prev.t[:, :, 128:130]), reads=[prev.b], writes=[ue.b])
                if kind == "halo":
                    tbk = bank_bf(bk["T"])
                    for j in range(2):
                        pe.op(lambda e, j=j: e.transpose(out=tbk[:, j * 128:(j + 1) * 128], in_=qkd.t[:, 512 + j * 128: 640 + j * 128],
                                                         identity=ident.t[:, :]), reads=[qkd.b, ident.b], writes=[PB[bk["T"]]])
                    act.op(lambda e: e.copy(out=kT[r3].t[:].rearrange("p g n -> p (g n)"), in_=tbk[:, 0:256]),
                           reads=[PB[bk["T"]]], writes=[kT[r3].b])
                    return
                for c in range(4):
                    uv = ue.t[:, c, 0:W_].rearrange("p (s l) -> p s l", s=NS)
                    av = cacc.t[:, c, 0:NQ].rearrange("p (s l) -> p s l", s=NS)
                    dve.op(lambda e, c=c, uv=uv, av=av: e.tensor_scalar(out=av, in0=uv[:, :, 0:L], scalar1=convw.t[:, c * 3:c * 3 + 1], scalar2=None,
                                                                      op0=ALU.mult), reads=[ue.b, convw.b], writes=[cacc.b])
                    for k in (1, 2):
                        dve.op(lambda e, c=c, uv=uv, av=av, k=k: e.scalar_tensor_tensor(out=av, in0=uv[:, :, k:k + L],
                                                                                       scalar=convw.t[:, c * 3 + k:c * 3 + k + 1], in1=av,
                                                                                       op0=ALU.mult, op1=ALU.add),
                               reads=[ue.b, convw.b, cacc.b], writes=[cacc.b])
                    pool.op(lambda e, c=c: e.tensor_tensor(out=cacc.t[:, c, 0:NQ], in0=cps(4 + c), in1=cacc.t[:, c, 0:NQ], op=ALU.mult),
                            reads=[cv_sb.b, cacc.b], writes=[cacc.b])
                act.op(lambda e: e.activation(out=csq.t[:, :, 0:NQ], in_=cacc.t[:, :, 0:NQ], func=AF.Square), reads=[cacc.b], writes=[csq.b])
                tbk = bank_bf(bk["T"])
                for j in range(6):
                    pe.op(lambda e, j=j: e.transpose(out=tbk[:, j * NQ:(j + 1) * NQ], in_=qkd.t[rows, j * 128:(j + 1) * 128],
                                                     identity=ident.t[rows, rows]), reads=[qkd.b, ident.b], writes=[PB[bk["T"]]])
                act.op(lambda e: e.copy(out=qT.t[:, :, 0:NQ], in_=tbk[:, 0:4 * NQ].rearrange("p (j n) -> p j n", j=4)),
                       reads=[PB[bk["T"]]], writes=[qT.b])
                dve.op(lambda e: e.tensor_copy(out=kT[r3].t[:, :, 0:NQ], in_=tbk[:, 4 * NQ:6 * NQ].rearrange("p (j n) -> p j n", j=2)),
                       reads=[PB[bk["T"]]], writes=[kT[r3].b])
                yield
                if kind == "sample":
                    def kchunk(kc, g, half):
                        if kc < 16:
                            return kcT.t[half * 64:(half + 1) * 64, g, kc * 128:(kc + 1) * 128], 128, kcT.b
                        return kT[r3].t[half * 64:(half + 1) * 64, g, 0:64], 64, kT[r3].b

                    def vchunk(kc, g):
                        if kc < 16:
                            return vc.t[:, kc, g * 64:(g + 1) * 64], 128, vc.b
                        return vb[r3].t[0:64, g * 64:(g + 1) * 64], 64, vb[r3].b
                    mask_t = masks
                else:
                    def kchunk(kc, g, half):
                        t_ = kT[r3p] if kc == 0 else kT[r3]
                        return t_.t[half * 64:(half + 1) * 64, g, :], 128, t_.b

                    def vchunk(kc, g):
                        t_ = vb[r3p] if kc == 0 else vb[r3]
                        return t_.t[:, g * 64:(g + 1) * 64], 128, t_.b
                    mask_t = mask0 if bi == 1 else maskp
                sbk = bk["S"]
                nsb = (G * NK + 511) // 512
                SB = [PB[sbk + i] for i in range(nsb)]
                ob = bk["O"]
                ptb = bk["PT"]
                npt = 2 if kind == "sample" else 1
                PTB = [PB[ptb + i_] for i_ in range(npt)]
                ptv = bank_bf(ptb, npt)
                for hg in range(8 // G):
                    for gi in range(G):
                        h = hg * G + gi
                        half, g = h % 2, h // 4
                        base = sbk * 512 + gi * NK
                        for kc in range(nchunk):
                            kap, kn, kbuf = kchunk(kc, g, half)
                            pe.op(lambda e, kc=kc, kap=kap, kn=kn, base=base, half=half, h=h: e.matmul(
                                ps[rows, base + kc * 128: base + kc * 128 + kn], lhsT=qT.t[half * 64:(half + 1) * 64, h // 2, 0:NQ], rhs=kap,
                                start=True, stop=False), reads=[qT.b, kbuf], writes=SB)
                            mrow = slice(half * 64, half * 64 + 64) if NQ == 64 else rows
                            pe.op(lambda e, kc=kc, kn=kn, base=base, mrow=mrow: e.matmul(
                                ps[rows, base + kc * 128: base + kc * 128 + kn], lhsT=ident.t[mrow, mrow],
                                rhs=mask_t.t[mrow, kc * 128: kc * 128 + kn], start=False, stop=True),
                                reads=[ident.b, mask_t.b], writes=SB)
                    sv = ps[rows, sbk * 512: sbk * 512 + G * NK].rearrange("p (g n) -> p g n", g=G)
                    h0 = hg * G
                    dve.op(lambda e, h0=h0: e.tensor_reduce(out=sq.t[rows, 8 + h0: 8 + h0 + G], in_=sv, axis=AX.X, op=ALU.max),
                           reads=SB, writes=[sq.b])
                    dve.op(lambda e, h0=h0: e.scalar_tensor_tensor(out=sq.t[rows, 16 + h0:16 + h0 + G], in0=sq.t[rows, 8 + h0: 8 + h0 + G], scalar=-0.125,
                                                                   in1=nsink.t[rows, h0:h0 + G], op0=ALU.mult, op1=ALU.min),
                           reads=[sq.b, nsink.b], writes=[sq.b])
                    for gi in range(G):
                        h = h0 + gi
                        act.op(lambda e, gi=gi, h=h: e.activation(out=P_bf.t[rows, gi * NK:(gi + 1) * NK], in_=sv[:, gi, :], func=AF.Exp,
                                                                  bias=sq.t[rows, 16 + h:17 + h], scale=0.125, accum_out=sq.t[rows, 24 + h:25 + h]),
                               reads=SB + [sq.b], writes=[P_bf.b, sq.b])
                    slot = 0
                    for gi in range(G):
                        for kc in range(nchunk):
                            kn = 64 if (kind == "sample" and kc == 16) else 128
                            pe.op(lambda e, gi=gi, kc=kc, kn=kn, slot=slot: e.transpose(
                                out=ptv[0:kn, slot * NQ:(slot + 1) * NQ], in_=P_bf.t[rows, gi * NK + kc * 128: gi * NK + kc * 128 + kn],
                                identity=ident.t[rows, rows]), reads=[P_bf.b, ident.b], writes=PTB)
                            slot += 1
                    nsl = slot
                    hs = (nsl // 2) * NQ
                    if kind == "sample":
                        if hg % 2 == 0:
                            act.op(lambda e: e.copy(out=PT_sb.t[:, 0:16 * NQ], in_=ptv[:, 0:16 * NQ]), reads=PTB, writes=[PT_sb.b])
                            act.op(lambda e: e.copy(out=PT_sb.t[0:64, 16 * NQ:17 * NQ], in_=ptv[0:64, 16 * NQ:17 * NQ]), reads=PTB, writes=[PT_sb.b])
                        else:
                            dve.op(lambda e: e.tensor_copy(out=PT_sb.t[:, 0:16 * NQ], in_=ptv[:, 0:16 * NQ]), reads=PTB, writes=[PT_sb.b])
                            dve.op(lambda e: e.tensor_copy(out=PT_sb.t[0:64, 16 * NQ:17 * NQ], in_=ptv[0:64, 16 * NQ:17 * NQ]), reads=PTB, writes=[PT_sb.b])
                    elif hg % 2 == 0:
                        act.op(lambda e: e.copy(out=PT_sb.t[:, 0:nsl * NQ], in_=ptv[:, 0:nsl * NQ]), reads=PTB, writes=[PT_sb.b])
                    else:
                        dve.op(lambda e: e.tensor_copy(out=PT_sb.t[:, 0:nsl * NQ], in_=ptv[:, 0:nsl * NQ]), reads=PTB, writes=[PT_sb.b])
                    slot = 0
                    for gi in range(G):
                        h = h0 + gi
                        g = h // 4
                        for kc in range(nchunk):
                            vap, kn, vbuf = vchunk(kc, g)
                            pe.op(lambda e, h=h, kc=kc, vap=vap, kn=kn, slot=slot: e.matmul(
                                ps[rows, ob * 512 + h * 64: ob * 512 + (h + 1) * 64], lhsT=PT_sb.t[0:kn, slot * NQ:(slot + 1) * NQ], rhs=vap,
                                start=(kc == 0), stop=(kc == nchunk - 1)), reads=[PT_sb.b, vbuf], writes=[PB[ob]])
                            slot += 1
                    yield
                dve.op(lambda e: e.tensor_tensor(out=sq.t[rows, 32:40], in0=sq.t[rows, 16:24], in1=psink.t[rows, :], op=ALU.add),
                       reads=[sq.b, psink.b], writes=[sq.b])
                act.op(lambda e: e.activation(out=sq.t[rows, 32:40], in_=sq.t[rows, 32:40], func=AF.Exp), reads=[sq.b], writes=[sq.b])
                dve.op(lambda e: e.tensor_tensor(out=sq.t[rows, 32:40], in0=sq.t[rows, 32:40], in1=sq.t[rows, 24:32], op=ALU.add),
                       reads=[sq.b], writes=[sq.b])
                dve.op(lambda e: e.reciprocal(out=sq.t[rows, 32:40], in_=sq.t[rows, 32:40]), reads=[sq.b], writes=[sq.b])
                ov3 = O_sb.t[rows, :].rearrange("p (h d) -> p h d", h=8)
                dve.op(lambda e: e.tensor_tensor(out=ov3, in0=ps[rows, ob * 512:(ob + 1) * 512].rearrange("p (h d) -> p h d", h=8),
                                                 in1=sq.t[rows, 32:40].unsqueeze(2).to_broadcast([NQ, 8, 64]), op=ALU.mult),
                       reads=[PB[ob], sq.b], writes=[O_sb.b])
                pool.op(lambda e: e.tensor_tensor(out=tmp.t[rows, 0:512], in0=O_sb.t[rows, :], in1=O_sb.t[rows, :], op=ALU.mult),
                        reads=[O_sb.b], writes=[tmp.b])
                dve.op(lambda e: e.tensor_reduce(out=sq.t[rows, 40:48], in_=tmp.t[rows, 0:512].rearrange("p (h d) -> p h d", h=8), axis=AX.X, op=ALU.add),
                       reads=[tmp.b], writes=[sq.b])
                act.op(lambda e: e.activation(out=sq.t[rows, 40:48], in_=sq.t[rows, 40:48], func=AF.Ln, scale=1.0 / 64, bias=EPS),
                       reads=[sq.b], writes=[sq.b])
                act.op(lambda e: e.activation(out=sq.t[rows, 40:48], in_=sq.t[rows, 40:48], func=AF.Exp, scale=-0.5), reads=[sq.b], writes=[sq.b])
                dve.op(lambda e: e.tensor_tensor(out=ov3, in0=ov3, in1=sq.t[rows, 40:48].unsqueeze(2).to_broadcast([NQ, 8, 64]), op=ALU.mult),
                       reads=[O_sb.b, sq.b], writes=[O_sb.b])
                pool.op(lambda e: e.tensor_tensor(out=On_bf.t[rows, :], in0=O_sb.t[rows, :], in1=gnat.t[rows, :], op=ALU.mult),
                        reads=[O_sb.b, gnat.b], writes=[On_bf.b])
                tbo = bank_bf(bk["T2"])
                for j in range(4):
                    pe.op(lambda e, j=j: e.transpose(out=tbo[:, j * NQ:(j + 1) * NQ], in_=On_bf.t[rows, j * 128:(j + 1) * 128],
                                                     identity=ident.t[rows, rows]), reads=[On_bf.b, ident.b], writes=[PB[bk["T2"]]])
                act.op(lambda e: e.copy(out=mixT.t[:, 0:4, 0:NQ], in_=tbo[:, 0:4 * NQ].rearrange("p (j n) -> p j n", j=4)),
                       reads=[PB[bk["T2"]]], writes=[mixT.b])
                gb = bk["GS"]
                for c in range(4):
                    pe.op(lambda e, c=c: e.matmul(ps[:, gb * 512 + c * 128: gb * 512 + c * 128 + NQ], lhsT=g64.t[:, :], rhs=csq.t[:, c, 0:NQ],
                                                  start=True, stop=True), reads=[g64.b, csq.b], writes=[PB[gb]])
                gsv = ps[:, gb * 512:(gb + 1) * 512].rearrange("p (c n) -> p c n", c=4)[:, :, 0:NQ]
                act.op(lambda e: e.activation(out=crs.t[:, :, 0:NQ], in_=gsv, func=AF.Ln, scale=1.0 / 64, bias=EPS), reads=[PB[gb]], writes=[crs.b])
                act.op(lambda e: e.activation(out=crs.t[:, :, 0:NQ], in_=crs.t[:, :, 0:NQ], func=AF.Exp, scale=-0.5), reads=[crs.b], writes=[crs.b])
                pool.op(lambda e: e.tensor_tensor(out=crs.t[:, :, 0:NQ], in0=crs.t[:, :, 0:NQ], in1=cacc.t[:, :, 0:NQ], op=ALU.mult),
                        reads=[crs.b, cacc.b], writes=[crs.b])
                for c in range(4):
                    dve.op(lambda e, c=c: e.tensor_scalar(out=mixT.t[:, 4 + c, 0:NQ], in0=crs.t[:, c, 0:NQ], scalar1=gncv.t[:, c:c + 1], scalar2=None,
                                                          op0=ALU.mult), reads=[crs.b, gncv.b], writes=[mixT.b])
                if kind == "sample":
                    for k in (2, 3, 4):
                        sp.dma(dK, BC[k].t[0:64, :], MODV[k, 1:65, :], reads=[dr["MODV"]], writes=[BC[k].b], group=True)
                    def _fix234():
                        for k in (2, 3, 4):
                            BC[k].b.w = (dK, S.dcount[dK])
                    S.defer(_fix234, writes=[BC[2].b, BC[3].b, BC[4].b])
                mb = bk["MIX"]
                for hf in range(2):
                    for c in range(8):
                        pe.op(lambda e, c=c, hf=hf: e.matmul(ps[rows, (mb + hf) * 512:(mb + hf + 1) * 512], lhsT=mixT.t[:, c, 0:NQ],
                                                             rhs=w_out_bf.t[:, c, hf * 512:(hf + 1) * 512], start=(c == 0), stop=(c == 7)),
                              reads=[mixT.b, w_out_bf.b], writes=[PB[mb + hf]])
                dve.op(lambda e: e.tensor_tensor(out=tmp.t[rows, :], in0=ps[rows, mb * 512:(mb + 2) * 512], in1=BC[2].t[rows, :], op=ALU.mult),
                       reads=[PB[mb], PB[mb + 1], BC[2].b], writes=[tmp.b])
                pool.op(lambda e: e.tensor_tensor(out=x1.t[rows, :], in0=tmp.t[rows, :], in1=xt.t[rows, :], op=ALU.add),
                        reads=[tmp.b, xt.b], writes=[x1.b])
                tok0 = NPB * 128 if kind == "sample" else (bi - 1) * 128
                sp.dma(dOX[par], X1[tok0:tok0 + NQ, :], x1.t[rows, :], reads=[x1.b], writes=[])
                yield
                act.op(lambda e: e.activation(out=junk2.t[rows, :], in_=x1.t[rows, :], func=AF.Square, accum_out=sq.t[rows, 2:3]),
                       reads=[x1.b], writes=[junk2.b, sq.b])
                act.op(lambda e: e.activation(out=sq.t[rows, 3:4], in_=sq.t[rows, 2:3], func=AF.Ln, scale=1.0 / D, bias=EPS),
                       reads=[sq.b], writes=[sq.b])
                act.op(lambda e: e.activation(out=sq.t[rows, 3:4], in_=sq.t[rows, 3:4], func=AF.Exp, scale=-0.5), reads=[sq.b], writes=[sq.b])
                dve.op(lambda e: e.scalar_tensor_tensor(out=tmp.t[rows, :], in0=x1.t[rows, :], scalar=sq.t[rows, 3:4], in1=BC[3].t[rows, :],
                                                        op0=ALU.mult, op1=ALU.mult), reads=[x1.b, sq.b, BC[3].b], writes=[tmp.b])
                pool.op(lambda e: e.tensor_tensor(out=h2_bf.t[rows, :], in0=tmp.t[rows, :], in1=BC[4].t[rows, :], op=ALU.add),
                        reads=[tmp.b, BC[4].b], writes=[h2_bf.b])
                sp.dma(dOH[par], H2[tok0:tok0 + NQ, :], h2_bf.t[rows, :], reads=[h2_bf.b], writes=[])
                tb2 = bank_bf(bk["H2T"])
                for c in range(8):
                    pe.op(lambda e, c=c: e.transpose(out=tb2[:, c * NQ:(c + 1) * NQ], in_=h2_bf.t[rows, c * 128:(c + 1) * 128],
                                                     identity=ident.t[rows, rows]), reads=[h2_bf.b, ident.b], writes=[PB[bk["H2T"]]])
                act.op(lambda e: e.copy(out=h2T.t[:, 0:8 * NQ], in_=tb2[:, 0:8 * NQ]), reads=[PB[bk["H2T"]]], writes=[h2T.b])
                lb = bk["LG"]
                for c in range(8):
                    pe.op(lambda e, c=c: e.matmul(ps[rows, lb * 512: lb * 512 + 36], lhsT=h2T.t[:, c * NQ:(c + 1) * NQ], rhs=w_r_bf.t[:, c, :],
                                                  start=(c == 0), stop=(c == 7)), reads=[h2T.b, w_r_bf.b], writes=[PB[lb]])
                dve.op(lambda e: e.tensor_tensor(out=lg.t[rows, :], in0=ps[rows, lb * 512: lb * 512 + 36], in1=brt.t[rows, :], op=ALU.add),
                       reads=[PB[lb], brt.b], writes=[lg.b])
                ti = NPB if kind == "sample" else bi - 1
                R = rt.t
                dve.op(lambda e: e.memset(R[rows, 0:8], 0.0), writes=[rt.b])
                dve.op(lambda e: e.tensor_reduce(out=R[rows, 0:1], in_=lg.t[rows, 0:4], axis=AX.X, op=ALU.max), reads=[lg.b], writes=[rt.b])
                dve.op(lambda e: e.tensor_scalar(out=R[rows, 1:2], in0=R[rows, 0:1], scalar1=-1.0, scalar2=None, op0=ALU.mult), reads=[rt.b], writes=[rt.b])
                act.op(lambda e: e.activation(out=R[rows, 4:8], in_=lg.t[rows, 0:4], func=AF.Exp, bias=R[rows, 1:2], scale=1.0, accum_out=R[rows, 2:3]),
                       reads=[lg.b, rt.b], writes=[rt.b])
                dve.op(lambda e: e.tensor_scalar(out=R[rows, 8:12], in0=lg.t[rows, 0:4], scalar1=R[rows, 0:1], scalar2=-1.0e30,
                                                 op0=ALU.is_lt, op1=ALU.mult), reads=[lg.b, rt.b], writes=[rt.b])
                dve.op(lambda e: e.tensor_tensor(out=R[rows, 16:48].rearrange("p (g n) -> p g n", g=4),
                                                 in0=lg.t[rows, 4:36].rearrange("p (g n) -> p g n", g=4),
                                                 in1=R[rows, 8:12].unsqueeze(2).to_broadcast([NQ, 4, 8]), op=ALU.add), reads=[lg.b, rt.b], writes=[rt.b])
                dve.op(lambda e: e.max(out=R[rows, 48:56], in_=R[rows, 16:48]), reads=[rt.b], writes=[rt.b])
                dve.op(lambda e: e.tensor_scalar(out=R[rows, 56:57], in0=R[rows, 48:49], scalar1=-1.0, scalar2=None, op0=ALU.mult), reads=[rt.b], writes=[rt.b])
                act.op(lambda e: e.activation(out=R[rows, 64:96], in_=R[rows, 16:48], func=AF.Exp, bias=R[rows, 56:57], scale=1.0),
                       reads=[rt.b], writes=[rt.b])
                selv = sel_all.t[rows, ti, :]
                dve.op(lambda e: e.tensor_scalar(out=selv, in0=R[rows, 16:48], scalar1=R[rows, 49:50], scalar2=None, op0=ALU.is_ge),
                       reads=[rt.b], writes=[sel_all.b])
                dve.op(lambda e: e.tensor_tensor(out=R[rows, 64:96], in0=R[rows, 64:96], in1=selv, op=ALU.mult), reads=[rt.b, sel_all.b], writes=[rt.b])
                dve.op(lambda e: e.tensor_reduce(out=R[rows, 57:58], in_=R[rows, 64:96], axis=AX.X, op=ALU.add), reads=[rt.b], writes=[rt.b])
                dve.op(lambda e: e.tensor_tensor(out=R[rows, 58:59], in0=R[rows, 57:58], in1=R[rows, 2:3], op=ALU.mult), reads=[rt.b], writes=[rt.b])
                dve.op(lambda e: e.reciprocal(out=R[rows, 58:59], in_=R[rows, 58:59]), reads=[rt.b], writes=[rt.b])
                dve.op(lambda e: e.tensor_scalar(out=gate_all.t[rows, ti, :], in0=R[rows, 64:96], scalar1=R[rows, 58:59], scalar2=None, op0=ALU.mult),
                       reads=[rt.b], writes=[gate_all.b])
                for i_, (l_, r_) in enumerate(((triu.t[rows, rows], selv), (onesf.t[:, rows], cumsel.t[:, :]))):
                    pe.op(lambda e, l_=l_, r_=r_, i_=i_: e.matmul(ps[rows, lb * 512 + 64: lb * 512 + 96], lhsT=l_, rhs=r_, start=(i_ == 0), stop=(i_ == 1)),
                          reads=[triu.b, onesf.b, sel_all.b, cumsel.b], writes=[PB[lb]])
                dve.op(lambda e: e.tensor_tensor(out=rs_all.t[rows, ti, :], in0=ps[rows, lb * 512 + 64: lb * 512 + 96], in1=selv, op=ALU.mult),
                       reads=[PB[lb], sel_all.b], writes=[rs_all.b])
                pool.op(lambda e: e.tensor_tensor(out=cumsel.t[rows, :], in0=cumsel.t[rows, :], in1=selv, op=ALU.add),
                        reads=[cumsel.b, sel_all.b], writes=[cumsel.b])
                if last_p:
                    act.dma(dNK, nk_p, qk_f.t[:, 512:640], reads=[qk_f.b], writes=[])
                    act.dma(dNV, nv_p, v_f.t[:, :], reads=[v_f.b], writes=[])
                    for c in range(4):
                        act.dma(dOut, ncv_p[:, c * 128:(c + 1) * 128].rearrange("j p -> p j"), ue.t[:, c, 128:130], reads=[ue.b], writes=[],
                                group=True, allow_slow_non_contiguous=True)
                if kind == "sample":
                    act.dma(dOut, nk_s[:, 0:124, :], ck_nat[:, 4:128, :], group=True)
                    act.dma(dOut, nv_s[:, 0:124, :], cv_nat[:, 4:128, :], group=True)
                    for s_ in range(NSEQ_S):
                        act.dma(dOut, nk_s[s_, 124:128, :], qk_f.t[4 * s_:4 * s_ + 4, 512:640], reads=[qk_f.b], writes=[], group=True)
                        act.dma(dOut, nv_s[s_, 124:128, :], v_f.t[4 * s_:4 * s_ + 4, :], reads=[v_f.b], writes=[], group=True)
                    for c in range(4):
                        for j_ in range(2):
                            act.dma(dOut, ncv_s[:, j_, c * 128:(c + 1) * 128].rearrange("s p -> p s"),
                                    ue.t[:, c, :].rearrange("p (s l) -> p s l", s=NSEQ_S)[:, :, 4 + j_], reads=[ue.b], writes=[],
                                    group=True, allow_slow_non_contiguous=True)

            progs = [S.record(block(0, "halo"))] + [S.record(block(bi, "prompt")) for bi in range(1, NBLK)]
            progs.append(S.record(block(NBLK, "sample")))
            S.merge_emit(progs, window=PIPE_WINDOW)
            for k_ in ("X1", "H2"):
                dr[k_].w = None
                dr[k_].r = []
            barrier_all(S, dOX + dOH + [dNK, dNV])

        sW.close()
        with ExitStack() as s2:
            cnt = mk(s2, "cnt", [128, NE], F32)
            thr = mk(s2, "thr", [128, 128], F32)
            big = mk(s2, "big", [128, NB * NE], F32)
            nblk = mk(s2, "nblk", [128, NE], F32)
            pend = [mk(s2, "pend%d" % i, [128, NE], F32) for i in range(2)]
            pstart = mk(s2, "pstart", [128, NE], F32)
            blke = mk(s2, "blke", [128, NB], F32)
            widx_f = mk(s2, "widx_f", [128, NB], F32)
            widx = mk(s2, "widx", [128, NB], I32)
            piota = mk(s2, "piota", [128, 1], F32)
            tokid = mk(s2, "tokid", [128, NTT], I32)
            dmt = mk(s2, "dmt", [128, NTT, NE], F32)
            eqt = mk(s2, "eqt", [128, NTT, NE], F32)
            dA = mk(s2, "dA", [128, NTT], F32)
            dB = mk(s2, "dB", [128, NTT], F32)
            gA = mk(s2, "gA", [128, NTT], F32)
            gB = mk(s2, "gB", [128, NTT], F32)
            dAi = mk(s2, "dAi", [128, NTT], I32)
            dBi = mk(s2, "dBi", [128, NTT], I32)
            twA = mk(s2, "twA", [128, NTT, 4], I32)
            twB = mk(s2, "twB", [128, NTT, 4], I32)
            d2 = S.dsem()
            sp.dma(d2, thr.t[:], thr_d.partition_broadcast(128), writes=[thr.b], group=True)
            sp.dma(d2, piota.t[:], piota_d, writes=[piota.b], group=True)
            sp.dma(d2, tokid.t[:], tokid_d, writes=[tokid.b], group=True)
            for t_ in (thr, piota, tokid):
                t_.b.w = (d2, S.dcount[d2])
            pe.op(lambda e: e.matmul(bank(0)[:, 0:NE], lhsT=onesf.t[:, :], rhs=cumsel.t[:, :], start=True, stop=True),
                  reads=[onesf.b, cumsel.b], writes=[PB[0]])
            dve.op(lambda e: e.tensor_copy(out=cnt.t[:], in_=bank(0)[:, 0:NE]), reads=[PB[0]], writes=[cnt.b])
            bv = big.t[:, 0:NE * 66].rearrange("p (e j) -> p e j", e=NE)
            dve.op(lambda e: e.tensor_tensor(out=bv, in0=cnt.t[:].unsqueeze(2).to_broadcast([128, NE, 66]),
                                             in1=thr.t[:, 0:66].unsqueeze(1).to_broadcast([128, NE, 66]), op=ALU.is_gt),
                   reads=[cnt.b, thr.b], writes=[big.b])
            dve.op(lambda e: e.tensor_reduce(out=nblk.t[:], in_=bv, axis=AX.X, op=ALU.add), reads=[big.b], writes=[nblk.b])
            dve.op(lambda e: e.tensor_scalar(out=nblk.t[:], in0=nblk.t[:], scalar1=128.0, scalar2=None, op0=ALU.mult), reads=[nblk.b], writes=[nblk.b])
            dve.op(lambda e: e.tensor_copy(out=pend[0].t[:], in_=nblk.t[:]), reads=[nblk.b], writes=[pend[0].b])
            cur = 0
            for sh in (1, 2, 4, 8, 16):
                a_, b_ = pend[cur], pend[1 - cur]
                dve.op(lambda e, a_=a_, b_=b_: e.tensor_copy(out=b_.t[:, 0:sh], in_=a_.t[:, 0:sh]), reads=[a_.b], writes=[b_.b])
                dve.op(lambda e, a_=a_, b_=b_, sh=sh: e.tensor_tensor(out=b_.t[:, sh:NE], in0=a_.t[:, sh:NE], in1=a_.t[:, 0:NE - sh], op=ALU.add),
                       reads=[a_.b], writes=[b_.b])
                cur = 1 - cur
            pe_ = pend[cur]
            dve.op(lambda e: e.tensor_tensor(out=pstart.t[:], in0=pe_.t[:], in1=nblk.t[:], op=ALU.subtract), reads=[pe_.b, nblk.b], writes=[pstart.b])
            bv2 = big.t[:, 0:NB * NE].rearrange("p (b e) -> p b e", b=NB)
            dve.op(lambda e: e.tensor_tensor(out=bv2, in0=pe_.t[:].unsqueeze(1).to_broadcast([128, NB, NE]),
                                             in1=thr.t[:, 0:NB].unsqueeze(2).to_broadcast([128, NB, NE]), op=ALU.is_le),
                   reads=[pe_.b, thr.b], writes=[big.b])
            dve.op(lambda e: e.tensor_reduce(out=blke.t[:], in_=bv2, axis=AX.X, op=ALU.add), reads=[big.b], writes=[blke.b])
            dve.op(lambda e: e.tensor_scalar(out=blke.t[:], in0=blke.t[:], scalar1=float(NE - 1), scalar2=None, op0=ALU.min), reads=[blke.b], writes=[blke.b])
            dve.op(lambda e: e.tensor_scalar(out=widx_f.t[:], in0=blke.t[:], scalar1=128.0, scalar2=piota.t[:, 0:1], op0=ALU.mult, op1=ALU.add),
                   reads=[blke.b, piota.b], writes=[widx_f.b])
            dve.op(lambda e: e.tensor_tensor(out=big.t[:, 0:NB - 1], in0=blke.t[:, 1:NB], in1=blke.t[:, 0:NB - 1], op=ALU.is_equal),
                   reads=[blke.b], writes=[big.b])
            dve.op(lambda e: e.scalar_tensor_tensor(out=widx_f.t[:, 1:NB], in0=big.t[:, 0:NB - 1], scalar=OOB, in1=widx_f.t[:, 1:NB],
                                                    op0=ALU.mult, op1=ALU.add), reads=[big.b, widx_f.b], writes=[widx_f.b])
            for b0 in STREAM_STARTS[1:NS_W]:
                dve.op(lambda e, b0=b0: e.tensor_scalar(out=widx_f.t[:, b0:b0 + 1], in0=blke.t[:, b0:b0 + 1], scalar1=128.0, scalar2=piota.t[:, 0:1],
                                                        op0=ALU.mult, op1=ALU.add), reads=[blke.b, piota.b, widx_f.b], writes=[widx_f.b])
            dve.op(lambda e: e.tensor_copy(out=widx.t[:], in_=widx_f.t[:]), reads=[widx_f.b], writes=[widx.b])
            dve.op(lambda e: e.tensor_tensor(out=dmt.t[:], in0=sel_all.t[:], in1=pstart.t[:].unsqueeze(1).to_broadcast([128, NTT, NE]), op=ALU.mult),
                   reads=[sel_all.b, pstart.b], writes=[dmt.b])
            dve.op(lambda e: e.tensor_tensor(out=dmt.t[:], in0=dmt.t[:], in1=rs_all.t[:], op=ALU.add), reads=[dmt.b, rs_all.b], writes=[dmt.b])
            dve.op(lambda e: e.tensor_reduce(out=dA.t[:], in_=dmt.t[:], axis=AX.X, op=ALU.max), reads=[dmt.b], writes=[dA.b])
            dve.op(lambda e: e.tensor_reduce(out=dB.t[:], in_=dmt.t[:], axis=AX.X, op=ALU.add), reads=[dmt.b], writes=[dB.b])
            dve.op(lambda e: e.tensor_tensor(out=dB.t[:], in0=dB.t[:], in1=dA.t[:], op=ALU.subtract), reads=[dB.b, dA.b], writes=[dB.b])
            dve.op(lambda e: e.tensor_tensor(out=eqt.t[:], in0=dmt.t[:], in1=dA.t[:].unsqueeze(2).to_broadcast([128, NTT, NE]), op=ALU.is_equal),
                   reads=[dmt.b, dA.b], writes=[eqt.b])
            dve.op(lambda e: e.tensor_tensor(out=eqt.t[:], in0=eqt.t[:], in1=gate_all.t[:], op=ALU.mult), reads=[eqt.b, gate_all.b], writes=[eqt.b])
            dve.op(lambda e: e.tensor_reduce(out=gA.t[:], in_=eqt.t[:], axis=AX.X, op=ALU.add), reads=[eqt.b], writes=[gA.b])
            dve.op(lambda e: e.tensor_reduce(out=gB.t[:], in_=gate_all.t[:], axis=AX.X, op=ALU.add), reads=[gate_all.b], writes=[gB.b])
            dve.op(lambda e: e.tensor_tensor(out=gB.t[:], in0=gB.t[:], in1=gA.t[:], op=ALU.subtract), reads=[gB.b, gA.b], writes=[gB.b])
            dve.op(lambda e: e.tensor_copy(out=dAi.t[:], in_=dA.t[:]), reads=[dA.b], writes=[dAi.b])
            dve.op(lambda e: e.tensor_copy(out=dBi.t[:], in_=dB.t[:]), reads=[dB.b], writes=[dBi.b])
            for tw_, g_, off in ((twA, gA, 0), (twB, gB, HROWS)):
                pool.op(lambda e, tw_=tw_: e.memset(tw_.t[:], 0), writes=[tw_.b])
                pool.op(lambda e, tw_=tw_: e.tensor_copy(out=tw_.t[:, :, 0], in_=tokid.t[:]), reads=[tokid.b], writes=[tw_.b])
                pool.op(lambda e, tw_=tw_, off=off: e.tensor_single_scalar(out=tw_.t[:, :, 1], in_=tokid.t[:], scalar=off, op=ALU.add),
                        reads=[tokid.b], writes=[tw_.b])
                pool.op(lambda e, tw_=tw_, g_=g_: e.tensor_copy(out=tw_.t[:].bitcast(F32)[:, :, 2], in_=g_.t[:]), reads=[g_.b], writes=[tw_.b])
            dScs = [S.dsem() for _ in range(6)]
            nsc = 0
            for ti in range(NTT):
                nq = 64 if ti == NPB else 128
                for tw_, di_ in ((twA, dAi), (twB, dBi)):
                    dSc = dScs[nsc % 6]
                    nsc += 1
                    pool.dma(dSc, TW[:, :], tw_.t[0:nq, ti, :], reads=[tw_.b, di_.b, dr["TW"]], writes=[],
                             indirect=dict(out_offset=bass.IndirectOffsetOnAxis(ap=di_.t[0:nq, ti:ti + 1], axis=0), in_offset=None,
                                           bounds_check=bc_tw, oob_is_err=False))
            for dSc in dScs:
                pool.wait_ev((dSc, S.dcount[dSc]))
                sp.wait_ev((dSc, S.dcount[dSc]))
            dr["TW"].w = None
            dr["TW"].r = []

            with ExitStack() as s3:
                Wb = [mk(s3, "Wb%d" % i, [128, WROW], BF16) for i in range(NS_W)]
                order = []
                for i_ in range(max(STREAM_STARTS[k_ + 1] - STREAM_STARTS[k_] for k_ in range(NS_W))):
                    for k_ in range(NS_W):
                        if STREAM_STARTS[k_] + i_ < STREAM_STARTS[k_ + 1]:
                            order.append((STREAM_STARTS[k_] + i_, k_))
                assert sorted(b_ for b_, _ in order) == list(range(NB))
                twb = [mk(s3, "twb%d" % i, [128, 4], I32) for i in range(4)]
                xg = [mk(s3, "xg%d" % i, [128, D], BF16) for i in range(2)]
                xgT = [mk(s3, "xgT%d" % i, [128, 8, 128], BF16) for i in range(2)]
                sg = [mk(s3, "sg%d" % i, [128, 512], F32) for i in range(2)]
                aT = [mk(s3, "aT%d" % i, [128, 4, 128], BF16) for i in range(2)]
                ysb = [mk(s3, "ysb%d" % i, [128, D], F32) for i in range(2)]
                dWg = [S.dsem() for _ in range(NS_W)]
                dTw = [S.dsem() for _ in range(4)]
                dXg = [S.dsem(), S.dsem()]
                dYs = [S.dsem(), S.dsem()]

                def load_tw(pos):
                    b, ws = order[pos]
                    p4 = pos % 4
                    sp.dma(dTw[p4], twb[p4].t[:], TW[b * 128:(b + 1) * 128, :], reads=[dr["TW"]], writes=[twb[p4].b])

                def load_w(pos):
                    b, ws = order[pos]
                    pool.dma(dWg[ws], Wb[ws].t[:, :], wall[:, :], reads=[widx.b], writes=[Wb[ws].b],
                             indirect=dict(out_offset=None, in_offset=bass.IndirectOffsetOnAxis(ap=widx.t[:, b:b + 1], axis=0),
                                           bounds_check=bc_w, oob_is_err=False))

                def load_x(pos):
                    p2, p4 = pos % 2, pos % 4
                    pool.dma(dXg[p2], xg[p2].t[:, :], H2[:, :], reads=[twb[p4].b, dr["H2"]], writes=[xg[p2].b],
                             indirect=dict(out_offset=None, in_offset=bass.IndirectOffsetOnAxis(ap=twb[p4].t[:, 0:1], axis=0)))

                for pos in range(3):
                    load_tw(pos)
                for pos in range(NS_W):
                    load_w(pos)
                load_x(0)
                load_x(1)
                for pos in range(NB):
                    if pos + 3 < NB:
                        load_tw(pos + 3)
                    b, ws = order[pos]
                    p2, p4 = pos % 2, pos % 4
                    W = Wb[ws]
                    tbm = bank_bf(0)
                    for c in range(8):
                        pe.op(lambda e, c=c: e.transpose(out=tbm[:, c * 128:(c + 1) * 128], in_=xg[p2].t[:, c * 128:(c + 1) * 128], identity=ident.t[:, :]),
                              reads=[xg[p2].b, ident.b], writes=[PB[0]])
                    act.op(lambda e: e.copy(out=xgT[p2].t[:].rearrange("p c n -> p (c n)"), in_=tbm[:, 0:1024]), reads=[PB[0]], writes=[xgT[p2].b])
                    if pos + 2 < NB:
                        load_x(pos + 2)
                    gbk, ubk = 1 + p2, 3 + p2
                    for (bk_, off) in ((gbk, 0), (ubk, 4096)):
                        for j in range(4):
                            for c in range(8):
                                pe.op(lambda e, c=c, j=j, bk_=bk_, off=off: e.matmul(
                                    ps[:, bk_ * 512 + j * 128: bk_ * 512 + (j + 1) * 128],
                                    lhsT=W.t[:, off + c * 512 + j * 128: off + c * 512 + (j + 1) * 128], rhs=xgT[p2].t[:, c, :],
                                    start=(c == 0), stop=(c == 7)), reads=[W.b, xgT[p2].b], writes=[PB[bk_]])
                    act.op(lambda e: e.activation(out=sg[p2].t[:], in_=bank(gbk), func=AF.Silu), reads=[PB[gbk]], writes=[sg[p2].b])
                    dve.op(lambda e: e.tensor_tensor(out=aT[p2].t[:].rearrange("p j n -> p (j n)"), in0=bank(ubk), in1=sg[p2].t[:], op=ALU.mult),
                           reads=[PB[ubk], sg[p2].b], writes=[aT[p2].b])
                    for hf in range(2):
                        for j in range(4):
                            pe.op(lambda e, hf=hf, j=j: e.matmul(ps[:, (5 + hf) * 512:(6 + hf) * 512], lhsT=aT[p2].t[:, j, :],
                                                                 rhs=W.t[:, 8192 + j * 1024 + hf * 512: 8192 + j * 1024 + (hf + 1) * 512],
                                                                 start=(j == 0), stop=(j == 3)), reads=[aT[p2].b, W.b], writes=[PB[5 + hf]])
                    gcol = twb[p4].t[:].bitcast(F32)[:, 2:3]
                    act.op(lambda e: e.mul(out=ysb[p2].t[:, 0:512], in_=bank(5), mul=gcol),
                           reads=[PB[5], twb[p4].b], writes=[ysb[p2].b])
                    dve.op(lambda e: e.tensor_scalar(out=ysb[p2].t[:, 512:1024], in0=bank(6), scalar1=gcol, scalar2=None, op0=ALU.mult),
                           reads=[PB[6], twb[p4].b], writes=[ysb[p2].b])
                    pool.dma(dYs[p2], Y[:, :], ysb[p2].t[:, :], reads=[ysb[p2].b, twb[p4].b], writes=[],
                             indirect=dict(out_offset=bass.IndirectOffsetOnAxis(ap=twb[p4].t[:, 1:2], axis=0), in_offset=None,
                                           bounds_check=bc_y, oob_is_err=False))
                    if pos + NS_W < NB:
                        load_w(pos + NS_W)
                for k_ in dYs:
                    pool.wait_ev((k_, S.dcount[k_]))
                    sp.wait_ev((k_, S.dcount[k_]))
                barrier_all(S)

        with ExitStack() as s4:
            GFp = mk(s4, "GFp", [128, D], F32)
            GFs = mk(s4, "GFs", [64, D], F32)
            FN = mk(s4, "FN", [128, D], F32)
            d4 = S.dsem()
            sp.dma(d4, GFp.t[:], MODV[5, 0:1, :].partition_broadcast(128), writes=[GFp.b], group=True)
            sp.dma(d4, GFs.t[:, :], MODV[5, 1:65, :], writes=[GFs.b], group=True)
            sp.dma(d4, FN.t[:], final_norm.partition_broadcast(128), writes=[FN.b], group=True)
            for t_ in (GFp, GFs, FN):
                t_.b.w = (d4, S.dcount[d4])
            NR = 3
            xa = [mk(s4, "xa%d" % i, [128, D], F32) for i in range(NR)]
            ya = [mk(s4, "ya%d" % i, [128, D], F32) for i in range(NR)]
            yb = [mk(s4, "yb%d" % i, [128, D], F32) for i in range(NR)]
            yo = [mk(s4, "yo%d" % i, [128, D], F32) for i in range(NR)]
            jk = mk(s4, "jk", [128, D], BF16)
            sf = [mk(s4, "sf%d" % i, [128, 4], F32) for i in range(NR)]
            dL = [S.dsem() for _ in range(NR)]
            dSt = [S.dsem() for _ in range(NR)]

            def ftile(ti):
                p = ti % NR
                nq = 64 if ti == NPB else 128
                r_ = slice(0, nq)
                t0 = ti * 128
                GF = GFs if ti == NPB else GFp
                sp.dma(dL[p], xa[p].t[r_, :], X1[t0:t0 + nq, :], writes=[xa[p].b], group=True)
                sp.dma(dL[p], ya[p].t[r_, :], Y[t0:t0 + nq, :], writes=[ya[p].b], group=True)
                sp.dma(dL[p], yb[p].t[r_, :], Y[HROWS + t0:HROWS + t0 + nq, :], writes=[yb[p].b], group=True)

                def _fix():
                    for t_ in (xa[p], ya[p], yb[p]):
                        t_.b.w = (dL[p], S.dcount[dL[p]])
                S.defer(_fix, writes=[xa[p].b, ya[p].b, yb[p].b])
                dve.op(lambda e: e.tensor_tensor(out=ya[p].t[r_, :], in0=ya[p].t[r_, :], in1=yb[p].t[r_, :], op=ALU.add),
                       reads=[ya[p].b, yb[p].b], writes=[ya[p].b])
                dve.op(lambda e: e.tensor_tensor(out=ya[p].t[r_, :], in0=ya[p].t[r_, :], in1=GF.t[r_, :], op=ALU.mult),
                       reads=[ya[p].b, GF.b], writes=[ya[p].b])
                pool.op(lambda e: e.tensor_tensor(out=xa[p].t[r_, :], in0=xa[p].t[r_, :], in1=ya[p].t[r_, :], op=ALU.add),
                        reads=[xa[p].b, ya[p].b], writes=[xa[p].b])
                dve.op(lambda e: e.memset(sf[p].t[:], 0.0), writes=[sf[p].b])
                act.op(lambda e: e.activation(out=jk.t[r_, :], in_=xa[p].t[r_, :], func=AF.Square, accum_out=sf[p].t[r_, 0:1]),
                       reads=[xa[p].b, sf[p].b], writes=[jk.b, sf[p].b])
                act.op(lambda e: e.activation(out=sf[p].t[r_, 1:2], in_=sf[p].t[r_, 0:1], func=AF.Ln, scale=1.0 / D, bias=EPS),
                       reads=[sf[p].b], writes=[sf[p].b])
                act.op(lambda e: e.activation(out=sf[p].t[r_, 1:2], in_=sf[p].t[r_, 1:2], func=AF.Exp, scale=-0.5), reads=[sf[p].b], writes=[sf[p].b])
                dve.op(lambda e: e.scalar_tensor_tensor(out=yo[p].t[r_, :], in0=xa[p].t[r_, :], scalar=sf[p].t[r_, 1:2], in1=FN.t[r_, :],
                                                        op0=ALU.mult, op1=ALU.mult), reads=[xa[p].b, sf[p].b, FN.b], writes=[yo[p].b])
                dst = y_s if ti == NPB else y_p[t0:t0 + nq, :]
                sp.dma(dSt[p], dst, yo[p].t[r_, :], reads=[yo[p].b], writes=[])
                yield

            S.merge_emit([S.record(ftile(ti)) for ti in range(NTT)], window=NR)
            for k_ in dSt:
                sp.wait_ev((k_, S.dcount[k_]))
            sp.wait_ev((dOut, S.dcount[dOut]))
            barrier_all(S)
    return nc


def barrier_all(S, dsems=()):
    snap = {k: e.n for k, e in S.E.items()}
    for name, e in S.E.items():
        for k, v in snap.items():
            if k == name or v == 0:
                continue
            e.wait_ev((k, v))
        for k in dsems:
            if S.dcount[k] > 0:
                e.wait_ev((k, S.dcount[k]))


_CACHE = {}


def _consts():
    if "c" in _CACHE:
        return _CACHE["c"]
    bf = ml_dtypes.bfloat16
    c = {}
    c["ident"] = np.eye(128, dtype=np.float32).astype(bf)
    c["triu"] = np.triu(np.ones((128, 128), np.float32), 1)
    c["ones"] = np.ones((128, 128), np.float32)
    g = np.zeros((128, 128), np.float32)
    g[:64, :64] = 1
    g[64:, 64:] = 1
    c["g64"] = g
    i = np.arange(128)[:, None]
    j = np.arange(128)[None, :]
    prev = np.where(j > i, 0.0, NEG)
    cur = np.where(j <= i, 0.0, NEG)
    c["maskp"] = np.concatenate([prev, cur], 1).astype(np.float32).astype(bf)
    c["mask0_first"] = np.concatenate([np.full((128, 128), NEG), cur], 1).astype(np.float32).astype(bf)
    ms = np.full((64, 2112), NEG, np.float32)
    for t in range(64):
        s, jj = t // 4, t % 4
        ms[t, s * 128 + jj + 1:(s + 1) * 128] = 0.0
        ms[t, 2048 + 4 * s: 2048 + 4 * s + jj + 1] = 0.0
    c["masks"] = np.concatenate([ms, ms], 0).astype(bf)
    c["thr"] = (128.0 * np.arange(128, dtype=np.float32))[None, :]
    c["piota"] = np.arange(128, dtype=np.float32)[:, None]
    tok = (np.arange(NTT)[None, :] * 128 + np.arange(128)[:, None]).astype(np.int32)
    c["tokid"] = tok
    tw = np.zeros((128, 4), np.int32)
    tw[:, 0] = NTOK
    tw[:, 1] = 2 * HROWS
    c["twinit"] = tw
    _CACHE["c"] = c
    return c


def _rope_tables(pos):
    half = 32
    inv = (10000.0 ** (-np.arange(half, dtype=np.float32) / half)).astype(np.float32)
    ang = pos.astype(np.float32)[:, None] * inv[None, :]
    return np.cos(ang).astype(np.float32), np.sin(ang).astype(np.float32)


def kernel(x_prompt, x_sample, cache_k, cache_v, state_conv, c_prompt, c_sample,
           attn_norm, ffn_norm, w_mod, b_mod, w_in, conv_w, attn_sinks,
           out_norm_attn, out_norm_conv, w_out, w_group, b_group, w_expert, b_expert,
           w_gate, w_up, w_down, final_norm):
    f = lambda a: np.ascontiguousarray(np.asarray(a, dtype=np.float32))
    x_prompt, x_sample, cache_k, cache_v, state_conv = map(f, (x_prompt, x_sample, cache_k, cache_v, state_conv))
    c_prompt, c_sample = f(c_prompt), f(c_sample)
    C = _consts()
    if "nc" not in _CACHE:
        _CACHE["nc"] = build_program()
    nc = _CACHE["nc"]

    wg, wu, wd = f(w_gate)[0], f(w_up)[0], f(w_down)[0]
    wall = np.concatenate([
        wg.reshape(NE, 8, 128, 512).transpose(0, 2, 1, 3).reshape(NE, 128, 4096),
        wu.reshape(NE, 8, 128, 512).transpose(0, 2, 1, 3).reshape(NE, 128, 4096),
        wd.reshape(NE, 4, 128, 1024).transpose(0, 2, 1, 3).reshape(NE, 128, 4096)], axis=2).reshape(NE * 128, WROW)
    wall = np.ascontiguousarray(wall)
    shared = {
        "w_mod": f(w_mod)[0], "b_mod": f(b_mod)[0][None, :], "attn_norm": f(attn_norm)[0][None, :], "ffn_norm": f(ffn_norm)[0][None, :],
        "final_norm": f(final_norm)[None, :], "w_in": f(w_in)[0], "w_out": f(w_out)[0],
        "w_r": np.ascontiguousarray(np.concatenate([f(w_group)[0], f(w_expert)[0]], 1)),
        "b_r": np.concatenate([f(b_group)[0], f(b_expert)[0]])[None, :],
        "convwT": np.ascontiguousarray(f(conv_w)[0].reshape(3, 4, 128).transpose(2, 1, 0).reshape(128, 12)),
        "sinks": f(attn_sinks)[0][None, :], "gn_attn": f(out_norm_attn)[0][None, :],
        "gn_convT": np.ascontiguousarray(f(out_norm_conv)[0].reshape(4, 128).T),
        "wall": wall,
        "ident": C["ident"], "triu": C["triu"], "ones": C["ones"], "g64": C["g64"], "maskp": C["maskp"], "masks": C["masks"],
        "tokid": C["tokid"], "piota": C["piota"], "thr": C["thr"], "twinit": C["twinit"],
    }
    cs_s, sn_s = _rope_tables(16384 + (np.arange(64) % 4))
    in_maps = []
    for i in range(8):
        seq, half = i // 2, i % 2
        xpc = np.zeros((NBLK * 128, D), np.float32)
        xpc[128:] = x_prompt[seq, half * 4096:(half + 1) * 4096]
        if half == 1:
            xpc[:128] = x_prompt[seq, 4096 - 128:4096]
        pos = half * 4096 - 128 + np.arange(NBLK * 128)
        cs, sn = _rope_tables(np.maximum(pos, 0))
        ss = slice(16 * i, 16 * i + 16)
        ck = cache_k[0, ss]
        kct = ck.transpose(2, 3, 0, 1).reshape(2, 64, 16 * 128)
        kct = np.concatenate([kct, kct], axis=1).transpose(1, 0, 2)
        m = dict(shared)
        m.update({
            "xp": xpc, "xs": np.ascontiguousarray(x_sample[ss].reshape(64, D)),
            "cT": np.ascontiguousarray(np.concatenate([c_prompt[seq:seq + 1], np.repeat(c_sample[ss], 4, axis=0)], 0).T),
            "kcT": np.ascontiguousarray(kct),
            "vc": np.ascontiguousarray(cache_v[0, ss].reshape(16, 128, 128).transpose(1, 0, 2)),
            "ck_nat": np.ascontiguousarray(ck.reshape(16, 128, 128)),
            "cv_nat": np.ascontiguousarray(cache_v[0, ss].reshape(16, 128, 128)),
            "stT": np.ascontiguousarray(state_conv[0, ss].reshape(16, 2, 4, 128).transpose(3, 2, 0, 1)),
            "mask0": C["mask0_first"] if half == 0 else C["maskp"],
            "cosp": np.ascontiguousarray(cs.reshape(NBLK, 128, 32).transpose(1, 0, 2)),
            "sinp": np.ascontiguousarray(sn.reshape(NBLK, 128, 32).transpose(1, 0, 2)),
            "coss": cs_s, "sins": sn_s,
            "flag": np.full((128, 1), float(half), np.float32),
        })
        in_maps.append(m)
    res = run_bass_kernel_spmd(nc, in_maps, core_ids=list(range(8)))
    R = res.results
    y_prompt = np.stack([np.concatenate([R[2 * s]["y_p"], R[2 * s + 1]["y_p"]], 0) for s in range(4)], 0)
    y_sample = np.concatenate([R[i]["y_s"].reshape(16, 4, D) for i in range(8)], 0)
    nkp = np.stack([R[2 * s + 1]["nk_p"].reshape(128, 2, 64) for s in range(4)], 0)[None]
    nvp = np.stack([R[2 * s + 1]["nv_p"].reshape(128, 2, 64) for s in range(4)], 0)[None]
    ncp = np.stack([R[2 * s + 1]["ncv_p"] for s in range(4)], 0)[None]
    nks = np.concatenate([R[i]["nk_s"].reshape(16, 128, 2, 64) for i in range(8)], 0)[None]
    nvs = np.concatenate([R[i]["nv_s"].reshape(16, 128, 2, 64) for i in range(8)], 0)[None]
    ncs = np.concatenate([R[i]["ncv_s"] for i in range(8)], 0)[None]
    return (y_prompt.astype(np.float32), y_sample.astype(np.float32), nkp.astype(np.float32), nvp.astype(np.float32),
            ncp.astype(np.float32), nks.astype(np.float32), nvs.astype(np.float32), ncs.astype(np.float32))
```

```python
import numpy as np
import ml_dtypes
from contextlib import ExitStack
import concourse.bass as bass
import concourse.mybir as mybir
from concourse.bass_utils import run_bass_kernel_spmd

F32 = mybir.dt.float32
BF16 = mybir.dt.bfloat16
I32 = mybir.dt.int32
AF = mybir.ActivationFunctionType
ALU = mybir.AluOpType
AX = mybir.AxisListType

D = 1024
NPB = 32
NBLK = NPB + 1
NSEQ_S = 16
NTOK = NPB * 128 + 64
NTT = 33
HROWS = 4224
NB = 97
NE = 32
EPS = 1e-5
NEG = -30000.0
OOB = 4096.0
WROW = 12288
NS_W = 5
PIPE_WINDOW = 3
STREAM_STARTS = [0, 20, 40, 59, 78, 97]
SAME_ENGINE_SYNC = ("pool", "act", "dve")


class Buf:
    __slots__ = ("name", "w", "r", "excl")

    def __init__(self, name="", excl=False):
        self.name = name
        self.w = None
        self.r = []
        self.excl = excl


class Eng:
    def __init__(self, S, name, eng, sem):
        self.S = S
        self.name = name
        self.eng = eng
        self.sem = sem
        self.n = 0
        self.seen = {}

    @staticmethod
    def _need(ev, deps):
        if ev is None:
            return
        k, v = ev
        if deps.get(k, 0) < v:
            deps[k] = v

    def sync(self, reads, writes):
        deps = {}
        for b in reads:
            self._need(b.w, deps)
            if b.excl:
                for ev in b.r:
                    if ev[0] != self.name:
                        self._need(ev, deps)
        for b in writes:
            self._need(b.w, deps)
            for ev in b.r:
                self._need(ev, deps)
        for k, v in deps.items():
            if k == self.name and self.name not in SAME_ENGINE_SYNC:
                continue
            if self.seen.get(k, 0) >= v:
                continue
            self.eng.wait_ge(self.S.sems[k], v)
            self.seen[k] = v

    def op(self, fn, reads=(), writes=(), cost=None):
        if self.S.rec is not None:
            self.S.rec.append(("op", self, fn, tuple(reads), tuple(writes), cost))
            return None
        self.sync(reads, writes)
        ins = fn(self.eng)
        self.n += 1
        ins.then_inc(self.sem, 1)
        ev = (self.name, self.n)
        for b in reads:
            b.r.append(ev)
            if len(b.r) > 24:
                b.r = self._compact(b.r)
        for b in writes:
            b.w = ev
            b.r = []
        return ins

    @staticmethod
    def _compact(evs):
        best = {}
        for k, v in evs:
            if best.get(k, 0) < v:
                best[k] = v
        return list(best.items())

    def dma(self, dsem, out, in_, reads=(), writes=(), indirect=None, group=False, **kw):
        if self.S.rec is not None:
            kw2 = dict(kw)
            kw2.update(indirect=indirect, group=group)
            self.S.rec.append(("dma", self, (dsem, out, in_), tuple(reads), tuple(writes), kw2))
            return None
        self.sync(reads, writes)
        if (not group or self.name == "pool") and self.S.dcount[dsem] > 0:
            self.wait_ev((dsem, self.S.dcount[dsem]))
        if indirect is None:
            ins = self.eng.dma_start(out=out, in_=in_, **kw)
        else:
            ins = self.eng.indirect_dma_start(out=out, in_=in_, **indirect, **kw)
        self.S.dcount[dsem] += 16
        ins.then_inc(self.S.sems[dsem], 16)
        ev = (dsem, self.S.dcount[dsem])
        for b in reads:
            b.r.append(ev)
        for b in writes:
            b.w = ev
            b.r = []
        return ins

    def wait_ev(self, ev):
        k, v = ev
        if self.seen.get(k, 0) >= v:
            return
        self.eng.wait_ge(self.S.sems[k], v)
        self.seen[k] = v


class Sched:
    def __init__(self, nc, stack, n_dma_sems):
        self.nc = nc
        self.sems = {}
        self.dcount = {}
        self.E = {}
        self.rec = None
        for name, eng in (("pe", nc.tensor), ("act", nc.scalar), ("dve", nc.vector),
                          ("pool", nc.gpsimd), ("sp", nc.sync)):
            s = stack.enter_context(nc.semaphore("s_" + name))
            self.sems[name] = s
            self.E[name] = Eng(self, name, eng, s)
        self.free = []
        for i in range(n_dma_sems):
            k = "d%d" % i
            self.sems[k] = stack.enter_context(nc.semaphore(k))
            self.dcount[k] = 0
            self.free.append(k)

    def dsem(self):
        return self.free.pop()

    def defer(self, fn, reads=(), writes=()):
        if self.rec is not None:
            self.rec.append(("call", None, fn, tuple(reads), tuple(writes), None))
        else:
            fn()

    def record(self, gen):
        assert self.rec is None
        self.rec = []
        for _ in gen:
            pass
        r, self.rec = self.rec, None
        return r

    def merge_emit(self, progs, window=2):
        ENG_COST = {"pe": 0.09, "dve": 0.43, "act": 0.50, "pool": 0.85, "sp": 0.05}
        eng_free = {}
        buf_ready = {}
        active = []
        nxt = 0

        def pend(rs):
            pr, pw = {}, {}
            for rec in rs:
                for b in rec[3]:
                    pr[id(b)] = pr.get(id(b), 0) + 1
                for b in rec[4]:
                    pw[id(b)] = pw.get(id(b), 0) + 1
            return pr, pw

        while nxt < len(progs) or active:
            while nxt < len(progs) and len(active) < window:
                pr, pw = pend(progs[nxt])
                active.append([progs[nxt], 0, pr, pw])
                nxt += 1
            best = None
            for ai, a in enumerate(active):
                rec = a[0][a[1]]
                ok = True
                for o in active[:ai]:
                    for b in rec[4]:
                        if o[2].get(id(b), 0) or o[3].get(id(b), 0):
                            ok = False
                            break
                    if ok:
                        for b in rec[3]:
                            if o[3].get(id(b), 0):
                                ok = False
                                break
                    if not ok:
                        break
                if not ok:
                    continue
                kind, eng = rec[0], rec[1]
                en = eng.name if eng is not None else "sp"
                t0 = eng_free.get(en, 0.0)
                for b in rec[3] + rec[4]:
                    t0 = max(t0, buf_ready.get(id(b), 0.0))
                if best is None or t0 < best[0] - 1e-9:
                    best = (t0, ai, en)
            t0, ai, en = best
            a = active[ai]
            rec = a[0][a[1]]
            kind, eng = rec[0], rec[1]
            if kind == "op":
                eng.op(rec[2], reads=rec[3], writes=rec[4])
                c = rec[5] if rec[5] is not None else ENG_COST[en]
                eng_free[en] = t0 + c
                done = t0 + c + 0.15
            elif kind == "dma":
                dsem, out, in_ = rec[2]
                eng.dma(dsem, out, in_, reads=rec[3], writes=rec[4], **rec[5])
                eng_free[en] = t0 + 0.05
                done = t0 + 2.5
            else:
                rec[2]()
                done = t0
            for b in rec[4]:
                buf_ready[id(b)] = done
                a[3][id(b)] -= 1
            for b in rec[3]:
                a[2][id(b)] -= 1
            a[1] += 1
            if a[1] == len(a[0]):
                active.pop(ai)


class T:
    def __init__(self, t, name):
        self.t = t
        self.b = Buf(name)


def build_program():
    nc = bass.Bass("TRN2", target_bir_lowering=False)

    def din(name, shape, dt=F32):
        return nc.dram_tensor(name, list(shape), dt, kind="ExternalInput").ap()

    def dout(name, shape, dt=F32):
        return nc.dram_tensor(name, list(shape), dt, kind="ExternalOutput").ap()

    def dscr(name, shape, dt):
        return nc.dram_tensor(name, list(shape), dt, kind="Internal").ap()

    xp = din("xp", [NBLK * 128, D])
    xs = din("xs", [64, D])
    cT = din("cT", [D, 65])
    w_mod = din("w_mod", [D, 6 * D])
    b_mod = din("b_mod", [1, 6 * D])
    attn_norm = din("attn_norm", [1, D])
    ffn_norm = din("ffn_norm", [1, D])
    final_norm = din("final_norm", [1, D])
    w_in = din("w_in", [D, 2304])
    w_out = din("w_out", [D, D])
    w_r = din("w_r", [D, 36])
    b_r = din("b_r", [1, 36])
    convwT = din("convwT", [128, 12])
    sinks = din("sinks", [1, 8])
    gn_attn = din("gn_attn", [1, 512])
    gn_convT = din("gn_convT", [128, 4])
    wall = din("wall", [NE * 128, WROW])
    kcT_d = din("kcT", [128, 2, NSEQ_S * 128])
    vc_d = din("vc", [128, NSEQ_S, 128])
    ck_nat = din("ck_nat", [NSEQ_S, 128, 128])
    cv_nat = din("cv_nat", [NSEQ_S, 128, 128])
    stT = din("stT", [128, 4, NSEQ_S, 2])
    ident_d = din("ident", [128, 128], BF16)
    triu_d = din("triu", [128, 128])
    ones_d = din("ones", [128, 128])
    g64_d = din("g64", [128, 128])
    maskp_d = din("maskp", [128, 256], BF16)
    mask0_d = din("mask0", [128, 256], BF16)
    masks_d = din("masks", [128, 2112], BF16)
    cosp_d = din("cosp", [128, NBLK, 32])
    sinp_d = din("sinp", [128, NBLK, 32])
    coss_d = din("coss", [64, 32])
    sins_d = din("sins", [64, 32])
    flag_d = din("flag", [128, 1])
    tokid_d = din("tokid", [128, NTT], I32)
    piota_d = din("piota", [128, 1])
    thr_d = din("thr", [1, 128])
    twinit_d = din("twinit", [128, 4], I32)
    y_p = dout("y_p", [NPB * 128, D])
    y_s = dout("y_s", [64, D])
    nk_p = dout("nk_p", [128, 128])
    nv_p = dout("nv_p", [128, 128])
    ncv_p = dout("ncv_p", [2, 512])
    nk_s = dout("nk_s", [NSEQ_S, 128, 128])
    nv_s = dout("nv_s", [NSEQ_S, 128, 128])
    ncv_s = dout("ncv_s", [NSEQ_S, 2, 512])
    H2 = dscr("H2", [HROWS, D], BF16)
    X1 = dscr("X1", [HROWS, D], F32)
    Y = dscr("Y", [2 * HROWS, D], F32)
    TW = dscr("TW", [NB * 128, 4], I32)
    MODV = dscr("MODV", [6, 65, D], F32)

    dr = {k: Buf(k) for k in ("H2", "X1", "Y", "TW", "MODV", "out")}

    with ExitStack() as st0:
        S = Sched(nc, st0, 64)
        pe, act, dve, pool, sp = (S.E[k] for k in ("pe", "act", "dve", "pool", "sp"))

        def mk(stack, name, shape, dt):
            return T(stack.enter_context(nc.sbuf_tensor("sb_" + name, list(shape), dt)), name)

        def breg(name, val):
            r = nc.gpsimd.alloc_register(name)
            nc.gpsimd.reg_mov(r, val)
            return r

        bc_tw = breg("bc_tw", NB * 128 - 1)
        bc_w = breg("bc_w", NE * 128 - 1)
        bc_y = breg("bc_y", 2 * HROWS - 1)
        ps = st0.enter_context(nc.psum_tensor("ps", [128, 4096], F32))
        PB = [Buf("pb%d" % i, excl=True) for i in range(8)]

        def bank(i, n=1):
            return ps[:, i * 512:(i + n) * 512]

        def bank_bf(i, n=1):
            return ps[:, i * 512:(i + n) * 512].bitcast(BF16)

        ident = mk(st0, "ident", [128, 128], BF16)
        onesf = mk(st0, "onesf", [128, 128], F32)
        triu = mk(st0, "triu", [128, 128], F32)
        dC = S.dsem()
        sp.dma(dC, ident.t[:], ident_d, writes=[ident.b], group=True)
        sp.dma(dC, onesf.t[:], ones_d, writes=[onesf.b], group=True)
        sp.dma(dC, triu.t[:], triu_d, writes=[triu.b], group=True)
        dOut = S.dsem()
        dNK, dNV = S.dsem(), S.dsem()
        qk_f0 = mk(st0, "qk_f", [128, 640], F32)
        v_f0 = mk(st0, "v_f", [128, 128], F32)
        qk_fL = [qk_f0, qk_f0]
        v_fL = [v_f0, v_f0]
        uext = [mk(st0, "uext%d" % i, [128, 4, 130], F32) for i in range(2)]
        uexts = mk(st0, "uexts", [128, 4, NSEQ_S * 6], F32)
        for t_ in (ident, onesf, triu):
            t_.b.w = (dC, S.dcount[dC])
        twi = mk(st0, "twi", [128, 4], I32)
        dI = S.dsem()
        gate_all = mk(st0, "gate_all", [128, NTT, NE], F32)
        rs_all = mk(st0, "rs_all", [128, NTT, NE], F32)
        sel_all = mk(st0, "sel_all", [128, NTT, NE], F32)
        cumsel = mk(st0, "cumsel", [128, NE], F32)
        pool.op(lambda e: e.memset(gate_all.t[:], 0.0), writes=[gate_all.b])
        pool.op(lambda e: e.memset(rs_all.t[:], 0.0), writes=[rs_all.b])
        pool.op(lambda e: e.memset(sel_all.t[:], 0.0), writes=[sel_all.b])
        pool.op(lambda e: e.memset(cumsel.t[:], 0.0), writes=[cumsel.b])
        sW = ExitStack()
        w_in_bf = mk(sW, "w_in_bf", [128, 8, 2304], BF16)
        w_out_bf = mk(sW, "w_out_bf", [128, 8, D], BF16)
        w_r_bf = mk(sW, "w_r_bf", [128, 8, 36], BF16)
        kcT = mk(sW, "kcT", [128, 2, NSEQ_S * 128], BF16)
        vc = mk(sW, "vc", [128, NSEQ_S, 128], BF16)
        w_in_parts = [Buf("w_in_c%d" % c) for c in range(8)]
        for c in range(8):
            pool.dma(S.dsem(), w_in_bf.t[:, c, :], w_in[c * 128:(c + 1) * 128, :], writes=[w_in_parts[c]])
        pool.dma(S.dsem(), w_out_bf.t[:], w_out.rearrange("(c p) n -> p c n", p=128), writes=[w_out_bf.b])
        pool.dma(S.dsem(), w_r_bf.t[:], w_r.rearrange("(c p) n -> p c n", p=128), writes=[w_r_bf.b])
        pool.dma(S.dsem(), kcT.t[:], kcT_d, writes=[kcT.b])
        pool.dma(S.dsem(), vc.t[:], vc_d, writes=[vc.b])

        with ExitStack() as s0:
            cTt = mk(s0, "cTt", [128, 8, 65], F32)
            silu = mk(s0, "silu", [128, 8, 65], F32)
            wmb = [mk(s0, "wmb%d" % i, [128, 8, 512], F32) for i in range(2)]
            mod17 = mk(s0, "mod17", [65, 6 * D], F32)
            bm17 = mk(s0, "bm17", [65, 6 * D], F32)
            an17 = mk(s0, "an17", [65, D], F32)
            fn17 = mk(s0, "fn17", [65, D], F32)
            a17 = mk(s0, "a17", [65, 2 * D], F32)
            d0 = S.dsem()
            sp.dma(d0, cTt.t[:], cT.rearrange("(c p) r -> p c r", p=128), writes=[cTt.b], group=True)
            sp.dma(d0, bm17.t[:], b_mod.partition_broadcast(65), writes=[bm17.b], group=True)
            sp.dma(d0, an17.t[:], attn_norm.partition_broadcast(65), writes=[an17.b], group=True)
            sp.dma(d0, fn17.t[:], ffn_norm.partition_broadcast(65), writes=[fn17.b], group=True)
            for t_ in (cTt, bm17, an17, fn17):
                t_.b.w = (d0, S.dcount[d0])
            act.op(lambda e: e.activation(out=silu.t[:], in_=cTt.t[:], func=AF.Silu), reads=[cTt.b], writes=[silu.b])
            act.dma(dI, twi.t[:], twinit_d, writes=[twi.b])
            for b_ in range(NB):
                act.dma(dI, TW[b_ * 128:(b_ + 1) * 128, :], twi.t[:], reads=[twi.b], writes=[dr["TW"]], group=True)
            dr["TW"].w = (dI, S.dcount[dI])
            dW = [S.dsem(), S.dsem()]
            for j in range(12):
                wb = wmb[j % 2]
                sp.dma(dW[j % 2], wb.t[:], w_mod[:, j * 512:(j + 1) * 512].rearrange("(c p) n -> p c n", p=128),
                       writes=[wb.b])
                pb = PB[j % 2]
                for c in range(8):
                    pe.op(lambda e, c=c, wb=wb, j=j: e.matmul(bank(j % 2)[0:65, :], lhsT=silu.t[:, c, :], rhs=wb.t[:, c, :],
                                                              start=(c == 0), stop=(c == 7)),
                          reads=[silu.b, wb.b], writes=[pb])
                dve.op(lambda e, j=j: e.tensor_tensor(out=mod17.t[:, j * 512:(j + 1) * 512], in0=bank(j % 2)[0:65, :],
                                                      in1=bm17.t[:, j * 512:(j + 1) * 512], op=ALU.add),
                       reads=[pb, bm17.b], writes=[mod17.b])
            dve.op(lambda e: e.scalar_tensor_tensor(out=a17.t[:, 0:D], in0=mod17.t[:, D:2 * D], scalar=1.0, in1=an17.t[:],
                                                    op0=ALU.add, op1=ALU.mult), reads=[mod17.b, an17.b], writes=[a17.b])
            dve.op(lambda e: e.scalar_tensor_tensor(out=a17.t[:, D:2 * D], in0=mod17.t[:, 4 * D:5 * D], scalar=1.0, in1=fn17.t[:],
                                                    op0=ALU.add, op1=ALU.mult), reads=[mod17.b, fn17.b], writes=[a17.b])
            dM = S.dsem()
            srcs = [a17.t[:, 0:D], mod17.t[:, 0:D], mod17.t[:, 2 * D:3 * D], a17.t[:, D:2 * D],
                    mod17.t[:, 3 * D:4 * D], mod17.t[:, 5 * D:6 * D]]
            for k in range(6):
                sp.dma(dM, MODV[k], srcs[k], reads=[a17.b, mod17.b], writes=[dr["MODV"]], group=True)
            dr["MODV"].w = (dM, S.dcount[dM])
            for e_ in S.E.values():
                e_.wait_ev(dr["MODV"].w)
            barrier_all(S)

        with ExitStack() as s1:
            BC = [mk(s1, "bc%d" % k, [128, D], F32) for k in range(6)]
            g64 = mk(s1, "g64", [128, 128], F32)
            maskp = mk(s1, "maskp", [128, 256], BF16)
            mask0 = mk(s1, "mask0", [128, 256], BF16)
            masks = mk(s1, "masks", [128, 2112], BF16)
            cosp = mk(s1, "cosp", [128, NBLK, 32], F32)
            sinp = mk(s1, "sinp", [128, NBLK, 32], F32)
            coss = mk(s1, "coss", [64, 32], F32)
            sins = mk(s1, "sins", [64, 32], F32)
            flag = mk(s1, "flag", [128, 1], F32)
            convw = mk(s1, "convw", [128, 12], F32)
            gncv = mk(s1, "gncv", [128, 4], F32)
            gnat = mk(s1, "gnat", [128, 512], F32)
            nsink = mk(s1, "nsink", [128, 8], F32)
            psink = mk(s1, "psink", [128, 8], F32)
            brt = mk(s1, "brt", [128, 36], F32)
            dK = S.dsem()
            cl = [(g64, g64_d), (maskp, maskp_d), (mask0, mask0_d), (masks, masks_d), (cosp, cosp_d), (sinp, sinp_d),
                  (coss, coss_d), (sins, sins_d), (flag, flag_d), (convw, convwT), (gncv, gn_convT),
                  (gnat, gn_attn.partition_broadcast(128)), (psink, sinks.partition_broadcast(128)),
                  (brt, b_r.partition_broadcast(128))]
            for t_, src in cl:
                sp.dma(dK, t_.t[:], src, writes=[t_.b], group=True)
            for k in range(6):
                sp.dma(dK, BC[k].t[:], MODV[k, 0:1, :].partition_broadcast(128), reads=[dr["MODV"]], writes=[BC[k].b], group=True)
            for t_, _ in cl:
                t_.b.w = (dK, S.dcount[dK])
            for k in range(6):
                BC[k].b.w = (dK, S.dcount[dK])
            dve.op(lambda e: e.tensor_scalar(out=nsink.t[:], in0=psink.t[:], scalar1=-1.0, scalar2=None, op0=ALU.mult),
                   reads=[psink.b], writes=[nsink.b])

            def mk2(name, shape, dt):
                return [mk(s1, "%s_%d" % (name, i), shape, dt) for i in range(2)]

            def mk1(name, shape, dt):
                t_ = mk(s1, name, shape, dt)
                return [t_, t_]

            def mk3(name, shape, dt):
                return [mk(s1, "%s_%d" % (name, i), shape, dt) for i in range(3)]

            Xt = mk3("xt", [128, D], F32)
            tmpL = mk3("tmp", [128, D], F32)
            hTL = mk3("hT", [128, 8 * 128], BF16)
            h2TL = hTL
            stt = mk3("stt", [128, 64], F32)
            caccL = mk3("cacc", [128, 4, 128], F32)
            csqL = mk3("csq", [128, 4, 128], F32)
            crsL = csqL
            kT = [mk(s1, "kT_%d" % i, [128, 2, 128], BF16) for i in range(3)]
            vb = [mk(s1, "vb_%d" % i, [128, 128], BF16) for i in range(3)]
            junkL = mk1("junk", [128, D], BF16)
            junk2 = mk(s1, "junk2", [128, D], BF16)
            h_bfL = mk1("h_bf", [128, D], BF16)
            qkvL = mk1("qkv_sb", [128, 768], F32)
            cvL = mk1("cv_sb", [128, 12, 128], F32)
            qkdL = mk1("qkd", [128, 768], BF16)
            qTL = mk2("qT", [128, 4, 128], BF16)
            r1L = mk1("r1", [128, 10, 32], F32)
            r2L = mk1("r2", [128, 10, 32], F32)
            P_bfL = mk1("P_bf", [128, 2176], BF16)
            PT_sbL = mk1("PT_sb", [128, 1088], BF16)
            O_sbL = mk1("O_sb", [128, 512], F32)
            On_bfL = mk1("On_bf", [128, 512], BF16)
            mixTL = mk1("mixT", [128, 8, 128], BF16)
            h2_bfL = mk1("h2_bf", [128, D], BF16)
            lgL = mk1("lg", [128, 36], F32)
            rtL = mk1("rt", [128, 160], F32)
            h2_bf = h2_bfL[0]

            dX = [S.dsem(), S.dsem(), S.dsem()]
            dOX = [S.dsem(), S.dsem()]
            dOH = [S.dsem(), S.dsem()]
            dS = S.dsem()
            for c in range(4):
                sp.dma(dS, uexts.t[:, c, :].rearrange("p (s l) -> p s l", s=NSEQ_S)[:, :, 0:2], stT[:, c, :, :], writes=[uexts.b],
                       allow_slow_non_contiguous=True, group=True)
            pool.op(lambda e: e.memset(h2_bf.t[:], 0.0), writes=[h2_bf.b])
            sp.dma(dS, H2[NTOK:NTOK + 64, :], h2_bf.t[0:64, :], reads=[h2_bf.b], writes=[dr["H2"]], group=True)
            sp.wait_ev((dS, S.dcount[dS]))
            uexts.b.w = (dS, S.dcount[dS])
            h2_bf.b.r = [(dS, S.dcount[dS])]

            def block(bi, kind):
                NQ = 64 if kind == "sample" else 128
                par = bi % 2
                r3, r3p = bi % 3, (bi - 1) % 3
                xt = Xt[r3]
                sq = stt[r3]
                tmp, junk, h_bf, hT, h2T = tmpL[r3], junkL[par], h_bfL[par], hTL[r3], h2TL[r3]
                qkv_sb, cv_sb, qkd, qT, r1, r2 = qkvL[par], cvL[par], qkdL[par], qTL[par], r1L[par], r2L[par]
                P_bf, PT_sb, O_sb, On_bf, mixT = P_bfL[par], PT_sbL[par], O_sbL[par], On_bfL[par], mixTL[par]
                cacc, csq, crs, x1, h2_bf, lg, rt = caccL[r3], csqL[r3], crsL[r3], Xt[r3], h2_bfL[par], lgL[par], rtL[par]
                qk_f, v_f = qk_fL[par], v_fL[par]
                if kind == "sample":
                    src = xs
                    cos_ap, sin_ap = coss.t[0:64, :], sins.t[0:64, :]
                    bk = dict(T=0, S=3, PT=1, O=0, T2=0, GS=3, MIX=4, H2T=0, LG=6)
                    G, NK, nchunk = 1, 2112, 17
                    for k in (0, 1):
                        sp.dma(dK, BC[k].t[0:64, :], MODV[k, 1:65, :], reads=[dr["MODV"]], writes=[BC[k].b], group=True)
                    def _fix01():
                        for k in (0, 1):
                            BC[k].b.w = (dK, S.dcount[dK])
                    S.defer(_fix01, writes=[BC[0].b, BC[1].b])
                else:
                    src = xp[bi * 128:(bi + 1) * 128, :]
                    cos_ap, sin_ap = cosp.t[:, bi, :], sinp.t[:, bi, :]
                    bk = dict(T=0, S=3, PT=4, O=5, T2=6, GS=7, MIX=6, H2T=6, LG=7)
                    G, NK, nchunk = 2, 256, 2
                rows = slice(0, NQ)
                sp.dma(dX[r3], xt.t[rows, :], src, writes=[xt.b])
                dve.op(lambda e: e.memset(sq.t[:], 0.0), writes=[sq.b])
                act.op(lambda e: e.activation(out=junk.t[rows, :], in_=xt.t[rows, :], func=AF.Square, accum_out=sq.t[rows, 0:1]),
                       reads=[xt.b], writes=[junk.b, sq.b])
                act.op(lambda e: e.activation(out=sq.t[rows, 1:2], in_=sq.t[rows, 0:1], func=AF.Ln, scale=1.0 / D, bias=EPS),
                       reads=[sq.b], writes=[sq.b])
                act.op(lambda e: e.activation(out=sq.t[rows, 1:2], in_=sq.t[rows, 1:2], func=AF.Exp, scale=-0.5), reads=[sq.b], writes=[sq.b])
                dve.op(lambda e: e.scalar_tensor_tensor(out=tmp.t[rows, :], in0=xt.t[rows, :], scalar=sq.t[rows, 1:2], in1=BC[0].t[rows, :],
                                                        op0=ALU.mult, op1=ALU.mult), reads=[xt.b, sq.b, BC[0].b], writes=[tmp.b])
                pool.op(lambda e: e.tensor_tensor(out=h_bf.t[rows, :], in0=tmp.t[rows, :], in1=BC[1].t[rows, :], op=ALU.add),
                        reads=[tmp.b, BC[1].b], writes=[h_bf.b])
                yield
                tb = bank_bf(bk["T"])
                for c in range(8):
                    pe.op(lambda e, c=c: e.transpose(out=tb[:, c * NQ:(c + 1) * NQ], in_=h_bf.t[rows, c * 128:(c + 1) * 128],
                                                     identity=ident.t[rows, rows]), reads=[h_bf.b, ident.b], writes=[PB[bk["T"]]])
                act.op(lambda e: e.copy(out=hT.t[:, 0:8 * NQ], in_=tb[:, 0:8 * NQ]), reads=[PB[bk["T"]]], writes=[hT.b])
                for lo, hi, iob in ((0, 512, 1), (512, 768, 2)):
                    for c in range(8):
                        pe.op(lambda e, c=c, lo=lo, hi=hi, iob=iob: e.matmul(ps[rows, iob * 512: iob * 512 + (hi - lo)], lhsT=hT.t[:, c * NQ:(c + 1) * NQ],
                                                                             rhs=w_in_bf.t[:, c, lo:hi], start=(c == 0), stop=(c == 7)),
                              reads=[hT.b, w_in_parts[c]], writes=[PB[iob]])
                    if iob == 1:
                        act.op(lambda e, lo=lo, hi=hi, iob=iob: e.copy(out=qkv_sb.t[rows, lo:hi], in_=ps[rows, iob * 512: iob * 512 + (hi - lo)]),
                               reads=[PB[iob]], writes=[qkv_sb.b])
                    else:
                        dve.op(lambda e, lo=lo, hi=hi, iob=iob: e.tensor_copy(out=qkv_sb.t[rows, lo:hi], in_=ps[rows, iob * 512: iob * 512 + (hi - lo)]),
                               reads=[PB[iob]], writes=[qkv_sb.b])
                for i_ in range(3):
                    iob = 1 + (i_ % 2)
                    for j in range(4 * i_, 4 * i_ + 4):
                        for c in range(8):
                            pe.op(lambda e, c=c, j=j, iob=iob: e.matmul(ps[:, iob * 512 + (j % 4) * 128: iob * 512 + (j % 4) * 128 + NQ],
                                                                        lhsT=w_in_bf.t[:, c, 768 + j * 128: 768 + (j + 1) * 128],
                                                                        rhs=hT.t[:, c * NQ:(c + 1) * NQ], start=(c == 0), stop=(c == 7)),
                                  reads=[hT.b, w_in_parts[c]], writes=[PB[iob]])
                    if i_ != 1:
                        act.op(lambda e, i_=i_, iob=iob: e.copy(out=cv_sb.t[:, 4 * i_:4 * i_ + 4, 0:NQ],
                                                                in_=bank(iob).rearrange("p (c n) -> p c n", c=4)[:, :, 0:NQ]),
                               reads=[PB[iob]], writes=[cv_sb.b])
                    else:
                        dve.op(lambda e, i_=i_, iob=iob: e.tensor_copy(out=cv_sb.t[:, 4 * i_:4 * i_ + 4, 0:NQ],
                                                                       in_=bank(iob).rearrange("p (c n) -> p c n", c=4)[:, :, 0:NQ]),
                               reads=[PB[iob]], writes=[cv_sb.b])
                yield
                qkvs = qkv_sb.t[rows, :]
                xv = qkvs[:, 0:640].rearrange("p (h t d) -> p h t d", h=10, t=2)
                ov = qk_f.t[rows, :].rearrange("p (h t d) -> p h t d", h=10, t=2)
                cb_ = cos_ap.unsqueeze(1).to_broadcast([NQ, 10, 32])
                sb_ = sin_ap.unsqueeze(1).to_broadcast([NQ, 10, 32])
                rq = [qkv_sb.b]
                dve.op(lambda e: e.tensor_tensor(out=r1.t[rows], in0=xv[:, :, 0, :], in1=cb_, op=ALU.mult), reads=rq + [cosp.b, coss.b], writes=[r1.b])
                dve.op(lambda e: e.tensor_tensor(out=r2.t[rows], in0=xv[:, :, 1, :], in1=sb_, op=ALU.mult), reads=rq + [sinp.b, sins.b], writes=[r2.b])
                pool.op(lambda e: e.tensor_tensor(out=ov[:, :, 0, :], in0=r1.t[rows], in1=r2.t[rows], op=ALU.subtract),
                        reads=[r1.b, r2.b], writes=[qk_f.b])
                dve.op(lambda e: e.tensor_tensor(out=r1.t[rows], in0=xv[:, :, 1, :], in1=cb_, op=ALU.mult), reads=rq, writes=[r1.b])
                dve.op(lambda e: e.tensor_tensor(out=r2.t[rows], in0=xv[:, :, 0, :], in1=sb_, op=ALU.mult), reads=rq, writes=[r2.b])
                pool.op(lambda e: e.tensor_tensor(out=ov[:, :, 1, :], in0=r1.t[rows], in1=r2.t[rows], op=ALU.add),
                        reads=[r1.b, r2.b], writes=[qk_f.b])
                act.op(lambda e: e.copy(out=qkd.t[rows, 0:512], in_=qk_f.t[rows, 0:512]), reads=[qk_f.b], writes=[qkd.b])
                kdup = qkd.t[rows, 512:768].rearrange("p (g t d) -> p g t d", g=2, t=2)
                ksrc = qk_f.t[rows, 512:640].rearrange("p (g d) -> p g d", g=2)
                pool.op(lambda e: e.tensor_copy(out=kdup[:, :, 0, :], in_=ksrc), reads=[qk_f.b], writes=[qkd.b])
                pool.op(lambda e: e.tensor_copy(out=kdup[:, :, 1, :], in_=ksrc), reads=[qk_f.b], writes=[qkd.b])
                pool.op(lambda e: e.tensor_copy(out=vb[r3].t[rows, :], in_=qkvs[:, 640:768]), reads=[qkv_sb.b], writes=[vb[r3].b])
                last_p = (kind == "prompt" and bi == NPB)
                if last_p or kind == "sample":
                    pool.op(lambda e: e.tensor_copy(out=v_f.t[rows, :], in_=qkvs[:, 640:768]), reads=[qkv_sb.b], writes=[v_f.b])
                NS, L = (NSEQ_S, 4) if kind == "sample" else (1, 128)
                ue = uexts if kind == "sample" else uext[par]
                W_ = NS * (L + 2)

                def cps(j):
                    return cv_sb.t[:, j, 0:NQ]

                for c in range(4):
                    uv = ue.t[:, c, 0:W_].rearrange("p (s l) -> p s l", s=NS)
                    pool.op(lambda e, c=c, uv=uv: e.tensor_tensor(out=uv[:, :, 2:2 + L], in0=cps(8 + c).rearrange("p (s l) -> p s l", s=NS),
                                                                  in1=cps(c).rearrange("p (s l) -> p s l", s=NS), op=ALU.mult),
                            reads=[cv_sb.b], writes=[ue.b])
                if kind != "sample":
                    prev = uext[1 - par]
                    if bi == 1:
                        pool.op(lambda e: e.tensor_scalar(out=ue.t[:, :, 0:2], in0=prev.t[:, :, 128:130], scalar1=flag.t[:, 0:1], scalar2=None,
                                                          op0=ALU.mult), reads=[prev.b, flag.b], writes=[ue.b])
                    elif bi > 1:
                        pool.op(lambda e: e.tensor_copy(out=ue.t[:, :, 0:2], in_=prev.t[:, :, 128:130]), reads=[prev.b], writes=[ue.b])
                if kind == "halo":
                    tbk = bank_bf(bk["T"])
                    for j in range(2):
                        pe.op(lambda e, j=j: e.transpose(out=tbk[:, j * 128:(j + 1) * 128], in_=qkd.t[:, 512 + j * 128: 640 + j * 128],
                                                         identity=ident.t[:, :]), reads=[qkd.b, ident.b], writes=[PB[bk["T"]]])
                    act.op(lambda e: e.copy(out=kT[r3].t[:].rearrange("p g n -> p (g n)"), in_=tbk[:, 0:256]),
                           reads=[PB[bk["T"]]], writes=[kT[r3].b])
                    return
                for c in range(4):
                    uv = ue.t[:, c, 0:W_].rearrange("p (s l) -> p s l", s=NS)
                    av = cacc.t[:, c, 0:NQ].rearrange("p (s l) -> p s l", s=NS)
                    dve.op(lambda e, c=c, uv=uv, av=av: e.tensor_scalar(out=av, in0=uv[:, :, 0:L], scalar1=convw.t[:, c * 3:c * 3 + 1], scalar2=None,
                                                                      op0=ALU.mult), reads=[ue.b, convw.b], writes=[cacc.b])
                    for k in (1, 2):
                        dve.op(lambda e, c=c, uv=uv, av=av, k=k: e.scalar_tensor_tensor(out=av, in0=uv[:, :, k:k + L],
                                                                                       scalar=convw.t[:, c * 3 + k:c * 3 + k + 1], in1=av,
                                                                                       op0=ALU.mult, op1=ALU.add),
                               reads=[ue.b, convw.b, cacc.b], writes=[cacc.b])
                    pool.op(lambda e, c=c: e.tensor_tensor(out=cacc.t[:, c, 0:NQ], in0=cps(4 + c), in1=cacc.t[:, c, 0:NQ], op=ALU.mult),
                            reads=[cv_sb.b, cacc.b], writes=[cacc.b])
                act.op(lambda e: e.activation(out=csq.t[:, :, 0:NQ], in_=cacc.t[:, :, 0:NQ], func=AF.Square), reads=[cacc.b], writes=[csq.b])
                tbk = bank_bf(bk["T"])
                for j in range(6):
                    pe.op(lambda e, j=j: e.transpose(out=tbk[:, j * NQ:(j + 1) * NQ], in_=qkd.t[rows, j * 128:(j + 1) * 128],
                                                     identity=ident.t[rows, rows]), reads=[qkd.b, ident.b], writes=[PB[bk["T"]]])
                act.op(lambda e: e.copy(out=qT.t[:, :, 0:NQ], in_=tbk[:, 0:4 * NQ].rearrange("p (j n) -> p j n", j=4)),
                       reads=[PB[bk["T"]]], writes=[qT.b])
                dve.op(lambda e: e.tensor_copy(out=kT[r3].t[:, :, 0:NQ], in_=tbk[:, 4 * NQ:6 * NQ].rearrange("p (j n) -> p j n", j=2)),
                       reads=[PB[bk["T"]]], writes=[kT[r3].b])
                yield
                if kind == "sample":
                    def kchunk(kc, g, half):
                        if kc < 16:
                            return kcT.t[half * 64:(half + 1) * 64, g, kc * 128:(kc + 1) * 128], 128, kcT.b
                        return kT[r3].t[half * 64:(half + 1) * 64, g, 0:64], 64, kT[r3].b

                    def vchunk(kc, g):
                        if kc < 16:
                            return vc.t[:, kc, g * 64:(g + 1) * 64], 128, vc.b
                        return vb[r3].t[0:64, g * 64:(g + 1) * 64], 64, vb[r3].b
                    mask_t = masks
                else:
                    def kchunk(kc, g, half):
                        t_ = kT[r3p] if kc == 0 else kT[r3]
                        return t_.t[half * 64:(half + 1) * 64, g, :], 128, t_.b

                    def vchunk(kc, g):
                        t_ = vb[r3p] if kc == 0 else vb[r3]
                        return t_.t[:, g * 64:(g + 1) * 64], 128, t_.b
                    mask_t = mask0 if bi == 1 else maskp
                sbk = bk["S"]
                nsb = (G * NK + 511) // 512
                SB = [PB[sbk + i] for i in range(nsb)]
                ob = bk["O"]
                ptb = bk["PT"]
                npt = 2 if kind == "sample" else 1
                PTB = [PB[ptb + i_] for i_ in range(npt)]
                ptv = bank_bf(ptb, npt)
                for hg in range(8 // G):
                    for gi in range(G):
                        h = hg * G + gi
                        half, g = h % 2, h // 4
                        base = sbk * 512 + gi * NK
                        for kc in range(nchunk):
                            kap, kn, kbuf = kchunk(kc, g, half)
                            pe.op(lambda e, kc=kc, kap=kap, kn=kn, base=base, half=half, h=h: e.matmul(
                                ps[rows, base + kc * 128: base + kc * 128 + kn], lhsT=qT.t[half * 64:(half + 1) * 64, h // 2, 0:NQ], rhs=kap,
                                start=True, stop=False), reads=[qT.b, kbuf], writes=SB)
                            mrow = slice(half * 64, half * 64 + 64) if NQ == 64 else rows
                            pe.op(lambda e, kc=kc, kn=kn, base=base, mrow=mrow: e.matmul(
                                ps[rows, base + kc * 128: base + kc * 128 + kn], lhsT=ident.t[mrow, mrow],
                                rhs=mask_t.t[mrow, kc * 128: kc * 128 + kn], start=False, stop=True),
                                reads=[ident.b, mask_t.b], writes=SB)
                    sv = ps[rows, sbk * 512: sbk * 512 + G * NK].rearrange("p (g n) -> p g n", g=G)
                    h0 = hg * G
                    dve.op(lambda e, h0=h0: e.tensor_reduce(out=sq.t[rows, 8 + h0: 8 + h0 + G], in_=sv, axis=AX.X, op=ALU.max),
                           reads=SB, writes=[sq.b])
                    dve.op(lambda e, h0=h0: e.scalar_tensor_tensor(out=sq.t[rows, 16 + h0:16 + h0 + G], in0=sq.t[rows, 8 + h0: 8 + h0 + G], scalar=-0.125,
                                                                   in1=nsink.t[rows, h0:h0 + G], op0=ALU.mult, op1=ALU.min),
                           reads=[sq.b, nsink.b], writes=[sq.b])
                    for gi in range(G):
                        h = h0 + gi
                        act.op(lambda e, gi=gi, h=h: e.activation(out=P_bf.t[rows, gi * NK:(gi + 1) * NK], in_=sv[:, gi, :], func=AF.Exp,
                                                                  bias=sq.t[rows, 16 + h:17 + h], scale=0.125, accum_out=sq.t[rows, 24 + h:25 + h]),
                               reads=SB + [sq.b], writes=[P_bf.b, sq.b])
                    slot = 0
                    for gi in range(G):
                        for kc in range(nchunk):
                            kn = 64 if (kind == "sample" and kc == 16) else 128
                            pe.op(lambda e, gi=gi, kc=kc, kn=kn, slot=slot: e.transpose(
                                out=ptv[0:kn, slot * NQ:(slot + 1) * NQ], in_=P_bf.t[rows, gi * NK + kc * 128: gi * NK + kc * 128 + kn],
                                identity=ident.t[rows, rows]), reads=[P_bf.b, ident.b], writes=PTB)
                            slot += 1
                    nsl = slot
                    hs = (nsl // 2) * NQ
                    if kind == "sample":
                        if hg % 2 == 0:
                            act.op(lambda e: e.copy(out=PT_sb.t[:, 0:16 * NQ], in_=ptv[:, 0:16 * NQ]), reads=PTB, writes=[PT_sb.b])
                            act.op(lambda e: e.copy(out=PT_sb.t[0:64, 16 * NQ:17 * NQ], in_=ptv[0:64, 16 * NQ:17 * NQ]), reads=PTB, writes=[PT_sb.b])
                        else:
                            dve.op(lambda e: e.tensor_copy(out=PT_sb.t[:, 0:16 * NQ], in_=ptv[:, 0:16 * NQ]), reads=PTB, writes=[PT_sb.b])
                            dve.op(lambda e: e.tensor_copy(out=PT_sb.t[0:64, 16 * NQ:17 * NQ], in_=ptv[0:64, 16 * NQ:17 * NQ]), reads=PTB, writes=[PT_sb.b])
                    elif hg % 2 == 0:
                        act.op(lambda e: e.copy(out=PT_sb.t[:, 0:nsl * NQ], in_=ptv[:, 0:nsl * NQ]), reads=PTB, writes=[PT_sb.b])
                    else:
                        dve.op(lambda e: e.tensor_copy(out=PT_sb.t[:, 0:nsl * NQ], in_=ptv[:, 0:nsl * NQ]), reads=PTB, writes=[PT_sb.b])
                    slot = 0
                    for gi in range(G):
                        h = h0 + gi
                        g = h // 4
                        for kc in range(nchunk):
                            vap, kn, vbuf = vchunk(kc, g)
                            pe.op(lambda e, h=h, kc=kc, vap=vap, kn=kn, slot=slot: e.matmul(
                                ps[rows, ob * 512 + h * 64: ob * 512 + (h + 1) * 64], lhsT=PT_sb.t[0:kn, slot * NQ:(slot + 1) * NQ], rhs=vap,
                                start=(kc == 0), stop=(kc == nchunk - 1)), reads=[PT_sb.b, vbuf], writes=[PB[ob]])
                            slot += 1
                    yield
                dve.op(lambda e: e.tensor_tensor(out=sq.t[rows, 32:40], in0=sq.t[rows, 16:24], in1=psink.t[rows, :], op=ALU.add),
                       reads=[sq.b, psink.b], writes=[sq.b])
                act.op(lambda e: e.activation(out=sq.t[rows, 32:40], in_=sq.t[rows, 32:40], func=AF.Exp), reads=[sq.b], writes=[sq.b])
                dve.op(lambda e: e.tensor_tensor(out=sq.t[rows, 32:40], in0=sq.t[rows, 32:40], in1=sq.t[rows, 24:32], op=ALU.add),
                       reads=[sq.b], writes=[sq.b])
                dve.op(lambda e: e.reciprocal(out=sq.t[rows, 32:40], in_=sq.t[rows, 32:40]), reads=[sq.b], writes=[sq.b])
                ov3 = O_sb.t[rows, :].rearrange("p (h d) -> p h d", h=8)
                dve.op(lambda e: e.tensor_tensor(out=ov3, in0=ps[rows, ob * 512:(ob + 1) * 512].rearrange("p (h d) -> p h d", h=8),
                                                 in1=sq.t[rows, 32:40].unsqueeze(2).to_broadcast([NQ, 8, 64]), op=ALU.mult),
                       reads=[PB[ob], sq.b], writes=[O_sb.b])
                pool.op(lambda e: e.tensor_tensor(out=tmp.t[rows, 0:512], in0=O_sb.t[rows, :], in1=O_sb.t[rows, :], op=ALU.mult),
                        reads=[O_sb.b], writes=[tmp.b])
                dve.op(lambda e: e.tensor_reduce(out=sq.t[rows, 40:48], in_=tmp.t[rows, 0:512].rearrange("p (h d) -> p h d", h=8), axis=AX.X, op=ALU.add),
                       reads=[tmp.b], writes=[sq.b])
                act.op(lambda e: e.activation(out=sq.t[rows, 40:48], in_=sq.t[rows, 40:48], func=AF.Ln, scale=1.0 / 64, bias=EPS),
                       reads=[sq.b], writes=[sq.b])
                act.op(lambda e: e.activation(out=sq.t[rows, 40:48], in_=sq.t[rows, 40:48], func=AF.Exp, scale=-0.5), reads=[sq.b], writes=[sq.b])
                dve.op(lambda e: e.tensor_tensor(out=ov3, in0=ov3, in1=sq.t[rows, 40:48].unsqueeze(2).to_broadcast([NQ, 8, 64]), op=ALU.mult),
                       reads=[O_sb.b, sq.b], writes=[O_sb.b])
                pool.op(lambda e: e.tensor_tensor(out=On_bf.t[rows, :], in0=O_sb.t[rows, :], in1=gnat.t[rows, :], op=ALU.mult),
                        reads=[O_sb.b, gnat.b], writes=[On_bf.b])
                tbo = bank_bf(bk["T2"])
                for j in range(4):
                    pe.op(lambda e, j=j: e.transpose(out=tbo[:, j * NQ:(j + 1) * NQ], in_=On_bf.t[rows, j * 128:(j + 1) * 128],
                                                     identity=ident.t[rows, rows]), reads=[On_bf.b, ident.b], writes=[PB[bk["T2"]]])
                act.op(lambda e: e.copy(out=mixT.t[:, 0:4, 0:NQ], in_=tbo[:, 0:4 * NQ].rearrange("p (j n) -> p j n", j=4)),
                       reads=[PB[bk["T2"]]], writes=[mixT.b])
                gb = bk["GS"]
                for c in range(4):
                    pe.op(lambda e, c=c: e.matmul(ps[:, gb * 512 + c * 128: gb * 512 + c * 128 + NQ], lhsT=g64.t[:, :], rhs=csq.t[:, c, 0:NQ],
                                                  start=True, stop=True), reads=[g64.b, csq.b], writes=[PB[gb]])
                gsv = ps[:, gb * 512:(gb + 1) * 512].rearrange("p (c n) -> p c n", c=4)[:, :, 0:NQ]
                act.op(lambda e: e.activation(out=crs.t[:, :, 0:NQ], in_=gsv, func=AF.Ln, scale=1.0 / 64, bias=EPS), reads=[PB[gb]], writes=[crs.b])
                act.op(lambda e: e.activation(out=crs.t[:, :, 0:NQ], in_=crs.t[:, :, 0:NQ], func=AF.Exp, scale=-0.5), reads=[crs.b], writes=[crs.b])
                pool.op(lambda e: e.tensor_tensor(out=crs.t[:, :, 0:NQ], in0=crs.t[:, :, 0:NQ], in1=cacc.t[:, :, 0:NQ], op=ALU.mult),
                        reads=[crs.b, cacc.b], writes=[crs.b])
                for c in range(4):
                    dve.op(lambda e, c=c: e.tensor_scalar(out=mixT.t[:, 4 + c, 0:NQ], in0=crs.t[:, c, 0:NQ], scalar1=gncv.t[:, c:c + 1], scalar2=None,
                                                          op0=ALU.mult), reads=[crs.b, gncv.b], writes=[mixT.b])
                if kind == "sample":
                    for k in (2, 3, 4):
                        sp.dma(dK, BC[k].t[0:64, :], MODV[k, 1:65, :], reads=[dr["MODV"]], writes=[BC[k].b], group=True)
                    def _fix234():
                        for k in (2, 3, 4):
                            BC[k].b.w = (dK, S.dcount[dK])
                    S.defer(_fix234, writes=[BC[2].b, BC[3].b, BC[4].b])
                mb = bk["MIX"]
                for hf in range(2):
                    for c in range(8):
                        pe.op(lambda e, c=c, hf=hf: e.matmul(ps[rows, (mb + hf) * 512:(mb + hf + 1) * 512], lhsT=mixT.t[:, c, 0:NQ],
                                                             rhs=w_out_bf.t[:, c, hf * 512:(hf + 1) * 512], start=(c == 0), stop=(c == 7)),
                              reads=[mixT.b, w_out_bf.b], writes=[PB[mb + hf]])
                dve.op(lambda e: e.tensor_tensor(out=tmp.t[rows, :], in0=ps[rows, mb * 512:(mb + 2) * 512], in1=BC[2].t[rows, :], op=ALU.mult),
                       reads=[PB[mb], PB[mb + 1], BC[2].b], writes=[tmp.b])
                pool.op(lambda e: e.tensor_tensor(out=x1.t[rows, :], in0=tmp.t[rows, :], in1=xt.t[rows, :], op=ALU.add),
                        reads=[tmp.b, xt.b], writes=[x1.b])
                tok0 = NPB * 128 if kind == "sample" else (bi - 1) * 128
                sp.dma(dOX[par], X1[tok0:tok0 + NQ, :], x1.t[rows, :], reads=[x1.b], writes=[])
                yield
                act.op(lambda e: e.activation(out=junk2.t[rows, :], in_=x1.t[rows, :], func=AF.Square, accum_out=sq.t[rows, 2:3]),
                       reads=[x1.b], writes=[junk2.b, sq.b])
                act.op(lambda e: e.activation(out=sq.t[rows, 3:4], in_=sq.t[rows, 2:3], func=AF.Ln, scale=1.0 / D, bias=EPS),
                       reads=[sq.b], writes=[sq.b])
                act.op(lambda e: e.activation(out=sq.t[rows, 3:4], in_=sq.t[rows, 3:4], func=AF.Exp, scale=-0.5), reads=[sq.b], writes=[sq.b])
                dve.op(lambda e: e.scalar_tensor_tensor(out=tmp.t[rows, :], in0=x1.t[rows, :], scalar=sq.t[rows, 3:4], in1=BC[3].t[rows, :],
                                                        op0=ALU.mult, op1=ALU.mult), reads=[x1.b, sq.b, BC[3].b], writes=[tmp.b])
                pool.op(lambda e: e.tensor_tensor(out=h2_bf.t[rows, :], in0=tmp.t[rows, :], in1=BC[4].t[rows, :], op=ALU.add),
                        reads=[tmp.b, BC[4].b], writes=[h2_bf.b])
                sp.dma(dOH[par], H2[tok0:tok0 + NQ, :], h2_bf.t[rows, :], reads=[h2_bf.b], writes=[])
                tb2 = bank_bf(bk["H2T"])
                for c in range(8):
                    pe.op(lambda e, c=c: e.transpose(out=tb2[:, c * NQ:(c + 1) * NQ], in_=h2_bf.t[rows, c * 128:(c + 1) * 128],
                                                     identity=ident.t[rows, rows]), reads=[h2_bf.b, ident.b], writes=[PB[bk["H2T"]]])
                act.op(lambda e: e.copy(out=h2T.t[:, 0:8 * NQ], in_=tb2[:, 0:8 * NQ]), reads=[PB[bk["H2T"]]], writes=[h2T.b])
                lb = bk["LG"]
                for c in range(8):
                    pe.op(lambda e, c=c: e.matmul(ps[rows, lb * 512: lb * 512 + 36], lhsT=h2T.t[:, c * NQ:(c + 1) * NQ], rhs=w_r_bf.t[:, c, :],
                                                  start=(c == 0), stop=(c == 7)), reads=[h2T.b, w_r_bf.b], writes=[PB[lb]])
                dve.op(lambda e: e.tensor_tensor(out=lg.t[rows, :], in0=ps[rows, lb * 512: lb * 512 + 36], in1=brt.t[rows, :], op=ALU.add),
                       reads=[PB[lb], brt.b], writes=[lg.b])
                ti = NPB if kind == "sample" else bi - 1
                R = rt.t
                dve.op(lambda e: e.memset(R[rows, 0:8], 0.0), writes=[rt.b])
                dve.op(lambda e: e.tensor_reduce(out=R[rows, 0:1], in_=lg.t[rows, 0:4], axis=AX.X, op=ALU.max), reads=[lg.b], writes=[rt.b])
                dve.op(lambda e: e.tensor_scalar(out=R[rows, 1:2], in0=R[rows, 0:1], scalar1=-1.0, scalar2=None, op0=ALU.mult), reads=[rt.b], writes=[rt.b])
                act.op(lambda e: e.activation(out=R[rows, 4:8], in_=lg.t[rows, 0:4], func=AF.Exp, bias=R[rows, 1:2], scale=1.0, accum_out=R[rows, 2:3]),
                       reads=[lg.b, rt.b], writes=[rt.b])
                dve.op(lambda e: e.tensor_scalar(out=R[rows, 8:12], in0=lg.t[rows, 0:4], scalar1=R[rows, 0:1], scalar2=-1.0e30,
                                                 op0=ALU.is_lt, op1=ALU.mult), reads=[lg.b, rt.b], writes=[rt.b])
                dve.op(lambda e: e.tensor_tensor(out=R[rows, 16:48].rearrange("p (g n) -> p g n", g=4),
                                                 in0=lg.t[rows, 4:36].rearrange("p (g n) -> p g n", g=4),
                                                 in1=R[rows, 8:12].unsqueeze(2).to_broadcast([NQ, 4, 8]), op=ALU.add), reads=[lg.b, rt.b], writes=[rt.b])
                dve.op(lambda e: e.max(out=R[rows, 48:56], in_=R[rows, 16:48]), reads=[rt.b], writes=[rt.b])
                dve.op(lambda e: e.tensor_scalar(out=R[rows, 56:57], in0=R[rows, 48:49], scalar1=-1.0, scalar2=None, op0=ALU.mult), reads=[rt.b], writes=[rt.b])
                act.op(lambda e: e.activation(out=R[rows, 64:96], in_=R[rows, 16:48], func=AF.Exp, bias=R[rows, 56:57], scale=1.0),
                       reads=[rt.b], writes=[rt.b])
                selv = sel_all.t[rows, ti, :]
                dve.op(lambda e: e.tensor_scalar(out=selv, in0=R[rows, 16:48], scalar1=R[rows, 49:50], scalar2=None, op0=ALU.is_ge),
                       reads=[rt.b], writes=[sel_all.b])
                dve.op(lambda e: e.tensor_tensor(out=R[rows, 64:96], in0=R[rows, 64:96], in1=selv, op=ALU.mult), reads=[rt.b, sel_all.b], writes=[rt.b])
                dve.op(lambda e: e.tensor_reduce(out=R[rows, 57:58], in_=R[rows, 64:96], axis=AX.X, op=ALU.add), reads=[rt.b], writes=[rt.b])
                dve.op(lambda e: e.tensor_tensor(out=R[rows, 58:59], in0=R[rows, 57:58], in1=R[rows, 2:3], op=ALU.mult), reads=[rt.b], writes=[rt.b])
                dve.op(lambda e: e.reciprocal(out=R[rows, 58:59], in_=R[rows, 58:59]), reads=[rt.b], writes=[rt.b])
                dve.op(lambda e: e.tensor_scalar(out=gate_all.t[rows, ti, :], in0=R[rows, 64:96], scalar1=R[rows, 58:59], scalar2=None, op0=ALU.mult),
                       reads=[rt.b], writes=[gate_all.b])
                for i_, (l_, r_) in enumerate(((triu.t[rows, rows], selv), (onesf.t[:, rows], cumsel.t[:, :]))):
                    pe.op(lambda e, l_=l_, r_=r_, i_=i_: e.matmul(ps[rows, lb * 512 + 64: lb * 512 + 96], lhsT=l_, rhs=r_, start=(i_ == 0), stop=(i_ == 1)),
                          reads=[triu.b, onesf.b, sel_all.b, cumsel.b], writes=[PB[lb]])
                dve.op(lambda e: e.tensor_tensor(out=rs_all.t[rows, ti, :], in0=ps[rows, lb * 512 + 64: lb * 512 + 96], in1=selv, op=ALU.mult),
                       reads=[PB[lb], sel_all.b], writes=[rs_all.b])
                pool.op(lambda e: e.tensor_tensor(out=cumsel.t[rows, :], in0=cumsel.t[rows, :], in1=selv, op=ALU.add),
                        reads=[cumsel.b, sel_all.b], writes=[cumsel.b])
                if last_p:
                    act.dma(dNK, nk_p, qk_f.t[:, 512:640], reads=[qk_f.b], writes=[])
                    act.dma(dNV, nv_p, v_f.t[:, :], reads=[v_f.b], writes=[])
                    for c in range(4):
                        act.dma(dOut, ncv_p[:, c * 128:(c + 1) * 128].rearrange("j p -> p j"), ue.t[:, c, 128:130], reads=[ue.b], writes=[],
                                group=True, allow_slow_non_contiguous=True)
                if kind == "sample":
                    act.dma(dOut, nk_s[:, 0:124, :], ck_nat[:, 4:128, :], group=True)
                    act.dma(dOut, nv_s[:, 0:124, :], cv_nat[:, 4:128, :], group=True)
                    for s_ in range(NSEQ_S):
                        act.dma(dOut, nk_s[s_, 124:128, :], qk_f.t[4 * s_:4 * s_ + 4, 512:640], reads=[qk_f.b], writes=[], group=True)
                        act.dma(dOut, nv_s[s_, 124:128, :], v_f.t[4 * s_:4 * s_ + 4, :], reads=[v_f.b], writes=[], group=True)
                    for c in range(4):
                        for j_ in range(2):
                            act.dma(dOut, ncv_s[:, j_, c * 128:(c + 1) * 128].rearrange("s p -> p s"),
                                    ue.t[:, c, :].rearrange("p (s l) -> p s l", s=NSEQ_S)[:, :, 4 + j_], reads=[ue.b], writes=[],
                                    group=True, allow_slow_non_contiguous=True)

            progs = [S.record(block(0, "halo"))] + [S.record(block(bi, "prompt")) for bi in range(1, NBLK)]
            progs.append(S.record(block(NBLK, "sample")))
            S.merge_emit(progs, window=PIPE_WINDOW)
            for k_ in ("X1", "H2"):
                dr[k_].w = None
                dr[k_].r = []
            barrier_all(S, dOX + dOH + [dNK, dNV])

        sW.close()
        with ExitStack() as s2:
            cnt = mk(s2, "cnt", [128, NE], F32)
            thr = mk(s2, "thr", [128, 128], F32)
            big = mk(s2, "big", [128, NB * NE], F32)
            nblk = mk(s2, "nblk", [128, NE], F32)
            pend = [mk(s2, "pend%d" % i, [128, NE], F32) for i in range(2)]
            pstart = mk(s2, "pstart", [128, NE], F32)
            blke = mk(s2, "blke", [128, NB], F32)
            widx_f = mk(s2, "widx_f", [128, NB], F32)
            widx = mk(s2, "widx", [128, NB], I32)
            piota = mk(s2, "piota", [128, 1], F32)
            tokid = mk(s2, "tokid", [128, NTT], I32)
            dmt = mk(s2, "dmt", [128, NTT, NE], F32)
            eqt = mk(s2, "eqt", [128, NTT, NE], F32)
            dA = mk(s2, "dA", [128, NTT], F32)
            dB = mk(s2, "dB", [128, NTT], F32)
            gA = mk(s2, "gA", [128, NTT], F32)
            gB = mk(s2, "gB", [128, NTT], F32)
            dAi = mk(s2, "dAi", [128, NTT], I32)
            dBi = mk(s2, "dBi", [128, NTT], I32)
            twA = mk(s2, "twA", [128, NTT, 4], I32)
            twB = mk(s2, "twB", [128, NTT, 4], I32)
            d2 = S.dsem()
            sp.dma(d2, thr.t[:], thr_d.partition_broadcast(128), writes=[thr.b], group=True)
            sp.dma(d2, piota.t[:], piota_d, writes=[piota.b], group=True)
            sp.dma(d2, tokid.t[:], tokid_d, writes=[tokid.b], group=True)
            for t_ in (thr, piota, tokid):
                t_.b.w = (d2, S.dcount[d2])
            pe.op(lambda e: e.matmul(bank(0)[:, 0:NE], lhsT=onesf.t[:, :], rhs=cumsel.t[:, :], start=True, stop=True),
                  reads=[onesf.b, cumsel.b], writes=[PB[0]])
            dve.op(lambda e: e.tensor_copy(out=cnt.t[:], in_=bank(0)[:, 0:NE]), reads=[PB[0]], writes=[cnt.b])
            bv = big.t[:, 0:NE * 66].rearrange("p (e j) -> p e j", e=NE)
            dve.op(lambda e: e.tensor_tensor(out=bv, in0=cnt.t[:].unsqueeze(2).to_broadcast([128, NE, 66]),
                                             in1=thr.t[:, 0:66].unsqueeze(1).to_broadcast([128, NE, 66]), op=ALU.is_gt),
                   reads=[cnt.b, thr.b], writes=[big.b])
            dve.op(lambda e: e.tensor_reduce(out=nblk.t[:], in_=bv, axis=AX.X, op=ALU.add), reads=[big.b], writes=[nblk.b])
            dve.op(lambda e: e.tensor_scalar(out=nblk.t[:], in0=nblk.t[:], scalar1=128.0, scalar2=None, op0=ALU.mult), reads=[nblk.b], writes=[nblk.b])
            dve.op(lambda e: e.tensor_copy(out=pend[0].t[:], in_=nblk.t[:]), reads=[nblk.b], writes=[pend[0].b])
            cur = 0
            for sh in (1, 2, 4, 8, 16):
                a_, b_ = pend[cur], pend[1 - cur]
                dve.op(lambda e, a_=a_, b_=b_: e.tensor_copy(out=b_.t[:, 0:sh], in_=a_.t[:, 0:sh]), reads=[a_.b], writes=[b_.b])
                dve.op(lambda e, a_=a_, b_=b_, sh=sh: e.tensor_tensor(out=b_.t[:, sh:NE], in0=a_.t[:, sh:NE], in1=a_.t[:, 0:NE - sh], op=ALU.add),
                       reads=[a_.b], writes=[b_.b])
                cur = 1 - cur
            pe_ = pend[cur]
            dve.op(lambda e: e.tensor_tensor(out=pstart.t[:], in0=pe_.t[:], in1=nblk.t[:], op=ALU.subtract), reads=[pe_.b, nblk.b], writes=[pstart.b])
            bv2 = big.t[:, 0:NB * NE].rearrange("p (b e) -> p b e", b=NB)
            dve.op(lambda e: e.tensor_tensor(out=bv2, in0=pe_.t[:].unsqueeze(1).to_broadcast([128, NB, NE]),
                                             in1=thr.t[:, 0:NB].unsqueeze(2).to_broadcast([128, NB, NE]), op=ALU.is_le),
                   reads=[pe_.b, thr.b], writes=[big.b])
            dve.op(lambda e: e.tensor_reduce(out=blke.t[:], in_=bv2, axis=AX.X, op=ALU.add), reads=[big.b], writes=[blke.b])
            dve.op(lambda e: e.tensor_scalar(out=blke.t[:], in0=blke.t[:], scalar1=float(NE - 1), scalar2=None, op0=ALU.min), reads=[blke.b], writes=[blke.b])
            dve.op(lambda e: e.tensor_scalar(out=widx_f.t[:], in0=blke.t[:], scalar1=128.0, scalar2=piota.t[:, 0:1], op0=ALU.mult, op1=ALU.add),
                   reads=[blke.b, piota.b], writes=[widx_f.b])
            dve.op(lambda e: e.tensor_tensor(out=big.t[:, 0:NB - 1], in0=blke.t[:, 1:NB], in1=blke.t[:, 0:NB - 1], op=ALU.is_equal),
                   reads=[blke.b], writes=[big.b])
            dve.op(lambda e: e.scalar_tensor_tensor(out=widx_f.t[:, 1:NB], in0=big.t[:, 0:NB - 1], scalar=OOB, in1=widx_f.t[:, 1:NB],
                                                    op0=ALU.mult, op1=ALU.add), reads=[big.b, widx_f.b], writes=[widx_f.b])
            for b0 in STREAM_STARTS[1:NS_W]:
                dve.op(lambda e, b0=b0: e.tensor_scalar(out=widx_f.t[:, b0:b0 + 1], in0=blke.t[:, b0:b0 + 1], scalar1=128.0, scalar2=piota.t[:, 0:1],
                                                        op0=ALU.mult, op1=ALU.add), reads=[blke.b, piota.b, widx_f.b], writes=[widx_f.b])
            dve.op(lambda e: e.tensor_copy(out=widx.t[:], in_=widx_f.t[:]), reads=[widx_f.b], writes=[widx.b])
            dve.op(lambda e: e.tensor_tensor(out=dmt.t[:], in0=sel_all.t[:], in1=pstart.t[:].unsqueeze(1).to_broadcast([128, NTT, NE]), op=ALU.mult),
                   reads=[sel_all.b, pstart.b], writes=[dmt.b])
            dve.op(lambda e: e.tensor_tensor(out=dmt.t[:], in0=dmt.t[:], in1=rs_all.t[:], op=ALU.add), reads=[dmt.b, rs_all.b], writes=[dmt.b])
            dve.op(lambda e: e.tensor_reduce(out=dA.t[:], in_=dmt.t[:], axis=AX.X, op=ALU.max), reads=[dmt.b], writes=[dA.b])
            dve.op(lambda e: e.tensor_reduce(out=dB.t[:], in_=dmt.t[:], axis=AX.X, op=ALU.add), reads=[dmt.b], writes=[dB.b])
            dve.op(lambda e: e.tensor_tensor(out=dB.t[:], in0=dB.t[:], in1=dA.t[:], op=ALU.subtract), reads=[dB.b, dA.b], writes=[dB.b])
            dve.op(lambda e: e.tensor_tensor(out=eqt.t[:], in0=dmt.t[:], in1=dA.t[:].unsqueeze(2).to_broadcast([128, NTT, NE]), op=ALU.is_equal),
                   reads=[dmt.b, dA.b], writes=[eqt.b])
            dve.op(lambda e: e.tensor_tensor(out=eqt.t[:], in0=eqt.t[:], in1=gate_all.t[:], op=ALU.mult), reads=[eqt.b, gate_all.b], writes=[eqt.b])
            dve.op(lambda e: e.tensor_reduce(out=gA.t[:], in_=eqt.t[:], axis=AX.X, op=ALU.add), reads=[eqt.b], writes=[gA.b])
            dve.op(lambda e: e.tensor_reduce(out=gB.t[:], in_=gate_all.t[:], axis=AX.X, op=ALU.add), reads=[gate_all.b], writes=[gB.b])
            dve.op(lambda e: e.tensor_tensor(out=gB.t[:], in0=gB.t[:], in1=gA.t[:], op=ALU.subtract), reads=[gB.b, gA.b], writes=[gB.b])
            dve.op(lambda e: e.tensor_copy(out=dAi.t[:], in_=dA.t[:]), reads=[dA.b], writes=[dAi.b])
            dve.op(lambda e: e.tensor_copy(out=dBi.t[:], in_=dB.t[:]), reads=[dB.b], writes=[dBi.b])
            for tw_, g_, off in ((twA, gA, 0), (twB, gB, HROWS)):
                pool.op(lambda e, tw_=tw_: e.memset(tw_.t[:], 0), writes=[tw_.b])
                pool.op(lambda e, tw_=tw_: e.tensor_copy(out=tw_.t[:, :, 0], in_=tokid.t[:]), reads=[tokid.b], writes=[tw_.b])
                pool.op(lambda e, tw_=tw_, off=off: e.tensor_single_scalar(out=tw_.t[:, :, 1], in_=tokid.t[:], scalar=off, op=ALU.add),
                        reads=[tokid.b], writes=[tw_.b])
                pool.op(lambda e, tw_=tw_, g_=g_: e.tensor_copy(out=tw_.t[:].bitcast(F32)[:, :, 2], in_=g_.t[:]), reads=[g_.b], writes=[tw_.b])
            dScs = [S.dsem() for _ in range(6)]
            nsc = 0
            for ti in range(NTT):
                nq = 64 if ti == NPB else 128
                for tw_, di_ in ((twA, dAi), (twB, dBi)):
                    dSc = dScs[nsc % 6]
                    nsc += 1
                    pool.dma(dSc, TW[:, :], tw_.t[0:nq, ti, :], reads=[tw_.b, di_.b, dr["TW"]], writes=[],
                             indirect=dict(out_offset=bass.IndirectOffsetOnAxis(ap=di_.t[0:nq, ti:ti + 1], axis=0), in_offset=None,
                                           bounds_check=bc_tw, oob_is_err=False))
            for dSc in dScs:
                pool.wait_ev((dSc, S.dcount[dSc]))
                sp.wait_ev((dSc, S.dcount[dSc]))
            dr["TW"].w = None
            dr["TW"].r = []

            with ExitStack() as s3:
                Wb = [mk(s3, "Wb%d" % i, [128, WROW], BF16) for i in range(NS_W)]
                order = []
                for i_ in range(max(STREAM_STARTS[k_ + 1] - STREAM_STARTS[k_] for k_ in range(NS_W))):
                    for k_ in range(NS_W):
                        if STREAM_STARTS[k_] + i_ < STREAM_STARTS[k_ + 1]:
                            order.append((STREAM_STARTS[k_] + i_, k_))
                assert sorted(b_ for b_, _ in order) == list(range(NB))
                twb = [mk(s3, "twb%d" % i, [128, 4], I32) for i in range(4)]
                xg = [mk(s3, "xg%d" % i, [128, D], BF16) for i in range(2)]
                xgT = [mk(s3, "xgT%d" % i, [128, 8, 128], BF16) for i in range(2)]
                sg = [mk(s3, "sg%d" % i, [128, 512], F32) for i in range(2)]
                aT = [mk(s3, "aT%d" % i, [128, 4, 128], BF16) for i in range(2)]
                ysb = [mk(s3, "ysb%d" % i, [128, D], F32) for i in range(2)]
                dWg = [S.dsem() for _ in range(NS_W)]
                dTw = [S.dsem() for _ in range(4)]
                dXg = [S.dsem(), S.dsem()]
                dYs = [S.dsem(), S.dsem()]

                def load_tw(pos):
                    b, ws = order[pos]
                    p4 = pos % 4
                    sp.dma(dTw[p4], twb[p4].t[:], TW[b * 128:(b + 1) * 128, :], reads=[dr["TW"]], writes=[twb[p4].b])

                def load_w(pos):
                    b, ws = order[pos]
                    pool.dma(dWg[ws], Wb[ws].t[:, :], wall[:, :], reads=[widx.b], writes=[Wb[ws].b],
                             indirect=dict(out_offset=None, in_offset=bass.IndirectOffsetOnAxis(ap=widx.t[:, b:b + 1], axis=0),
                                           bounds_check=bc_w, oob_is_err=False))

                def load_x(pos):
                    p2, p4 = pos % 2, pos % 4
                    pool.dma(dXg[p2], xg[p2].t[:, :], H2[:, :], reads=[twb[p4].b, dr["H2"]], writes=[xg[p2].b],
                             indirect=dict(out_offset=None, in_offset=bass.IndirectOffsetOnAxis(ap=twb[p4].t[:, 0:1], axis=0)))

                for pos in range(3):
                    load_tw(pos)
                for pos in range(NS_W):
                    load_w(pos)
                load_x(0)
                load_x(1)
                for pos in range(NB):
                    if pos + 3 < NB:
                        load_tw(pos + 3)
                    b, ws = order[pos]
                    p2, p4 = pos % 2, pos % 4
                    W = Wb[ws]
                    tbm = bank_bf(0)
                    for c in range(8):
                        pe.op(lambda e, c=c: e.transpose(out=tbm[:, c * 128:(c + 1) * 128], in_=xg[p2].t[:, c * 128:(c + 1) * 128], identity=ident.t[:, :]),
                              reads=[xg[p2].b, ident.b], writes=[PB[0]])
                    act.op(lambda e: e.copy(out=xgT[p2].t[:].rearrange("p c n -> p (c n)"), in_=tbm[:, 0:1024]), reads=[PB[0]], writes=[xgT[p2].b])
                    if pos + 2 < NB:
                        load_x(pos + 2)
                    gbk, ubk = 1 + p2, 3 + p2
                    for (bk_, off) in ((gbk, 0), (ubk, 4096)):
                        for j in range(4):
                            for c in range(8):
                                pe.op(lambda e, c=c, j=j, bk_=bk_, off=off: e.matmul(
                                    ps[:, bk_ * 512 + j * 128: bk_ * 512 + (j + 1) * 128],
                                    lhsT=W.t[:, off + c * 512 + j * 128: off + c * 512 + (j + 1) * 128], rhs=xgT[p2].t[:, c, :],
                                    start=(c == 0), stop=(c == 7)), reads=[W.b, xgT[p2].b], writes=[PB[bk_]])
                    act.op(lambda e: e.activation(out=sg[p2].t[:], in_=bank(gbk), func=AF.Silu), reads=[PB[gbk]], writes=[sg[p2].b])
                    dve.op(lambda e: e.tensor_tensor(out=aT[p2].t[:].rearrange("p j n -> p (j n)"), in0=bank(ubk), in1=sg[p2].t[:], op=ALU.mult),
                           reads=[PB[ubk], sg[p2].b], writes=[aT[p2].b])
                    for hf in range(2):
                        for j in range(4):
                            pe.op(lambda e, hf=hf, j=j: e.matmul(ps[:, (5 + hf) * 512:(6 + hf) * 512], lhsT=aT[p2].t[:, j, :],
                                                                 rhs=W.t[:, 8192 + j * 1024 + hf * 512: 8192 + j * 1024 + (hf + 1) * 512],
                                                                 start=(j == 0), stop=(j == 3)), reads=[aT[p2].b, W.b], writes=[PB[5 + hf]])
                    gcol = twb[p4].t[:].bitcast(F32)[:, 2:3]
                    act.op(lambda e: e.mul(out=ysb[p2].t[:, 0:512], in_=bank(5), mul=gcol),
                           reads=[PB[5], twb[p4].b], writes=[ysb[p2].b])
                    dve.op(lambda e: e.tensor_scalar(out=ysb[p2].t[:, 512:1024], in0=bank(6), scalar1=gcol, scalar2=None, op0=ALU.mult),
                           reads=[PB[6], twb[p4].b], writes=[ysb[p2].b])
                    pool.dma(dYs[p2], Y[:, :], ysb[p2].t[:, :], reads=[ysb[p2].b, twb[p4].b], writes=[],
                             indirect=dict(out_offset=bass.IndirectOffsetOnAxis(ap=twb[p4].t[:, 1:2], axis=0), in_offset=None,
                                           bounds_check=bc_y, oob_is_err=False))
                    if pos + NS_W < NB:
                        load_w(pos + NS_W)
                for k_ in dYs:
                    pool.wait_ev((k_, S.dcount[k_]))
                    sp.wait_ev((k_, S.dcount[k_]))
                barrier_all(S)

        with ExitStack() as s4:
            GFp = mk(s4, "GFp", [128, D], F32)
            GFs = mk(s4, "GFs", [64, D], F32)
            FN = mk(s4, "FN", [128, D], F32)
            d4 = S.dsem()
            sp.dma(d4, GFp.t[:], MODV[5, 0:1, :].partition_broadcast(128), writes=[GFp.b], group=True)
            sp.dma(d4, GFs.t[:, :], MODV[5, 1:65, :], writes=[GFs.b], group=True)
            sp.dma(d4, FN.t[:], final_norm.partition_broadcast(128), writes=[FN.b], group=True)
            for t_ in (GFp, GFs, FN):
                t_.b.w = (d4, S.dcount[d4])
            NR = 3
            xa = [mk(s4, "xa%d" % i, [128, D], F32) for i in range(NR)]
            ya = [mk(s4, "ya%d" % i, [128, D], F32) for i in range(NR)]
            yb = [mk(s4, "yb%d" % i, [128, D], F32) for i in range(NR)]
            yo = [mk(s4, "yo%d" % i, [128, D], F32) for i in range(NR)]
            jk = mk(s4, "jk", [128, D], BF16)
            sf = [mk(s4, "sf%d" % i, [128, 4], F32) for i in range(NR)]
            dL = [S.dsem() for _ in range(NR)]
            dSt = [S.dsem() for _ in range(NR)]

            def ftile(ti):
                p = ti % NR
                nq = 64 if ti == NPB else 128
                r_ = slice(0, nq)
                t0 = ti * 128
                GF = GFs if ti == NPB else GFp
                sp.dma(dL[p], xa[p].t[r_, :], X1[t0:t0 + nq, :], writes=[xa[p].b], group=True)
                sp.dma(dL[p], ya[p].t[r_, :], Y[t0:t0 + nq, :], writes=[ya[p].b], group=True)
                sp.dma(dL[p], yb[p].t[r_, :], Y[HROWS + t0:HROWS + t0 + nq, :], writes=[yb[p].b], group=True)

                def _fix():
                    for t_ in (xa[p], ya[p], yb[p]):
                        t_.b.w = (dL[p], S.dcount[dL[p]])
                S.defer(_fix, writes=[xa[p].b, ya[p].b, yb[p].b])
                dve.op(lambda e: e.tensor_tensor(out=ya[p].t[r_, :], in0=ya[p].t[r_, :], in1=yb[p].t[r_, :], op=ALU.add),
                       reads=[ya[p].b, yb[p].b], writes=[ya[p].b])
                dve.op(lambda e: e.tensor_tensor(out=ya[p].t[r_, :], in0=ya[p].t[r_, :], in1=GF.t[r_, :], op=ALU.mult),
                       reads=[ya[p].b, GF.b], writes=[ya[p].b])
                pool.op(lambda e: e.tensor_tensor(out=xa[p].t[r_, :], in0=xa[p].t[r_, :], in1=ya[p].t[r_, :], op=ALU.add),
                        reads=[xa[p].b, ya[p].b], writes=[xa[p].b])
                dve.op(lambda e: e.memset(sf[p].t[:], 0.0), writes=[sf[p].b])
                act.op(lambda e: e.activation(out=jk.t[r_, :], in_=xa[p].t[r_, :], func=AF.Square, accum_out=sf[p].t[r_, 0:1]),
                       reads=[xa[p].b, sf[p].b], writes=[jk.b, sf[p].b])
                act.op(lambda e: e.activation(out=sf[p].t[r_, 1:2], in_=sf[p].t[r_, 0:1], func=AF.Ln, scale=1.0 / D, bias=EPS),
                       reads=[sf[p].b], writes=[sf[p].b])
                act.op(lambda e: e.activation(out=sf[p].t[r_, 1:2], in_=sf[p].t[r_, 1:2], func=AF.Exp, scale=-0.5), reads=[sf[p].b], writes=[sf[p].b])
                dve.op(lambda e: e.scalar_tensor_tensor(out=yo[p].t[r_, :], in0=xa[p].t[r_, :], scalar=sf[p].t[r_, 1:2], in1=FN.t[r_, :],
                                                        op0=ALU.mult, op1=ALU.mult), reads=[xa[p].b, sf[p].b, FN.b], writes=[yo[p].b])
                dst = y_s if ti == NPB else y_p[t0:t0 + nq, :]
                sp.dma(dSt[p], dst, yo[p].t[r_, :], reads=[yo[p].b], writes=[])
                yield

            S.merge_emit([S.record(ftile(ti)) for ti in range(NTT)], window=NR)
            for k_ in dSt:
                sp.wait_ev((k_, S.dcount[k_]))
            sp.wait_ev((dOut, S.dcount[dOut]))
            barrier_all(S)
    return nc


def barrier_all(S, dsems=()):
    snap = {k: e.n for k, e in S.E.items()}
    for name, e in S.E.items():
        for k, v in snap.items():
            if k == name or v == 0:
                continue
            e.wait_ev((k, v))
        for k in dsems:
            if S.dcount[k] > 0:
                e.wait_ev((k, S.dcount[k]))


_CACHE = {}


def _consts():
    if "c" in _CACHE:
        return _CACHE["c"]
    bf = ml_dtypes.bfloat16
    c = {}
    c["ident"] = np.eye(128, dtype=np.float32).astype(bf)
    c["triu"] = np.triu(np.ones((128, 128), np.float32), 1)
    c["ones"] = np.ones((128, 128), np.float32)
    g = np.zeros((128, 128), np.float32)
    g[:64, :64] = 1
    g[64:, 64:] = 1
    c["g64"] = g
    i = np.arange(128)[:, None]
    j = np.arange(128)[None, :]
    prev = np.where(j > i, 0.0, NEG)
    cur = np.where(j <= i, 0.0, NEG)
    c["maskp"] = np.concatenate([prev, cur], 1).astype(np.float32).astype(bf)
    c["mask0_first"] = np.concatenate([np.full((128, 128), NEG), cur], 1).astype(np.float32).astype(bf)
    ms = np.full((64, 2112), NEG, np.float32)
    for t in range(64):
        s, jj = t // 4, t % 4
        ms[t, s * 128 + jj + 1:(s + 1) * 128] = 0.0
        ms[t, 2048 + 4 * s: 2048 + 4 * s + jj + 1] = 0.0
    c["masks"] = np.concatenate([ms, ms], 0).astype(bf)
    c["thr"] = (128.0 * np.arange(128, dtype=np.float32))[None, :]
    c["piota"] = np.arange(128, dtype=np.float32)[:, None]
    tok = (np.arange(NTT)[None, :] * 128 + np.arange(128)[:, None]).astype(np.int32)
    c["tokid"] = tok
    tw = np.zeros((128, 4), np.int32)
    tw[:, 0] = NTOK
    tw[:, 1] = 2 * HROWS
    c["twinit"] = tw
    _CACHE["c"] = c
    return c


def _rope_tables(pos):
    half = 32
    inv = (10000.0 ** (-np.arange(half, dtype=np.float32) / half)).astype(np.float32)
    ang = pos.astype(np.float32)[:, None] * inv[None, :]
    return np.cos(ang).astype(np.float32), np.sin(ang).astype(np.float32)


def kernel(x_prompt, x_sample, cache_k, cache_v, state_conv, c_prompt, c_sample,
           attn_norm, ffn_norm, w_mod, b_mod, w_in, conv_w, attn_sinks,
           out_norm_attn, out_norm_conv, w_out, w_group, b_group, w_expert, b_expert,
           w_gate, w_up, w_down, final_norm):
    f = lambda a: np.ascontiguousarray(np.asarray(a, dtype=np.float32))
    x_prompt, x_sample, cache_k, cache_v, state_conv = map(f, (x_prompt, x_sample, cache_k, cache_v, state_conv))
    c_prompt, c_sample = f(c_prompt), f(c_sample)
    C = _consts()
    if "nc" not in _CACHE:
        _CACHE["nc"] = build_program()
    nc = _CACHE["nc"]

    wg, wu, wd = f(w_gate)[0], f(w_up)[0], f(w_down)[0]
    wall = np.concatenate([
        wg.reshape(NE, 8, 128, 512).transpose(0, 2, 1, 3).reshape(NE, 128, 4096),
        wu.reshape(NE, 8, 128, 512).transpose(0, 2, 1, 3).reshape(NE, 128, 4096),
        wd.reshape(NE, 4, 128, 1024).transpose(0, 2, 1, 3).reshape(NE, 128, 4096)], axis=2).reshape(NE * 128, WROW)
    wall = np.ascontiguousarray(wall)
    shared = {
        "w_mod": f(w_mod)[0], "b_mod": f(b_mod)[0][None, :], "attn_norm": f(attn_norm)[0][None, :], "ffn_norm": f(ffn_norm)[0][None, :],
        "final_norm": f(final_norm)[None, :], "w_in": f(w_in)[0], "w_out": f(w_out)[0],
        "w_r": np.ascontiguousarray(np.concatenate([f(w_group)[0], f(w_expert)[0]], 1)),
        "b_r": np.concatenate([f(b_group)[0], f(b_expert)[0]])[None, :],
        "convwT": np.ascontiguousarray(f(conv_w)[0].reshape(3, 4, 128).transpose(2, 1, 0).reshape(128, 12)),
        "sinks": f(attn_sinks)[0][None, :], "gn_attn": f(out_norm_attn)[0][None, :],
        "gn_convT": np.ascontiguousarray(f(out_norm_conv)[0].reshape(4, 128).T),
        "wall": wall,
        "ident": C["ident"], "triu": C["triu"], "ones": C["ones"], "g64": C["g64"], "maskp": C["maskp"], "masks": C["masks"],
        "tokid": C["tokid"], "piota": C["piota"], "thr": C["thr"], "twinit": C["twinit"],
    }
    cs_s, sn_s = _rope_tables(16384 + (np.arange(64) % 4))
    in_maps = []
    for i in range(8):
        seq, half = i // 2, i % 2
        xpc = np.zeros((NBLK * 128, D), np.float32)
        xpc[128:] = x_prompt[seq, half * 4096:(half + 1) * 4096]
        if half == 1:
            xpc[:128] = x_prompt[seq, 4096 - 128:4096]
        pos = half * 4096 - 128 + np.arange(NBLK * 128)
        cs, sn = _rope_tables(np.maximum(pos, 0))
        ss = slice(16 * i, 16 * i + 16)
        ck = cache_k[0, ss]
        kct = ck.transpose(2, 3, 0, 1).reshape(2, 64, 16 * 128)
        kct = np.concatenate([kct, kct], axis=1).transpose(1, 0, 2)
        m = dict(shared)
        m.update({
            "xp": xpc, "xs": np.ascontiguousarray(x_sample[ss].reshape(64, D)),
            "cT": np.ascontiguousarray(np.concatenate([c_prompt[seq:seq + 1], np.repeat(c_sample[ss], 4, axis=0)], 0).T),
            "kcT": np.ascontiguousarray(kct),
            "vc": np.ascontiguousarray(cache_v[0, ss].reshape(16, 128, 128).transpose(1, 0, 2)),
            "ck_nat": np.ascontiguousarray(ck.reshape(16, 128, 128)),
            "cv_nat": np.ascontiguousarray(cache_v[0, ss].reshape(16, 128, 128)),
            "stT": np.ascontiguousarray(state_conv[0, ss].reshape(16, 2, 4, 128).transpose(3, 2, 0, 1)),
            "mask0": C["mask0_first"] if half == 0 else C["maskp"],
            "cosp": np.ascontiguousarray(cs.reshape(NBLK, 128, 32).transpose(1, 0, 2)),
            "sinp": np.ascontiguousarray(sn.reshape(NBLK, 128, 32).transpose(1, 0, 2)),
            "coss": cs_s, "sins": sn_s,
            "flag": np.full((128, 1), float(half), np.float32),
        })
        in_maps.append(m)
    res = run_bass_kernel_spmd(nc, in_maps, core_ids=list(range(8)))
    R = res.results
    y_prompt = np.stack([np.concatenate([R[2 * s]["y_p"], R[2 * s + 1]["y_p"]], 0) for s in range(4)], 0)
    y_sample = np.concatenate([R[i]["y_s"].reshape(16, 4, D) for i in range(8)], 0)
    nkp = np.stack([R[2 * s + 1]["nk_p"].reshape(128, 2, 64) for s in range(4)], 0)[None]
    nvp = np.stack([R[2 * s + 1]["nv_p"].reshape(128, 2, 64) for s in range(4)], 0)[None]
    ncp = np.stack([R[2 * s + 1]["ncv_p"] for s in range(4)], 0)[None]
    nks = np.concatenate([R[i]["nk_s"].reshape(16, 128, 2, 64) for i in range(8)], 0)[None]
    nvs = np.concatenate([R[i]["nv_s"].reshape(16, 128, 2, 64) for i in range(8)], 0)[None]
    ncs = np.concatenate([R[i]["ncv_s"] for i in range(8)], 0)[None]
    return (y_prompt.astype(np.float32), y_sample.astype(np.float32), nkp.astype(np.float32), nvp.astype(np.float32),
            ncp.astype(np.float32), nks.astype(np.float32), nvs.astype(np.float32), ncs.astype(np.float32))
```
